# Optimizing a Trainium2 kernel written in Bass

```python
import jax, jax.numpy as jnp
from jax import lax
import numpy as np

D_MODEL = 1024
BATCH = 8
SEQ = 4096
DEPTH = 1

EPS = 1e-6
D_MIX = D_MODEL
GLA_HEADS = 4
GLA_DK = 64
GLA_DV = 128
GLA_GATE_RANK = 16
GLA_GATE_NORM = 16.0
GLA_CHUNK = 64
MLA_HEADS = 8
MLA_NOPE = 64
MLA_ROPE = 32
MLA_DV = 64
MLA_Q_RANK = 384
MLA_KV_RANK = 256
ROPE_THETA = 10000.0
Q_BLOCK = 128
N_GROUPS = 4
EXPERTS_PER_GROUP = 8
N_EXPERTS = N_GROUPS * EXPERTS_PER_GROUP
TOP_K = 2
D_EXPERT = 256
EXPERT_BLOCK = 128

IN_WIDTHS = (GLA_HEADS * GLA_DK, GLA_HEADS * GLA_DK, GLA_HEADS * GLA_DV, GLA_GATE_RANK,
             GLA_HEADS * GLA_DV, MLA_Q_RANK, MLA_KV_RANK, MLA_ROPE)
D_IN = 2 * GLA_HEADS * GLA_DK + 2 * GLA_HEADS * GLA_DV + GLA_GATE_RANK + MLA_Q_RANK + MLA_KV_RANK + MLA_ROPE

kernel_name = "hybrid_gla_mla_hier_moe"


def _split_points():
    pts, acc = [], 0
    for w in IN_WIDTHS[:-1]:
        acc += w
        pts.append(acc)
    return pts


def rmsnorm(x, w):
    x32 = x.astype(jnp.float32)
    y = x32 * lax.rsqrt(jnp.mean(x32 * x32, axis=-1, keepdims=True) + EPS)
    return (y * w.astype(jnp.float32)).astype(x.dtype)


def rope(x, pos):
    half = x.shape[-1] // 2
    inv = ROPE_THETA ** (-jnp.arange(half, dtype=jnp.float32) / half)
    ang = pos.astype(jnp.float32)[..., None] * inv
    cos, sin = jnp.cos(ang), jnp.sin(ang)
    x1, x2 = x[..., :half].astype(jnp.float32), x[..., half:].astype(jnp.float32)
    return jnp.concatenate([x1 * cos - x2 * sin, x1 * sin + x2 * cos], axis=-1).astype(x.dtype)


def gla_mixer(q, k, v, gate_lr, out_gate, gate_up, gate_bias, norm_w):
    B, S, _ = q.shape
    H, C = GLA_HEADS, GLA_CHUNK
    nc = S // C
    f32 = jnp.float32

    def heads(t, d):
        return t.reshape(B, nc, C, H, d).transpose(0, 3, 1, 2, 4).astype(f32)

    q = heads(q, GLA_DK) * (GLA_DK ** -0.5)
    k = heads(k, GLA_DK)
    v = heads(v, GLA_DV)
    z = (gate_lr @ gate_up + gate_bias).astype(f32)
    log_a = heads(jax.nn.log_sigmoid(z) / GLA_GATE_NORM, GLA_DK)
    b = jnp.cumsum(log_a, axis=3)
    b_last = b[:, :, :, -1:, :]
    q_e = q * jnp.exp(b)
    k_e = k * jnp.exp(-b)
    causal = jnp.tril(jnp.ones((C, C), dtype=bool))
    att = jnp.where(causal, jnp.einsum('bhnid,bhnjd->bhnij', q_e, k_e), 0.0)
    o_intra = jnp.einsum('bhnij,bhnjv->bhniv', att, v)
    upd = jnp.einsum('bhnjd,bhnjv->bhndv', k * jnp.exp(b_last - b), v)
    decay = jnp.exp(b_last[:, :, :, 0, :])

    def step(state, xs):
        d, u = xs
        return d[..., None] * state + u, state

    s0 = jnp.zeros((B, H, GLA_DK, GLA_DV), f32)
    _, s_prev = lax.scan(step, s0, (jnp.moveaxis(decay, 2, 0), jnp.moveaxis(upd, 2, 0)))
    s_prev = jnp.moveaxis(s_prev, 0, 2)
    o = o_intra + jnp.einsum('bhnid,bhndv->bhniv', q_e, s_prev)
    o = o * lax.rsqrt(jnp.mean(o * o, axis=-1, keepdims=True) + EPS) * norm_w.astype(f32)
    o = o.transpose(0, 2, 3, 1, 4).reshape(B, S, H * GLA_DV)
    return (o * jax.nn.silu(out_gate.astype(f32))).astype(out_gate.dtype)


def mla_mixer(c_q, c_kv, k_rope, positions, q_norm_w, w_uq, kv_norm_w, w_ukv):
    B, S, _ = c_q.shape
    H = MLA_HEADS
    q = (rmsnorm(c_q, q_norm_w) @ w_uq).reshape(B, S, H, MLA_NOPE + MLA_ROPE)
    q_nope = q[..., :MLA_NOPE]
    q_rope = rope(q[..., MLA_NOPE:], positions[:, :, None])
    kv = (rmsnorm(c_kv, kv_norm_w) @ w_ukv).reshape(B, S, H, MLA_NOPE + MLA_DV)
    k_nope, v = kv[..., :MLA_NOPE], kv[..., MLA_NOPE:]
    k_r = rope(k_rope, positions)
    scale = (MLA_NOPE + MLA_ROPE) ** -0.5
    nb = S // Q_BLOCK
    key_pos = jnp.arange(S)

    def block(args):
        qn, qr, i = args
        s = jnp.einsum('bqhd,bkhd->bhqk', qn, k_nope) + jnp.einsum('bqhr,bkr->bhqk', qr, k_r)
        q_pos = i * Q_BLOCK + jnp.arange(Q_BLOCK)
        s = jnp.where(key_pos[None, :] <= q_pos[:, None], s.astype(jnp.float32) * scale, -jnp.inf)
        p = jax.nn.softmax(s, axis=-1).astype(v.dtype)
        return jnp.einsum('bhqk,bkhd->bqhd', p, v)

    qn_b = q_nope.reshape(B, nb, Q_BLOCK, H, MLA_NOPE).transpose(1, 0, 2, 3, 4)
    qr_b = q_rope.reshape(B, nb, Q_BLOCK, H, MLA_ROPE).transpose(1, 0, 2, 3, 4)
    out = lax.map(block, (qn_b, qr_b, jnp.arange(nb)))
    return out.transpose(1, 0, 2, 3, 4).reshape(B, S, H * MLA_DV)


def hier_moe(x, wg, bg, we, be, w_gate, w_up, w_down):
    B, S, D = x.shape
    N = B * S
    xf = x.reshape(N, D)
    f32 = jnp.float32
    g_logits = (xf @ wg).astype(f32) + bg.astype(f32)
    g_prob = jax.nn.softmax(g_logits, axis=-1)
    g_sel = jnp.argmax(g_logits, axis=-1).astype(jnp.int32)
    p_g = jnp.take_along_axis(g_prob, g_sel[:, None], axis=1)
    e_logits = ((xf @ we).astype(f32) + be.astype(f32)).reshape(N, N_GROUPS, EXPERTS_PER_GROUP)
    e_logits = jnp.take_along_axis(e_logits, g_sel[:, None, None], axis=1)[:, 0]
    e_prob = jax.nn.softmax(e_logits, axis=-1)
    top_p, top_i = lax.top_k(e_prob, TOP_K)
    gate = p_g * top_p / jnp.sum(top_p, axis=-1, keepdims=True)
    expert_id = (g_sel[:, None] * EXPERTS_PER_GROUP + top_i.astype(jnp.int32)).reshape(-1)
    NK = N * TOP_K
    tok = jnp.repeat(jnp.arange(N, dtype=jnp.int32), TOP_K)
    order = jnp.argsort(expert_id)
    se, stok, sgate = expert_id[order], tok[order], gate.reshape(-1)[order]
    counts = jnp.bincount(expert_id, length=N_EXPERTS).astype(jnp.int32)
    starts = jnp.cumsum(counts) - counts
    padded = (counts + EXPERT_BLOCK - 1) // EXPERT_BLOCK * EXPERT_BLOCK
    pend = jnp.cumsum(padded)
    pstarts = pend - padded
    dest = pstarts[se] + (jnp.arange(NK, dtype=jnp.int32) - starts[se])
    nblk = -(-NK // EXPERT_BLOCK) + N_EXPERTS
    x_buf = jnp.zeros((nblk * EXPERT_BLOCK, D), xf.dtype).at[dest].set(xf[stok])
    blk_start = jnp.arange(nblk, dtype=jnp.int32) * EXPERT_BLOCK
    blk_expert = jnp.clip(jnp.searchsorted(pend, blk_start, side='right'), 0, N_EXPERTS - 1)

    def expert_block(args):
        xb, e = args
        hdn = jax.nn.silu(xb @ w_gate[e]) * (xb @ w_up[e])
        return hdn @ w_down[e]

    y_buf = lax.map(expert_block, (x_buf.reshape(nblk, EXPERT_BLOCK, D), blk_expert))
    y = y_buf.reshape(nblk * EXPERT_BLOCK, D)[dest] * sgate[:, None].astype(xf.dtype)
    out = jax.ops.segment_sum(y, stok, num_segments=N)
    return out.reshape(B, S, D)


def setup_inputs(seed: int = 0) -> dict:
    key = jax.random.key(seed)
    ks = jax.random.split(key, 24)
    f32 = jnp.float32
    L, D = DEPTH, D_MODEL

    def nrm(k, shape, scale):
        return jax.random.normal(k, shape, f32) * scale

    def gain(k, shape):
        return 1.0 + 0.02 * jax.random.normal(k, shape, f32)

    x = jax.random.normal(ks[0], (BATCH, SEQ, D), f32)
    offsets = jax.random.randint(ks[1], (BATCH, 1), 0, 2048, dtype=jnp.int32)
    positions = (offsets + jnp.arange(SEQ, dtype=jnp.int32)[None, :]).astype(jnp.int32)
    return {
        'x': x,
        'positions': positions,
        'attn_norm_w': gain(ks[2], (L, D)),
        'w_in': nrm(ks[3], (L, D, D_IN), D ** -0.5),
        'gla_gate_up': nrm(ks[4], (L, GLA_GATE_RANK, GLA_HEADS * GLA_DK), GLA_GATE_RANK ** -0.5),
        'gla_gate_bias': nrm(ks[5], (L, GLA_HEADS * GLA_DK), 0.02),
        'gla_norm_w': gain(ks[6], (L, GLA_DV)),
        'mla_q_norm_w': gain(ks[7], (L, MLA_Q_RANK)),
        'mla_w_uq': nrm(ks[8], (L, MLA_Q_RANK, MLA_HEADS * (MLA_NOPE + MLA_ROPE)), MLA_Q_RANK ** -0.5),
        'mla_kv_norm_w': gain(ks[9], (L, MLA_KV_RANK)),
        'mla_w_ukv': nrm(ks[10], (L, MLA_KV_RANK, MLA_HEADS * (MLA_NOPE + MLA_DV)), MLA_KV_RANK ** -0.5),
        'w_out': nrm(ks[11], (L, D_MIX, D), D_MIX ** -0.5),
        'ffn_norm_w': gain(ks[12], (L, D)),
        'router_group_w': nrm(ks[13], (L, D, N_GROUPS), D ** -0.5),
        'router_group_b': nrm(ks[14], (L, N_GROUPS), 0.01),
        'router_expert_w': nrm(ks[15], (L, D, N_EXPERTS), D ** -0.5),
        'router_expert_b': nrm(ks[16], (L, N_EXPERTS), 0.01),
        'expert_w_gate': nrm(ks[17], (L, N_EXPERTS, D, D_EXPERT), D ** -0.5),
        'expert_w_up': nrm(ks[18], (L, N_EXPERTS, D, D_EXPERT), D ** -0.5),
        'expert_w_down': nrm(ks[19], (L, N_EXPERTS, D_EXPERT, D), D_EXPERT ** -0.5),
        'final_norm_w': gain(ks[20], (D,)),
    }


def reference(x, positions, attn_norm_w, w_in, gla_gate_up, gla_gate_bias, gla_norm_w,
              mla_q_norm_w, mla_w_uq, mla_kv_norm_w, mla_w_ukv, w_out, ffn_norm_w,
              router_group_w, router_group_b, router_expert_w, router_expert_b,
              expert_w_gate, expert_w_up, expert_w_down, final_norm_w):
    h = x
    for l in range(DEPTH):
        xn = rmsnorm(h, attn_norm_w[l])
        g_q, g_k, g_v, g_lr, g_og, m_cq, m_ckv, m_kr = jnp.split(xn @ w_in[l], _split_points(), axis=-1)
        y_gla = gla_mixer(g_q, g_k, g_v, g_lr, g_og, gla_gate_up[l], gla_gate_bias[l], gla_norm_w[l])
        y_mla = mla_mixer(m_cq, m_ckv, m_kr, positions, mla_q_norm_w[l], mla_w_uq[l],
                          mla_kv_norm_w[l], mla_w_ukv[l])
        h = h + jnp.concatenate([y_gla, y_mla], axis=-1) @ w_out[l]
        h = h + hier_moe(rmsnorm(h, ffn_norm_w[l]), router_group_w[l], router_group_b[l],
                         router_expert_w[l], router_expert_b[l], expert_w_gate[l],
                         expert_w_up[l], expert_w_down[l])
    return rmsnorm(h, final_norm_w)
```

```python
import numpy as np
import ml_dtypes
import concourse.bass as bass
import concourse.mybir as mybir
from concourse.bass_utils import run_bass_kernel_spmd

F32 = mybir.dt.float32
BF16 = mybir.dt.bfloat16
I32 = mybir.dt.int32
AF = mybir.ActivationFunctionType
ALU = mybir.AluOpType
AX = mybir.AxisListType

S = 4096
NT = 32
D = 1024
KC = 8
EPS = 1e-6
NE = 32
CAP = 384
NSLOT = NE * CAP
TWO_PI = 2.0 * np.pi


class Buf:
    def __init__(self, name):
        self.name = name
        self.lw = None
        self.rd = []


ENGS = ["pe", "act", "dve", "pool", "sp"]
INORDER = {"sp", "pool"}
CRIT = 0
CP_PRIO = True
PA_CFG = {'a1': [0], 'a2': [1], 'a3': [1], 'a4': [0], 'b': [2], 'c1': [3], 'c2': [3], 'c3': [2]}
PE_SLOW = 1.05
SLAT = 0.2
XLAT = 0.4


def _free(ap):
    n = 1
    for s_ in list(ap.shape)[1:]:
        n *= int(s_)
    return n


def _est(eng, name, kw):
    try:
        if name in ("dma_start", "indirect_dma_start"):
            o = kw["in_"] if kw.get("out_offset", None) is not None else kw["out"]
            nbytes = int(o.shape[0]) * _free(o) * (2 if o.dtype == BF16 else 4)
            occ = 0.15 if eng in ("sp", "act") else 1.2
            return occ, 2.5 + nbytes / 150e3
        if name == "matmul":
            n = _free(kw["rhs"])
            f = 4 if kw["rhs"].dtype == F32 else 1
            d = max(64, n) * f / 2400.0 * PE_SLOW
            return d, d + 0.1
        if name == "transpose":
            f = 2 if kw["in_"].dtype == F32 else 1
            d = 128 * f / 2400.0 * PE_SLOW
            return d, d + 0.1
        if name == "activation":
            d = (_free(kw["in_"]) + 230) / 1400.0 + (0.1 if "accum_out" in kw else 0.0)
            return d, d
        n = _free(kw.get("out", kw.get("ap")))
        if name == "reciprocal":
            n *= 7
        if eng == "pool":
            d = 0.9 + n * 1.2 / 960.0
        else:
            d = 0.14 + n / 960.0
        return d, d
    except Exception:
        return 0.3, 0.3


class Sched:
    def __init__(self, nc, eng_sems, dma_sems):
        self.nc = nc
        self.eng_sems = eng_sems
        self.dma_pool = [(h, 0) for h in dma_sems]
        self.dma_used = []
        self.dma_map = {}
        self.dma_q = {}
        self.cnt = {e: 0 for e in ENGS}
        self.dma_cnt = {}
        self.dma_last = {}
        self.waited = {e: {} for e in ENGS}
        self.nodes = []
        self.base = 0
        self.tok = {}
        self.reorder = True

    def sem(self, key):
        if key[0] == "E":
            return self.eng_sems[key[1]]
        return self._sem_of[key[1]]

    def _node(self, eng, name, kw, reads, writes, dma_key=None):
        nid = self.base + len(self.nodes)
        deps = set()

        def add(d):
            if d is None or d < self.base:
                return
            deps.add(d)
            dk = self.nodes[d - self.base]["dma_key"]
            if dk is not None:
                deps.add(self.dma_last[dk])

        for b_ in reads:
            add(b_.lw)
        for b_ in writes:
            add(b_.lw)
            for r_ in b_.rd:
                add(r_)
        deps = {d for d in deps if d >= self.base}
        occ, lat = _est(eng, name, kw)
        self.nodes.append({"id": nid, "eng": eng, "name": name, "kw": kw, "deps": deps, "occ": occ, "lat": lat,
                           "dma_key": dma_key})
        for b_ in writes:
            b_.lw = nid
            b_.rd = []
        for b_ in reads:
            if b_.lw != nid:
                b_.rd.append(nid)
        return nid

    def op(self, eng, name, reads=(), writes=(), **kw):
        self._node(eng, name, kw, reads, writes)

    def dma(self, eng, sb, reads=(), writes=(), name="dma_start", **kw):
        bn = sb.name
        if bn not in self.dma_map:
            if eng == "sp" and self.dma_used:
                h, c0 = self.dma_used.pop()
            else:
                h, c0 = self.dma_pool.pop()
            self.dma_map[bn] = h
            self.dma_cnt[("D", bn)] = c0
            self.dma_q[bn] = eng
        assert self.dma_q[bn] == eng, "all DMAs touching one SBUF buffer must use one queue: " + bn
        k = ("D", bn)
        nid = self._node(eng, name, kw, reads, writes, dma_key=k)
        self.dma_last[k] = nid

    def _schedule(self):
        import heapq
        nodes = self.nodes
        n = len(nodes)
        succ = [[] for _ in range(n)]
        indeg = [0] * n
        for nd in nodes:
            i = nd["id"] - self.base
            for d in nd["deps"]:
                succ[d - self.base].append(i)
                indeg[i] += 1
        prio = list(range(n))
        if CP_PRIO:
            cp = [0.0] * n
            for i in range(n - 1, -1, -1):
                m = 0.0
                for s_ in succ[i]:
                    if cp[s_] > m:
                        m = cp[s_]
                cp[i] = m + nodes[i]["lat"]
            order_idx = sorted(range(n), key=lambda j: (-cp[j], j))
            for r_, j in enumerate(order_idx):
                prio[j] = r_
        fin = [0.0] * n
        why = [None] * n
        rdep = [None] * n
        last_on = {e: None for e in ENGS}
        ready_at = [0.0] * n
        free_at = {e: 0.0 for e in ENGS}
        waiting = {e: [] for e in ENGS}
        avail = {e: [] for e in ENGS}
        inorder_q = {e: [i for i in range(n) if nodes[i]["eng"] == e] for e in INORDER}
        inorder_pos = {e: 0 for e in INORDER}
        order = {e: [] for e in ENGS}
        for i in range(n):
            if indeg[i] == 0 and nodes[i]["eng"] not in INORDER:
                heapq.heappush(waiting[nodes[i]["eng"]], (0.0, i))
        done = 0
        while done < n:
            best = None
            for e in ENGS:
                if e in INORDER:
                    p = inorder_pos[e]
                    if p >= len(inorder_q[e]):
                        continue
                    i = inorder_q[e][p]
                    if indeg[i] > 0:
                        continue
                    stt = max(free_at[e], ready_at[i])
                    cand = (stt, i, e)
                else:
                    w, av = waiting[e], avail[e]
                    while w and w[0][0] <= free_at[e]:
                        j_ = heapq.heappop(w)[1]
                        heapq.heappush(av, (prio[j_], j_))
                    if av:
                        cand = (free_at[e], av[0][1], e)
                    elif w:
                        cand = (w[0][0], w[0][1], e)
                    else:
                        continue
                if best is None or cand < best:
                    best = cand
            assert best is not None, "scheduler deadlock"
            stt, i, e = best
            if e in INORDER:
                inorder_pos[e] += 1
            else:
                if avail[e] and avail[e][0][1] == i:
                    heapq.heappop(avail[e])
                else:
                    heapq.heappop(waiting[e])
            nd = nodes[i]
            why[i] = ("dep", rdep[i]) if (rdep[i] is not None and ready_at[i] >= free_at[e] - 1e-9) else ("eng", last_on[e])
            last_on[e] = i
            free_at[e] = stt + nd["occ"]
            fin[i] = stt + nd["lat"]
            order[e].append(i)
            done += 1
            for s_ in succ[i]:
                indeg[s_] -= 1
                lat = XLAT if nodes[s_]["eng"] != e else (0.0 if e == "pe" else SLAT)
                if fin[i] + lat > ready_at[s_]:
                    ready_at[s_] = fin[i] + lat
                    rdep[s_] = i
                if indeg[s_] == 0 and nodes[s_]["eng"] not in INORDER:
                    heapq.heappush(waiting[nodes[s_]["eng"]], (ready_at[s_], s_))
        self.est_us = max(fin) if n else 0.0
        if CRIT and n:
            i = max(range(n), key=lambda j: fin[j])
            path = []
            while i is not None and len(path) < 100000:
                path.append(i)
                i = why[i][1] if why[i] else None
            agg = {}
            for i in path:
                nd = nodes[i]
                o = nd["kw"].get("out", nd["kw"].get("ap"))
                key = (nd["eng"], nd["name"], str(getattr(getattr(o, "tensor", None), "name", "?")), why[i][0] if why[i] else "-")
                a_ = agg.setdefault(key, [0, 0.0])
                a_[0] += 1
                a_[1] += nd["lat"]
            print("  critical path (%d nodes):" % len(path))
            for k_, v_ in sorted(agg.items(), key=lambda kv: -kv[1][1])[:CRIT]:
                print("    %-60s n=%5d  t=%7.1f" % (k_, v_[0], v_[1]))
        self.est_busy = {e: sum(nodes[i]["occ"] for i in order[e]) for e in ENGS}
        return order

    def run_block(self, name=None):
        nodes = self.nodes
        n = len(nodes)
        if self.reorder:
            order = self._schedule()
        else:
            order = {e: [i for i in range(n) if nodes[i]["eng"] == e] for e in ENGS}
        tok = {}
        for e in ENGS:
            for i in order[e]:
                nd = nodes[i]
                if nd["dma_key"] is not None:
                    k = nd["dma_key"]
                    self.dma_cnt[k] = self.dma_cnt.get(k, 0) + 16
                    tok[i] = (k, self.dma_cnt[k], 16)
                else:
                    self.cnt[e] += 1
                    tok[i] = (("E", e), self.cnt[e], 1)
        prog = {e: [] for e in ENGS}
        for e in ENGS:
            wd = self.waited[e]
            for i in order[e]:
                nd = nodes[i]
                need = {}
                for d in nd["deps"]:
                    k, v, _ = tok[d - self.base]
                    if k == ("E", "pe") and e == "pe":
                        continue
                    if need.get(k, 0) < v:
                        need[k] = v
                for k, v in need.items():
                    if wd.get(k, 0) < v:
                        wd[k] = v
                        prog[e].append(("wait", k, v))
                prog[e].append(("inst", (nd["name"], nd["kw"]), tok[i][0], tok[i][2]))
        wd = self.waited["sp"]
        for k, v in self.dma_cnt.items():
            if wd.get(k, 0) < v:
                wd[k] = v
                prog["sp"].append(("wait", k, v))
        for e in ENGS:
            k, v = ("E", e), self.cnt[e]
            if e != "sp" and v > 0 and wd.get(k, 0) < v:
                wd[k] = v
                prog["sp"].append(("wait", k, v))
        self.base += n
        self.nodes = []
        nc = self.nc
        self._sem_of = dict(self.dma_map)
        clear_list = []
        for bn, h in self.dma_map.items():
            c_ = self.dma_cnt.pop(("D", bn))
            if self.dma_q[bn] == "sp":
                self.dma_used.append((h, c_))
            for e in ENGS:
                self.waited[e].pop(("D", bn), None)
        self.dma_map = {}
        self.dma_q = {}
        self.dma_last = {}

        def replay(lst, clear=()):
            def body(engine):
                for o in lst:
                    if o[0] == "wait":
                        engine.wait_ge(self.sem(o[1]), o[2])
                    else:
                        getattr(engine, o[1][0])(**o[1][1]).then_inc(self.sem(o[2]), o[3])
                for h in clear:
                    engine.sem_clear(h)
            return body

        with nc.Block() as block:
            block.tensor(replay(prog["pe"]))
            block.scalar(replay(prog["act"]))
            block.vector(replay(prog["dve"]))
            block.gpsimd(replay(prog["pool"]))
            block.sync(replay(prog["sp"], clear_list))
        if name:
            print("[sched] block %s: %d ops, est %.0f us, busy %s" % (name, n, getattr(self, "est_us", 0.0),
                  {e: int(v) for e, v in getattr(self, "est_busy", {}).items()}))


def build_program(debug=None):
    nc = bass.Bass("TRN2", target_bir_lowering=False)

    def din(name, shape, dt=F32):
        return nc.dram_tensor(name, list(shape), dt, kind="ExternalInput").ap()

    x = din("x", [S, D])
    pos = din("pos", [1, S], I32)
    g_attn = din("g_attn", [1, D])
    g_ffn = din("g_ffn", [1, D])
    g_fin = din("g_fin", [1, D])
    g_q = din("g_q", [1, 384])
    g_kv = din("g_kv", [1, 256])
    g_gla = din("g_gla", [1, 128])
    w_mla = din("w_mla", [D, 640])
    w_kr = din("w_kr", [D, 64])
    w_gf = din("w_gf", [D, 528])
    w_gt = din("w_gt", [D, 1280])
    w_uq = din("w_uq", [384, 8 * 128])
    w_ukn = din("w_ukn", [256, 512])
    w_uv = din("w_uv", [256, 512])
    gate_up = din("gate_up", [17, 256])
    w_out = din("w_out", [D, D])
    w_rt = din("w_rt", [D, 36])
    b_rt = din("b_rt", [1, 36])
    w_eg = din("w_eg", [NE, D, 256])
    w_eu = din("w_eu", [NE, D, 256])
    w_ed = din("w_ed", [NE, 256, D])
    c_ident = din("c_ident", [128, 128])
    c_rope = din("c_rope", [128, 2])
    c_tri = din("c_tri", [128, 4 * 128])
    c_gla = din("c_gla", [128, 256])
    c_ecap = din("c_ecap", [128, 32])
    out = nc.dram_tensor("out", [S, D], F32, kind="ExternalOutput").ap()
    dbg = None
    if debug:
        dbg = nc.dram_tensor("dbg", list(debug[:2]), F32, kind="ExternalOutput").ap()

    h1_d = nc.dram_tensor("h1_d", [S, D], F32).ap()
    xbuf_d = nc.dram_tensor("xbuf_d", [NSLOT, D], BF16).ap()
    ybuf_d = nc.dram_tensor("ybuf_d", [NSLOT, D], BF16).ap()
    wbf_d = nc.dram_tensor("wbf_d", [NE, 128, 6144], BF16).ap()

    from contextlib import ExitStack
    es = ExitStack()
    with es:
        eng_sems = {e: es.enter_context(nc.semaphore("sem_" + e)) for e in ENGS}
        dma_sems = [es.enter_context(nc.semaphore("dsem%d" % i)) for i in range(56)]
        sc = Sched(nc, eng_sems, dma_sems)

        def sb(stack, name, shape, dt):
            return stack.enter_context(nc.sbuf_tensor(name, list(shape), dt))

        def ps(stack, name, shape, dt):
            return stack.enter_context(nc.psum_tensor(name, list(shape), dt))

        ident_f = sb(es, "ident_f", [128, 128], F32)
        ident_b = sb(es, "ident_b", [128, 128], BF16)
        ymT = sb(es, "ymT", [128, NT, 512], BF16)
        B_ident = Buf("ident")
        B_ymT = Buf("ymT")
        sc.dma("sp", B_ident, writes=[B_ident], out=ident_f[:], in_=c_ident)
        sc.op("dve", "tensor_copy", out=ident_b[:], in_=ident_f[:], reads=[B_ident], writes=[B_ident])

        skip_front = bool(debug and len(debug) > 5 and debug[5])
        if skip_front:
            sc.op("dve", "memset", ap=ymT[:], constant=0.0, writes=[B_ymT])
        class TB:
            def __init__(self, t, name):
                self.t = t
                self.b = Buf(name)

        def mk(stack, name, shape, dt, n=None, psum=False):
            f = ps if psum else sb
            name = "m_" + name
            if n is None:
                return TB(f(stack, name, shape, dt), name)
            return [TB(f(stack, "%s%d" % (name, i), shape, dt), "%s%d" % (name, i)) for i in range(n)]

        def bl(lst):
            return [getattr(o, "b", o) for o in lst]

        def OP(eng, name, reads, writes, **kw):
            sc.op(eng, name, reads=bl(reads), writes=bl(writes), **kw)

        def DMA(eng, sbuf, reads, writes, name="dma_start", **kw):
            sc.dma(eng, getattr(sbuf, "b", sbuf), reads=bl(reads), writes=bl(writes), name=name, **kw)

        zero_t = sb(es, "zero_t", [128, D], BF16)
        B_zero = Buf("zero_t")
        es12 = ExitStack()
        with es12:
          if not skip_front:
              ctab = sb(es12, "ctab", [128, S], BF16)
              stab = sb(es12, "stab", [128, S], BF16)
              cnT = sb(es12, "cnT", [128, 5, S], BF16)
              krT = sb(es12, "krT", [128, S], BF16)
              B_tab = Buf("tab")
              wuq_sb = sb(es12, "wuq_sb", [128, 3, 1024], BF16)
              wukn_sb = sb(es12, "wukn_sb", [128, 2, 512], BF16)
              wuv_sb = sb(es12, "wuv_sb", [128, 2, 512], BF16)
              B_w2 = Buf("w2")
              B_cnT = Buf("cnT")
              B_krT = Buf("krT")

              e0 = ExitStack()
              if True:
                  ropec = sb(e0, "ropec", [128, 2], F32)
                  posi = sb(e0, "posi", [128, 1024], I32)
                  ang = sb(e0, "ang", [128, 1024], F32)
                  kf = sb(e0, "kf", [128, 1024], F32)
                  ki = sb(e0, "ki", [128, 1024], I32)
                  r1 = sb(e0, "r1", [128, 1024], F32)
                  tabt = [sb(e0, "tabt%d" % i, [128, 1024], BF16) for i in range(2)]
                  B_ropec, B_posi, B_ang, B_kf, B_ki, B_r1 = (Buf(n) for n in ["ropec", "posi", "ang", "kf", "ki", "r1"])
                  B_tabt = [Buf("tabt0"), Buf("tabt1")]
                  sc.dma("sp", B_ropec, writes=[B_ropec], out=ropec[:], in_=c_rope)
                  for g_ in range(4):
                      sc.dma("sp", B_posi, writes=[B_posi], out=posi[g_ * 32:(g_ + 1) * 32, :],
                             in_=pos[:, g_ * 1024:(g_ + 1) * 1024].partition_broadcast(32))
                  sc.op("dve", "tensor_copy", out=ang[:], in_=posi[:], reads=[B_posi], writes=[B_ang])
                  sc.op("dve", "tensor_scalar", out=ang[:], in0=ang[:], scalar1=ropec[:, 0:1], scalar2=None,
                        op0=ALU.mult, reads=[B_ang, B_ropec], writes=[B_ang])
                  for which in range(2):
                      shift = 0.0 if which == 0 else np.pi / 2.0
                      sc.op("dve", "tensor_scalar", out=kf[:], in0=ang[:], scalar1=shift, scalar2=1.0 / TWO_PI, op0=ALU.add, op1=ALU.mult,
                            reads=[B_ang], writes=[B_kf])
                      sc.op("dve", "tensor_copy", out=ki[:], in_=kf[:], reads=[B_kf], writes=[B_ki])
                      sc.op("dve", "tensor_copy", out=kf[:], in_=ki[:], reads=[B_ki], writes=[B_kf])
                      sc.op("dve", "scalar_tensor_tensor", out=r1[:], in0=kf[:], scalar=-TWO_PI, in1=ang[:],
                            op0=ALU.mult, op1=ALU.add, reads=[B_kf, B_ang], writes=[B_r1])
                      if which == 1:
                          sc.op("dve", "tensor_scalar", out=r1[:], in0=r1[:], scalar1=np.pi / 2.0, scalar2=None,
                                op0=ALU.add, reads=[B_r1], writes=[B_r1])
                      sc.op("dve", "tensor_scalar", out=kf[:], in0=r1[:], scalar1=np.pi, scalar2=-TWO_PI,
                            op0=ALU.is_gt, op1=ALU.mult, reads=[B_r1], writes=[B_kf])
                      sc.op("dve", "tensor_tensor", out=r1[:], in0=r1[:], in1=kf[:], op=ALU.add, reads=[B_r1, B_kf], writes=[B_r1])
                      sc.op("dve", "tensor_scalar", out=kf[:], in0=r1[:], scalar1=-np.pi, scalar2=TWO_PI,
                            op0=ALU.is_lt, op1=ALU.mult, reads=[B_r1], writes=[B_kf])
                      sc.op("dve", "tensor_tensor", out=r1[:], in0=r1[:], in1=kf[:], op=ALU.add, reads=[B_r1, B_kf], writes=[B_r1])
                      sc.op("dve", "tensor_scalar", out=r1[:], in0=r1[:], scalar1=3.1415925, scalar2=-3.1415925,
                            op0=ALU.min, op1=ALU.max, reads=[B_r1], writes=[B_r1])
                      if which == 0:
                          sc.op("act", "activation", out=tabt[0][:], in_=r1[:], func=AF.Sin, scale=ropec[:, 1:2],
                                reads=[B_r1, B_ropec], writes=[B_tabt[0]])
                      else:
                          sc.op("act", "activation", out=tabt[1][:], in_=r1[:], func=AF.Sin, reads=[B_r1], writes=[B_tabt[1]])
                      dst = stab if which == 0 else ctab
                      for g_ in range(4):
                          sc.dma("act", B_tabt[which], reads=[B_tabt[which]], writes=[B_tab], out=dst[64:96, g_ * 1024:(g_ + 1) * 1024],
                                 in_=tabt[which][g_ * 32:(g_ + 1) * 32, :])

              with ExitStack() as e1:
                  NXT = 4
                  xt = [sb(e1, "xt%d" % i, [128, D], F32) for i in range(NXT)]
                  B_xt = [Buf("xt%d" % i) for i in range(NXT)]
                  g1 = sb(e1, "g1", [128, D], F32)
                  gqk = sb(e1, "gqk", [128, 640], F32)
                  B_g = Buf("g1")
                  wm_sb = sb(e1, "wm_sb", [128, KC, 640], BF16)
                  wk_sb = sb(e1, "wk_sb", [128, KC, 64], BF16)
                  B_wm = Buf("wm_sb")
                  B_wk = Buf("wk_sb")
                  RS1 = 3
                  st = [sb(e1, "st%d" % i, [128, 8], F32) for i in range(RS1)]
                  B_st = [Buf("st%d" % i) for i in range(RS1)]
                  xs = [sb(e1, "xs%d" % i, [128, D], BF16) for i in range(RS1)]
                  B_xs = [Buf("xs%d" % i) for i in range(RS1)]
                  xnT = [sb(e1, "xnT%d" % i, [128, KC, 512], BF16) for i in range(2)]
                  B_xnT = [Buf("xnT%d" % i) for i in range(2)]
                  cn = [sb(e1, "cn%d" % i, [128, 640], BF16) for i in range(RS1)]
                  B_cn = [Buf("cn%d" % i) for i in range(RS1)]
                  kt1 = sb(e1, "kt1", [128, 512], F32)
                  kt2 = sb(e1, "kt2", [128, 512], F32)
                  B_kt = Buf("kt")
                  p_tp = ps(e1, "p_tp", [128, KC, 128], BF16)
                  p_mm = [ps(e1, "p_mm%d" % i, [128, 1024], F32) for i in range(2)]
                  p_tp2 = ps(e1, "p_tp2", [128, 5, 128], BF16)
                  p_kr = ps(e1, "p_kr", [128, 2, 512], F32)
                  B_ptp, B_ptp2, B_pkr = Buf("p_tp"), Buf("p_tp2"), Buf("p_kr")
                  B_pmm = [Buf("p_mm0"), Buf("p_mm1")]

                  sc.dma("sp", B_g, writes=[B_g], out=g1[:], in_=g_attn.partition_broadcast(128))
                  sc.dma("sp", B_g, writes=[B_g], out=gqk[:, 0:384], in_=g_q.partition_broadcast(128))
                  sc.dma("sp", B_g, writes=[B_g], out=gqk[:, 384:640], in_=g_kv.partition_broadcast(128))
                  for c in range(KC):
                      sc.dma("pool", B_wm, writes=[B_wm], out=wm_sb[:, c, :], in_=w_mla[c * 128:(c + 1) * 128, :])
                      sc.dma("pool", B_wk, writes=[B_wk], out=wk_sb[:, c, :], in_=w_kr[c * 128:(c + 1) * 128, :])

                  def load_w2():
                      for c in range(3):
                          sc.dma("pool", B_w2, reads=[B_cnT], writes=[B_w2], out=wuq_sb[:, c, :], in_=w_uq[c * 128:(c + 1) * 128, :])
                      for c in range(2):
                          sc.dma("pool", B_w2, reads=[B_cnT], writes=[B_w2], out=wukn_sb[:, c, :], in_=w_ukn[c * 128:(c + 1) * 128, :])
                          sc.dma("pool", B_w2, reads=[B_cnT], writes=[B_w2], out=wuv_sb[:, c, :], in_=w_uv[c * 128:(c + 1) * 128, :])

                  def load_x(i):
                      s_ = i % NXT
                      sc.dma("sp", B_xt[s_], writes=[B_xt[s_]], out=xt[s_][:], in_=x[i * 128:(i + 1) * 128, :])

                  load_x(0)
                  load_x(1)
                  load_x(2)
                  for i in range(NT):
                      if i + 3 < NT:
                          load_x(i + 3)
                      if i == 8:
                          load_w2()
                      s3, s2, sp_ = i % NXT, i % RS1, i % 2
                      blk, tb = i // 4, i % 4
                      xb = blk % 2
                      tcols = slice(tb * 128, (tb + 1) * 128)
                      gcols = slice(i * 128, (i + 1) * 128)
                      sc.op("act", "activation", out=xs[s2][:], in_=xt[s3][:], func=AF.Square, accum_out=st[s2][:, 0:1],
                            reads=[B_xt[s3]], writes=[B_st[s2], B_xs[s2]])
                      sc.op("act", "activation", out=st[s2][:, 1:2], in_=st[s2][:, 0:1], func=AF.Ln, scale=1.0 / D, bias=EPS,
                            reads=[B_st[s2]], writes=[B_st[s2]])
                      sc.op("act", "activation", out=st[s2][:, 2:3], in_=st[s2][:, 1:2], func=AF.Exp, scale=-0.5,
                            reads=[B_st[s2]], writes=[B_st[s2]])
                      sc.op("dve", "scalar_tensor_tensor", out=xs[s2][:], in0=xt[s3][:], scalar=st[s2][:, 2:3], in1=g1[:],
                                                                    op0=ALU.mult, op1=ALU.mult,
                            reads=[B_xt[s3], B_st[s2], B_g], writes=[B_xs[s2]])
                      for c in range(KC):
                          sc.op("pe", "transpose", out=p_tp[:, c, :], in_=xs[s2][:, c * 128:(c + 1) * 128], identity=ident_b[:],
                                reads=[B_xs[s2], B_ident], writes=[B_ptp])
                      sc.op("act", "activation", out=xnT[xb][:, :, tcols], in_=p_tp[:], func=AF.Copy,
                            reads=[B_ptp], writes=[B_xnT[xb]])
                      for c in range(KC):
                          sc.op("pe", "matmul", out=p_mm[sp_][:, 0:384], lhsT=xnT[xb][:, c, tcols], rhs=wm_sb[:, c, 0:384],
                                                              start=(c == 0), stop=(c == KC - 1),
                                reads=[B_xnT[xb], B_wm], writes=[B_pmm[sp_]])
                      for c in range(KC):
                          sc.op("pe", "matmul", out=p_mm[sp_][:, 512:768], lhsT=xnT[xb][:, c, tcols], rhs=wm_sb[:, c, 384:640],
                                                              start=(c == 0), stop=(c == KC - 1),
                                reads=[B_xnT[xb], B_wm], writes=[B_pmm[sp_]])
                      sc.op("act", "activation", out=cn[s2][:, 0:384], in_=p_mm[sp_][:, 0:384], func=AF.Square, accum_out=st[s2][:, 3:4],
                            reads=[B_pmm[sp_]], writes=[B_st[s2], B_cn[s2]])
                      sc.op("act", "activation", out=cn[s2][:, 384:640], in_=p_mm[sp_][:, 512:768], func=AF.Square, accum_out=st[s2][:, 4:5],
                            reads=[B_pmm[sp_]], writes=[B_st[s2], B_cn[s2]])
                      sc.op("act", "activation", out=st[s2][:, 5:6], in_=st[s2][:, 3:4], func=AF.Ln, scale=1.0 / 384, bias=EPS,
                            reads=[B_st[s2]], writes=[B_st[s2]])
                      sc.op("act", "activation", out=st[s2][:, 6:7], in_=st[s2][:, 4:5], func=AF.Ln, scale=1.0 / 256, bias=EPS,
                            reads=[B_st[s2]], writes=[B_st[s2]])
                      sc.op("act", "activation", out=st[s2][:, 5:7], in_=st[s2][:, 5:7], func=AF.Exp, scale=-0.5,
                            reads=[B_st[s2]], writes=[B_st[s2]])
                      sc.op("dve", "scalar_tensor_tensor", out=cn[s2][:, 0:384], in0=p_mm[sp_][:, 0:384], scalar=st[s2][:, 5:6],
                                                                    in1=gqk[:, 0:384], op0=ALU.mult, op1=ALU.mult,
                            reads=[B_pmm[sp_], B_st[s2], B_g], writes=[B_cn[s2]])
                      sc.op("dve", "scalar_tensor_tensor", out=cn[s2][:, 384:640], in0=p_mm[sp_][:, 512:768], scalar=st[s2][:, 6:7],
                                                                    in1=gqk[:, 384:640], op0=ALU.mult, op1=ALU.mult,
                            reads=[B_pmm[sp_], B_st[s2], B_g], writes=[B_cn[s2]])
                      for c in range(5):
                          sc.op("pe", "transpose", out=p_tp2[:, c, :], in_=cn[s2][:, c * 128:(c + 1) * 128], identity=ident_b[:],
                                reads=[B_cn[s2], B_ident], writes=[B_ptp2])
                      sc.op("act", "activation", out=cnT[:, :, gcols], in_=p_tp2[:], func=AF.Copy,
                            reads=[B_ptp2], writes=[B_cnT])
                      if tb == 3:
                          bcols = slice(blk * 512, (blk + 1) * 512)
                          for j in range(2):
                              for c in range(KC):
                                  sc.op("pe", "matmul", out=p_kr[64:96, j, :], lhsT=wk_sb[:, c, j * 32:(j + 1) * 32],
                                                                           rhs=xnT[xb][:, c, :], start=(c == 0), stop=(c == KC - 1),
                                        reads=[B_xnT[xb], B_wk], writes=[B_pkr])
                          P = slice(64, 96)
                          sc.op("dve", "tensor_tensor", out=kt1[P, :], in0=p_kr[P, 0, :], in1=ctab[P, bcols], op=ALU.mult,
                                reads=[B_pkr, B_tab], writes=[B_kt])
                          sc.op("dve", "tensor_tensor", out=kt2[P, :], in0=p_kr[P, 1, :], in1=stab[P, bcols], op=ALU.mult,
                                reads=[B_pkr, B_tab, B_kt], writes=[B_kt])
                          sc.op("dve", "tensor_tensor", out=krT[P, bcols], in0=kt1[P, :], in1=kt2[P, :], op=ALU.add,
                                reads=[B_kt], writes=[B_krT])
                  sc.run_block("p1")
              e0.close()

              with ExitStack() as e2:
                  SCALE = float(96 ** -0.5)
                  trif = sb(e2, "trif", [128, 128], F32)
                  trib = sb(e2, "trib", [128, 128], BF16)
                  ones_b = sb(e2, "ones_b", [128, 64], BF16)
                  B_c2 = Buf("c2")
                  QT = [sb(e2, "QT%d" % i, [128, S], BF16) for i in range(2)]
                  KT = [sb(e2, "KT%d" % i, [128, S], BF16) for i in range(2)]
                  VV = [sb(e2, "VV%d" % i, [128, NT, 65], BF16) for i in range(2)]
                  B_QT = [Buf("QT0"), Buf("QT1")]
                  B_KT = [Buf("KT0"), Buf("KT1")]
                  B_VV = [Buf("VV0"), Buf("VV1")]
                  NPT = 24
                  PT = [sb(e2, "PT%d" % i, [128, 512], BF16) for i in range(NPT)]
                  B_PT = [Buf("PT%d" % i) for i in range(NPT)]
                  qt1 = sb(e2, "qt1", [128, 512], F32)
                  qt2 = sb(e2, "qt2", [128, 512], F32)
                  B_qt = Buf("qt")
                  rr = [sb(e2, "rr%d" % i, [128, 8], F32) for i in range(2)]
                  B_rr = [Buf("rr0"), Buf("rr1")]
                  NST = 4
                  psT = [ps(e2, "psT%d" % i, [128, 512], F32) for i in range(NST)]
                  B_psT = [Buf("psT%d" % i) for i in range(NST)]
                  poT = [ps(e2, "poT%d" % i, [128, 512], F32) for i in range(2)]
                  B_poT = [Buf("poT0"), Buf("poT1")]
                  NBB = 2
                  pbb = [ps(e2, "pbb%d" % i, [128, 512], F32) for i in range(NBB)]
                  B_pbb = [Buf("pbb%d" % i) for i in range(NBB)]

                  sc.dma("sp", B_c2, writes=[B_c2], out=trif[:], in_=c_tri[:, 0:128])
                  sc.op("dve", "tensor_copy", out=trib[:], in_=trif[:], reads=[B_c2], writes=[B_c2])
                  sc.op("dve", "memset", ap=ones_b[:], constant=1.0, writes=[B_c2])
                  for i in range(2):
                      sc.op("dve", "memset", ap=VV[i][:, :, 64:65], constant=1.0, writes=[B_VV[i]])

                  sc.op("dve", "memset", ap=zero_t[:], constant=0.0, writes=[B_zero])
                  for r_ in range(NSLOT // 128):
                      sc.dma("sp", B_zero, reads=[B_zero], out=xbuf_d[r_ * 128:(r_ + 1) * 128, :], in_=zero_t[:])
                  st2 = {"bb": 0, "ps": 0, "pt": 0, "nq": 0}
                  P = slice(64, 96)

                  def build_jobs(h):
                      hb = h % 2
                      jobs = []
                      for b in range(8):
                          bc = slice(b * 512, (b + 1) * 512)

                          def jq(b=b, bc=bc):
                              k_ = st2["bb"] % NBB; st2["bb"] += 1
                              for c in range(3):
                                  sc.op("pe", "matmul", out=pbb[k_][0:96, :], lhsT=wuq_sb[:, c, h * 128:h * 128 + 96], rhs=cnT[:, c, bc],
                                        start=(c == 0), stop=(c == 2), reads=[B_w2, B_cnT], writes=[B_pbb[k_]])
                              sc.op("dve", "tensor_copy", out=QT[hb][0:64, bc], in_=pbb[k_][0:64, :],
                                    reads=[B_pbb[k_]], writes=[B_QT[hb]])
                              sc.op("dve", "tensor_tensor", out=qt1[P, :], in0=pbb[k_][P, :], in1=ctab[P, bc], op=ALU.mult,
                                    reads=[B_pbb[k_], B_tab], writes=[B_qt])
                              k2 = st2["bb"] % NBB; st2["bb"] += 1
                              for c in range(3):
                                  sc.op("pe", "matmul", out=pbb[k2][P, :], lhsT=wuq_sb[:, c, h * 128 + 96:h * 128 + 128], rhs=cnT[:, c, bc],
                                        start=(c == 0), stop=(c == 2), reads=[B_w2, B_cnT], writes=[B_pbb[k2]])
                              sc.op("dve", "tensor_tensor", out=qt2[P, :], in0=pbb[k2][P, :], in1=stab[P, bc], op=ALU.mult,
                                    reads=[B_pbb[k2], B_tab, B_qt], writes=[B_qt])
                              sc.op("dve", "tensor_tensor", out=QT[hb][P, bc], in0=qt1[P, :], in1=qt2[P, :], op=ALU.add,
                                    reads=[B_qt], writes=[B_QT[hb]])

                          def jk(b=b, bc=bc):
                              k_ = st2["bb"] % NBB; st2["bb"] += 1
                              for c in range(2):
                                  sc.op("pe", "matmul", out=pbb[k_][0:64, :], lhsT=wukn_sb[:, c, h * 64:(h + 1) * 64], rhs=cnT[:, 3 + c, bc],
                                        start=(c == 0), stop=(c == 1), reads=[B_w2, B_cnT], writes=[B_pbb[k_]])
                              sc.op("dve", "tensor_copy", out=KT[hb][0:64, bc], in_=pbb[k_][0:64, :],
                                    reads=[B_pbb[k_]], writes=[B_KT[hb]])
                              sc.op("pool", "tensor_copy", out=KT[hb][P, bc], in_=krT[P, bc], reads=[B_krT], writes=[B_KT[hb]])

                          def jv(b=b, bc=bc):
                              k_ = st2["bb"] % NBB; st2["bb"] += 1
                              for t in range(4):
                                  tcs = slice(b * 512 + t * 128, b * 512 + (t + 1) * 128)
                                  for c in range(2):
                                      sc.op("pe", "matmul", out=pbb[k_][:, t * 64:(t + 1) * 64], lhsT=cnT[:, 3 + c, tcs],
                                            rhs=wuv_sb[:, c, h * 64:(h + 1) * 64], start=(c == 0), stop=(c == 1),
                                            reads=[B_w2, B_cnT], writes=[B_pbb[k_]])
                              sc.op("dve", "tensor_copy", out=VV[hb][:, b * 4:(b + 1) * 4, 0:64],
                                    in_=pbb[k_][:, 0:256].rearrange("p (t d) -> p t d", t=4),
                                    reads=[B_pbb[k_]], writes=[B_VV[hb]])

                          jobs += [jq, jk, jv]
                      return jobs

                  def attention(h, side_jobs):
                      hb = h % 2
                      iters = []
                      for qb in range(8):
                          for kt in range(4 * qb + 4):
                              iters.append((qb, kt))
                      nside = len(side_jobs)
                      every = max(1, len(iters) // (nside + 1)) if nside else 0
                      for n, (qb, kt) in enumerate(iters):
                          j = kt - 4 * qb
                          c0 = max(j, 0) * 128
                          k_ = st2["ps"] % NST; st2["ps"] += 1
                          sc.op("pe", "matmul", out=psT[k_][:, c0:512], lhsT=KT[hb][0:96, kt * 128:(kt + 1) * 128],
                                rhs=QT[hb][0:96, qb * 512 + c0:(qb + 1) * 512], start=True, stop=True,
                                reads=[B_KT[hb], B_QT[hb]], writes=[B_psT[k_]])
                          r = st2["pt"] % NPT; st2["pt"] += 1
                          sc.op("act", "activation", out=PT[r][:, c0:512], in_=psT[k_][:, c0:512], func=AF.Exp, scale=SCALE,
                                reads=[B_psT[k_]], writes=[B_PT[r]])
                          if kt >= 4 * qb:
                              sc.op("dve", "tensor_tensor", out=PT[r][:, c0:c0 + 128], in0=PT[r][:, c0:c0 + 128], in1=trib[:], op=ALU.mult,
                                    reads=[B_PT[r], B_c2], writes=[B_PT[r]])
                          pb = qb % 2
                          for t in range(c0 // 128, 4):
                              sc.op("pe", "matmul", out=poT[pb][:, t * 128:t * 128 + 65], lhsT=PT[r][:, t * 128:(t + 1) * 128], rhs=VV[hb][:, kt, 0:65],
                                    start=(kt == 0 and t == 0), stop=(kt == 4 * qb + 3 and t == 3), skip_group_check=True,
                                    reads=[B_VV[hb], B_PT[r]], writes=[B_poT[pb]])
                          if kt == 4 * qb + 3:
                              q2 = st2["nq"] % 2; st2["nq"] += 1
                              pv = poT[pb][:].rearrange("p (t c) -> p t c", t=4)
                              sc.op("dve", "reciprocal", out=rr[q2][:, 0:4], in_=pv[:, :, 64], reads=[B_poT[pb]], writes=[B_rr[q2]])
                              for t in range(4):
                                  sc.op("dve", "tensor_scalar", out=ymT[:, qb * 4 + t, h * 64:(h + 1) * 64], in0=poT[pb][:, t * 128:t * 128 + 64],
                                        scalar1=rr[q2][:, t:t + 1], scalar2=None, op0=ALU.mult, reads=[B_poT[pb], B_rr[q2]], writes=[B_ymT])
                          if nside and n % every == every - 1 and side_jobs:
                              side_jobs.pop(0)()
                      while side_jobs:
                          side_jobs.pop(0)()

                  B_wconv = Buf("wconv")

                  def conv_job(e):
                      def job():
                          gu = wbf_d[e][:, 0:4096].rearrange("p (c n) -> p c n", n=512)
                          DMA("pool", B_wconv, [], [], out=gu[:, :, 0:256], in_=w_eg[e].rearrange("(p c) n -> p c n", p=128))
                          DMA("pool", B_wconv, [], [], out=gu[:, :, 256:512], in_=w_eu[e].rearrange("(p c) n -> p c n", p=128))
                          DMA("pool", B_wconv, [], [], out=wbf_d[e][:, 4096:6144].rearrange("p (c n) -> p c n", n=1024),
                              in_=w_ed[e].rearrange("(p c) n -> p c n", p=128))
                      return job

                  NH = 8 if not (debug and len(debug) > 2) else debug[2]
                  for j in build_jobs(0):
                      j()
                  for h in range(NH):
                      side = build_jobs(h + 1) if h + 1 < NH else []
                      convs = [conv_job(e) for e in range(4 * h, 4 * h + 4)] if NH == 8 else []
                      merged = []
                      while side or convs:
                          for _ in range(6):
                              if side:
                                  merged.append(side.pop(0))
                          if convs:
                              merged.append(convs.pop(0))
                      attention(h, merged)
                  sc.run_block("p2")

        stage = debug[3] if (debug and len(debug) > 3) else 7
        route_i = mk(es, "route_i", [128, NT, 2], I32)
        route_g = mk(es, "route_g", [128, NT, 2], F32)
        B_xbuf, B_ybuf, B_h1d = Buf("xbuf"), Buf("ybuf"), Buf("h1d")

        with ExitStack() as e3:
            NTILE = NT if not (debug and len(debug) > 4) else debug[4]
            wgf = mk(e3, "wgf", [128, KC, 528], BF16)
            wgt = mk(e3, "wgt", [128, KC, 1280], BF16)
            wo = mk(e3, "wo", [128, KC, D], BF16)
            wrt = mk(e3, "wrt", [128, KC, 36], F32)
            brt = mk(e3, "brt", [1, 36], F32)
            gup = mk(e3, "gup", [32, 256], F32)
            g1 = mk(e3, "g1", [128, D], F32)
            g2 = mk(e3, "g2", [128, D], F32)
            ggl = mk(e3, "ggl", [128, 512], F32)
            cgla = mk(e3, "cgla", [128, 256], F32)
            ctri = mk(e3, "ctri", [128, 512], F32)
            gmask = mk(e3, "gmask", [128, 512], BF16)
            ecap = mk(e3, "ecap", [128, 32], F32)
            ones_f = mk(e3, "ones_f", [128, 128], F32)
            jfr = mk(e3, "jfr", [128, 32], F32, 4)
            jfc = {"i": 0}

            def nextjf():
                jfc["i"] += 1
                return jfr[jfc["i"] % 4]
            NX = debug[10] if (debug and len(debug) > 10) else 5
            R2N = debug[9] if (debug and len(debug) > 9) else 2
            RBIG = 2
            xt = mk(e3, "xt", [128, D], F32, NX)
            st = mk(e3, "st", [128, 8], F32, R2N)
            xs = mk(e3, "xs", [128, D], BF16, R2N)
            xnT = mk(e3, "xnT", [128, KC, 128], BF16, R2N)
            R3 = 3
            v_sb = mk(e3, "v_sb", [128, 512], BF16, R3)
            gk_sb = mk(e3, "gk_sb", [128, 256], F32, R3)
            qk_sb = mk(e3, "qk_sb", [128, 4, 128], F32, R3)
            gmul = mk(e3, "gmul", [128, 512], F32, R3)
            glrT = mk(e3, "glrT", [32, 128], F32, R3)
            sg = mk(e3, "sg", [128, 512], F32)
            ez = mk(e3, "ez", [128, 256], F32)
            sp_sb = mk(e3, "sp_sb", [128, 256], F32, R2N)
            Eq = mk(e3, "Eq", [128, 2, 128], F32, R2N)
            Ek = mk(e3, "Ek", [128, 2, 128], F32, R2N)
            Er = mk(e3, "Er", [128, 256], F32, R2N)
            dec = mk(e3, "dec", [128, 2, 2], F32, R2N)
            qeT = mk(e3, "qeT", [128, 2, 128], BF16, R2N)
            qbd = mk(e3, "qbd", [128, 2, 256], BF16, R2N)
            keT = mk(e3, "keT", [128, 2, 128], BF16, R2N)
            kdz = mk(e3, "kdz", [128, 2, 256], BF16, R2N)
            attm = mk(e3, "attm", [128, 512], BF16, R2N)
            S32 = mk(e3, "S32", [128, 2, 128], F32)
            Sb = mk(e3, "Sb", [128, 2, 256], BF16)
            so = mk(e3, "so", [128, 8], F32, R2N)
            yg = mk(e3, "yg", [128, 512], BF16, R2N)
            ygT = mk(e3, "ygT", [128, 4, 128], BF16, R2N)
            ymTt = mk(e3, "ymTt", [128, 4, 128], BF16, R2N)
            h1s = mk(e3, "h1s", [128, D], F32, RBIG)
            xn2 = mk(e3, "xn2", [128, D], F32, RBIG)
            xn2b = mk(e3, "xn2b", [128, D], BF16, RBIG)
            xn2T = mk(e3, "xn2T", [128, KC, 128], F32)
            lg = mk(e3, "lg", [128, 36], F32, R2N)
            rs = mk(e3, "rs", [128, 16], F32, 2)
            mgt = mk(e3, "mgt", [128, 4], F32, R2N)
            els = mk(e3, "els", [128, 8], F32, R2N)
            els2 = mk(e3, "els2", [128, 8], F32, R2N)
            mk1 = mk(e3, "mk1", [128, 8], F32, R2N)
            mk2 = mk(e3, "mk2", [128, 8], F32, R2N)
            A1 = mk(e3, "A1", [128, 32], F32, R2N)
            A2 = mk(e3, "A2", [128, 32], F32, R2N)
            AA = mk(e3, "AA", [128, 32], F32, R2N)
            posc = mk(e3, "posc", [128, 32], F32, R2N)
            Rr = mk(e3, "Rr", [128, 32], F32)
            p_tp = mk(e3, "p_tp", [128, KC, 128], BF16, psum=True)
            NPA = 4
            pa = mk(e3, "pa", [128, 512], F32, NPA, psum=True)
            po = mk(e3, "po", [128, 512], F32, psum=True)
            pu = mk(e3, "pu", [128, 2, 2, 128], F32, psum=True)
            pmix = mk(e3, "pmix", [128, 512], F32, psum=True)
            pyT = TB(pmix.t[:, 0:256].bitcast(BF16).rearrange("p (m t) -> p m t", m=4), "pyT")
            psm = TB(pmix.t[:, 256:384], "psm")
            pyT.b = pmix.b
            psm.b = pmix.b

            for c in range(KC):
                r = slice(c * 128, (c + 1) * 128)
                DMA("pool", wgt, [], [wgt], out=wgt.t[:, c, :], in_=w_gt[r, :])
                DMA("pool", wgf, [], [wgf], out=wgf.t[:, c, :], in_=w_gf[r, :])
            for c in range(KC):
                r = slice(c * 128, (c + 1) * 128)
                DMA("pool", wo, [], [wo], out=wo.t[:, c, :], in_=w_out[r, :])
            for c in range(KC):
                r = slice(c * 128, (c + 1) * 128)
                DMA("sp", wrt, [], [wrt], out=wrt.t[:, c, :], in_=w_rt[r, :])
            DMA("sp", brt, [], [brt], out=brt.t[:], in_=b_rt)
            OP("dve", "memset", [], [gup], ap=gup.t[:], constant=0.0)
            DMA("sp", gup, [], [gup], out=gup.t[0:17, :], in_=gate_up)
            DMA("sp", g1, [], [g1], out=g1.t[:], in_=g_attn.partition_broadcast(128))
            DMA("sp", g2, [], [g2], out=g2.t[:], in_=g_ffn.partition_broadcast(128))
            for hh in range(4):
                DMA("sp", ggl, [], [ggl], out=ggl.t[:, hh * 128:(hh + 1) * 128], in_=g_gla.partition_broadcast(128))
            DMA("sp", cgla, [], [cgla], out=cgla.t[:], in_=c_gla)
            DMA("sp", ctri, [], [ctri], out=ctri.t[:], in_=c_tri)
            DMA("sp", ecap, [], [ecap], out=ecap.t[:], in_=c_ecap)
            OP("dve", "memset", [], [ones_f], ap=ones_f.t[:], constant=1.0)
            OP("dve", "memset", [], [S32], ap=S32.t[:], constant=0.0)
            OP("dve", "memset", [], [Sb], ap=Sb.t[:], constant=0.0)
            OP("dve", "memset", [], [Rr], ap=Rr.t[:], constant=0.0)
            for k_ in range(R3):
                OP("dve", "memset", [], [glrT[k_]], ap=glrT[k_].t[:], constant=1.0)
            for k_ in range(R2N):
                OP("dve", "memset", [], [qbd[k_]], ap=qbd[k_].t[:], constant=0.0)
                OP("dve", "memset", [], [kdz[k_]], ap=kdz[k_].t[:], constant=0.0)
            cnt = {"pa": 0}

            pac = {}

            def nextpa(tag="a"):
                g_ = PA_CFG.get(tag, PA_CFG.get(tag[0]))
                key = tuple(g_)
                k_ = pac.get(key, 0)
                pac[key] = k_ + 1
                return pa[g_[k_ % len(g_)]]

            def load_x(i):
                X = xt[i % NX]
                DMA("sp", X, [], [X], out=X.t[:], in_=x[i * 128:(i + 1) * 128, :])

            LN8 = float(np.log(0.125))

            def s1a(i):
                X, ST, XS, XT = xt[i % NX], st[i % R2N], xs[i % R2N], xnT[i % R2N]
                r3 = i % R3
                OP("act", "activation", [X], [ST, XS], out=XS.t[:], in_=X.t[:], func=AF.Square, accum_out=ST.t[:, 0:1])
                OP("act", "activation", [ST], [ST], out=ST.t[:, 1:2], in_=ST.t[:, 0:1], func=AF.Ln, scale=1.0 / D, bias=EPS)
                OP("act", "activation", [ST], [ST], out=ST.t[:, 2:3], in_=ST.t[:, 1:2], func=AF.Exp, scale=-0.5)
                OP("dve", "scalar_tensor_tensor", [X, ST, g1], [XS], out=XS.t[:], in0=X.t[:], scalar=ST.t[:, 2:3], in1=g1.t[:],
                   op0=ALU.mult, op1=ALU.mult)
                for c in range(KC):
                    OP("pe", "transpose", [XS, B_ident], [p_tp], out=p_tp.t[:, c, :], in_=XS.t[:, c * 128:(c + 1) * 128], identity=ident_b[:])
                OP("act", "activation", [p_tp], [XT], out=XT.t[:], in_=p_tp.t[:], func=AF.Copy)
                A = nextpa("a1")
                for c in range(KC):
                    OP("pe", "matmul", [XT, wgt], [A], out=A.t[:, 0:256], lhsT=XT.t[:, c, :], rhs=wgt.t[:, c, 0:256],
                       start=(c == 0), stop=(c == KC - 1))
                for c in range(KC):
                    OP("pe", "matmul", [XT, wgf], [A], out=A.t[0:16, 256:384], lhsT=wgf.t[:, c, 512:528], rhs=XT.t[:, c, :],
                       start=(c == 0), stop=(c == KC - 1))
                OP("act", "activation", [A], [gk_sb[r3]], out=gk_sb[r3].t[:], in_=A.t[:, 0:256], func=AF.Copy)
                OP("act", "activation", [A], [glrT[r3]], out=glrT[r3].t[0:16, :], in_=A.t[0:16, 256:384], func=AF.Copy)
                A = nextpa("a2")
                for c in range(KC):
                    OP("pe", "matmul", [XT, wgt], [A], out=A.t[:], lhsT=XT.t[:, c, :], rhs=wgt.t[:, c, 256:768],
                       start=(c == 0), stop=(c == KC - 1))
                OP("act", "activation", [A], [v_sb[r3]], out=v_sb[r3].t[:], in_=A.t[:], func=AF.Copy)
                A = nextpa("a3")
                for c in range(KC):
                    OP("pe", "matmul", [XT, wgt], [A], out=A.t[:], lhsT=XT.t[:, c, :], rhs=wgt.t[:, c, 768:1280],
                       start=(c == 0), stop=(c == KC - 1))
                OP("act", "activation", [A], [sg], out=sg.t[:], in_=A.t[:], func=AF.Exp, scale=-1.0)
                OP("act", "activation", [sg], [sg], out=sg.t[:], in_=sg.t[:], func=AF.Ln, bias=1.0)
                OP("act", "activation", [sg], [sg], out=sg.t[:], in_=sg.t[:], func=AF.Exp, scale=-1.0)
                OP("dve", "tensor_tensor", [A, sg], [sg], out=sg.t[:], in0=A.t[:], in1=sg.t[:], op=ALU.mult)
                OP("dve", "tensor_tensor", [sg, ggl], [gmul[r3]], out=gmul[r3].t[:], in0=sg.t[:], in1=ggl.t[:], op=ALU.mult)
                A = nextpa("a4")
                for m in range(4):
                    for c in range(KC):
                        OP("pe", "matmul", [XT, wgf], [A], out=A.t[:, m * 128:(m + 1) * 128], lhsT=wgf.t[:, c, m * 128:(m + 1) * 128],
                           rhs=XT.t[:, c, :], start=(c == 0), stop=(c == KC - 1))
                OP("act", "activation", [A], [qk_sb[r3]], out=qk_sb[r3].t[:], in_=A.t[:].rearrange("p (m t) -> p m t", m=4), func=AF.Copy)

            sub = debug[6] if (debug and len(debug) > 6) else 9

            def s1b(i):
                r3, r2 = i % R3, i % R2N
                A = nextpa("b1")
                OP("pe", "matmul", [glrT[r3], gup], [A], out=A.t[:, 0:256], lhsT=glrT[r3].t[0:17, :], rhs=gup.t[0:17, :], start=True, stop=True)
                OP("act", "activation", [A], [ez], out=ez.t[:], in_=A.t[:, 0:256], func=AF.Exp, scale=-1.0)
                OP("act", "activation", [ez], [sp_sb[r2]], out=sp_sb[r2].t[:], in_=ez.t[:], func=AF.Ln, bias=1.0)
                if sub < 2:
                    return
                OP("pe", "matmul", [sp_sb[r2], cgla], [A], out=A.t[:, 256:512], lhsT=cgla.t[:, 128:256], rhs=sp_sb[r2].t[:], start=True, stop=True)
                Bk = nextpa("b2")
                for m in range(2):
                    OP("pe", "matmul", [sp_sb[r2], cgla], [Bk], out=Bk.t[:, m * 128:(m + 1) * 128], lhsT=sp_sb[r2].t[:, m * 128:(m + 1) * 128],
                       rhs=cgla.t[:, 0:128], start=True, stop=True)
                bT = Bk.t[:, 0:256].rearrange("p (m t) -> p m t", m=2)
                if sub < 3:
                    return
                OP("act", "activation", [A], [Er[r2]], out=Er[r2].t[:], in_=A.t[:, 256:512], func=AF.Exp)
                OP("act", "activation", [Bk], [Eq[r2]], out=Eq[r2].t[:], in_=bT, func=AF.Exp, bias=LN8)
                OP("act", "activation", [Bk], [Ek[r2]], out=Ek[r2].t[:], in_=bT, func=AF.Exp, scale=-1.0)
                if sub < 4:
                    return
                bT4 = Bk.t[:, 0:256].rearrange("p (m c j) -> p m c j", m=2, c=2)
                OP("act", "activation", [Bk], [dec[r2]], out=dec[r2].t[:], in_=bT4[:, :, :, 63], func=AF.Exp)
                if sub < 5:
                    return
                OP("dve", "tensor_tensor", [qk_sb[r3], Eq[r2]], [qeT[r2]], out=qeT[r2].t[:], in0=qk_sb[r3].t[:, 0:2, :], in1=Eq[r2].t[:], op=ALU.mult)
                OP("dve", "tensor_tensor", [qk_sb[r3], Ek[r2]], [keT[r2]], out=keT[r2].t[:], in0=qk_sb[r3].t[:, 2:4, :], in1=Ek[r2].t[:], op=ALU.mult)
                OP("dve", "tensor_tensor", [qk_sb[r3], Eq[r2]], [qbd[r2]], out=qbd[r2].t[0:64, :, 0:128], in0=qk_sb[r3].t[0:64, 0:2, :], in1=Eq[r2].t[0:64, :, :], op=ALU.mult)
                OP("dve", "tensor_tensor", [qk_sb[r3], Eq[r2]], [qbd[r2]], out=qbd[r2].t[64:128, :, 128:256], in0=qk_sb[r3].t[64:128, 0:2, :], in1=Eq[r2].t[64:128, :, :], op=ALU.mult)
                OP("dve", "tensor_tensor", [gk_sb[r3], Er[r2]], [kdz[r2]], out=kdz[r2].t[0:64, 0, :], in0=gk_sb[r3].t[0:64, :], in1=Er[r2].t[0:64, :], op=ALU.mult)
                OP("dve", "tensor_tensor", [gk_sb[r3], Er[r2]], [kdz[r2]], out=kdz[r2].t[64:128, 1, :], in0=gk_sb[r3].t[64:128, :], in1=Er[r2].t[64:128, :], op=ALU.mult)
                if sub < 6:
                    return
                Ck = nextpa("b3")
                for hp in range(2):
                    OP("pe", "matmul", [keT[r2], qbd[r2]], [Ck], out=Ck.t[:, hp * 256:(hp + 1) * 256], lhsT=keT[r2].t[:, hp, :], rhs=qbd[r2].t[:, hp, :],
                       start=True, stop=True)
                OP("dve", "tensor_tensor", [Ck, gmask], [attm[r2]], out=attm[r2].t[:], in0=Ck.t[:], in1=gmask.t[:], op=ALU.mult)

            def s2(i):
                r3, r2 = i % R3, i % R2N
                for cc in range(2):
                    for hh in range(4):
                        pb_ = slice((hh % 2) * 64, (hh % 2) * 64 + 64)
                        OP("pe", "matmul", [kdz[r2], v_sb[r3]], [pu], out=pu.t[pb_, cc, hh // 2, :], lhsT=kdz[r2].t[:, cc, hh * 64:(hh + 1) * 64],
                           rhs=v_sb[r3].t[:, hh * 128:(hh + 1) * 128], start=True, stop=True)
                for hh in range(4):
                    OP("pe", "matmul", [attm[r2], v_sb[r3]], [po], out=po.t[:, hh * 128:(hh + 1) * 128], lhsT=attm[r2].t[:, hh * 128:(hh + 1) * 128],
                       rhs=v_sb[r3].t[:, hh * 128:(hh + 1) * 128], start=(hh == 0), stop=False, skip_group_check=True)
                for cc in range(2):
                    cs_ = slice(cc * 64, cc * 64 + 64)
                    for hp in range(2):
                        OP("pe", "matmul", [qeT[r2], Sb], [po], out=po.t[cs_, hp * 256:(hp + 1) * 256], lhsT=qeT[r2].t[:, hp, cs_],
                           rhs=Sb.t[:, hp, :], start=False, stop=(cc == 1 and hp == 1), skip_group_check=True)
                    for hp in range(2):
                        OP("dve", "scalar_tensor_tensor", [S32, dec[r2], pu], [S32], out=S32.t[:, hp, :], in0=S32.t[:, hp, :],
                           scalar=dec[r2].t[:, hp, cc:cc + 1], in1=pu.t[:, cc, hp, :], op0=ALU.mult, op1=ALU.add)
                    OP("act", "activation", [S32], [Sb], out=Sb.t[0:64, :, 0:128], in_=S32.t[0:64, :, :], func=AF.Copy)
                    OP("dve", "tensor_copy", [S32], [Sb], out=Sb.t[64:128, :, 128:256], in_=S32.t[64:128, :, :])
                SO = so[r2]
                for hh in range(4):
                    OP("act", "activation", [po], [SO, yg[r2]], out=yg[r2].t[:, hh * 128:(hh + 1) * 128], in_=po.t[:, hh * 128:(hh + 1) * 128], func=AF.Square,
                       accum_out=SO.t[:, hh:hh + 1])
                OP("act", "activation", [SO], [SO], out=SO.t[:, 4:8], in_=SO.t[:, 0:4], func=AF.Ln, scale=1.0 / 128, bias=EPS)
                OP("act", "activation", [SO], [SO], out=SO.t[:, 4:8], in_=SO.t[:, 4:8], func=AF.Exp, scale=-0.5)
                for hh in range(4):
                    hs_ = slice(hh * 128, (hh + 1) * 128)
                    OP("dve", "scalar_tensor_tensor", [po, SO, gmul[r3]], [yg[r2]], out=yg[r2].t[:, hs_], in0=po.t[:, hs_],
                       scalar=SO.t[:, 4 + hh:5 + hh], in1=gmul[r3].t[:, hs_], op0=ALU.mult, op1=ALU.mult)
                for hh in range(4):
                    OP("pe", "transpose", [yg[r2], B_ident], [pyT], out=pyT.t[:, hh, :], in_=yg[r2].t[:, hh * 128:(hh + 1) * 128], identity=ident_b[:])
                OP("act", "activation", [pyT], [ygT[r2]], out=ygT[r2].t[:], in_=pyT.t[:], func=AF.Copy)

            def s3(i):
                r2 = i % R2N
                rb = i % RBIG
                X = xt[i % NX]
                gc = slice(i * 128, (i + 1) * 128)
                H = h1s[rb]
                A = nextpa("c1")
                ymv = A.t[:, 0:256].bitcast(BF16).rearrange("p (m t) -> p m t", m=4)
                for c in range(4):
                    OP("pe", "transpose", [B_ymT, B_ident], [A], out=ymv[:, c, :], in_=ymT[:, i, c * 128:(c + 1) * 128], identity=ident_b[:])
                OP("act", "activation", [A], [ymTt[r2]], out=ymTt[r2].t[:], in_=ymv, func=AF.Copy)
                for half in range(2):
                    hc = slice(half * 512, (half + 1) * 512)
                    A = nextpa("c2")
                    for c in range(4):
                        OP("pe", "matmul", [ygT[r2], wo], [A], out=A.t[:], lhsT=ygT[r2].t[:, c, :], rhs=wo.t[:, c, hc], start=(c == 0), stop=False)
                    for c in range(4):
                        OP("pe", "matmul", [ymTt[r2], wo], [A], out=A.t[:], lhsT=ymTt[r2].t[:, c, :], rhs=wo.t[:, 4 + c, hc], start=False, stop=(c == 3))
                    OP("dve", "tensor_tensor", [A, X], [H], out=H.t[:, hc], in0=A.t[:], in1=X.t[:, hc], op=ALU.add)
                DMA("sp", H, [H], [], out=h1_d[gc, :], in_=H.t[:])
                if stage < 4:
                    return
                ST = st[r2]
                OP("act", "activation", [H], [ST, xn2b[rb]], out=xn2b[rb].t[:], in_=H.t[:], func=AF.Square, accum_out=ST.t[:, 4:5])
                OP("act", "activation", [ST], [ST], out=ST.t[:, 5:6], in_=ST.t[:, 4:5], func=AF.Ln, scale=1.0 / D, bias=EPS)
                OP("act", "activation", [ST], [ST], out=ST.t[:, 6:7], in_=ST.t[:, 5:6], func=AF.Exp, scale=-0.5)
                XN = xn2[rb]
                OP("dve", "scalar_tensor_tensor", [H, ST, g2], [XN], out=XN.t[:], in0=H.t[:], scalar=ST.t[:, 6:7], in1=g2.t[:],
                   op0=ALU.mult, op1=ALU.mult)
                OP("pool", "tensor_copy", [XN], [xn2b[rb]], out=xn2b[rb].t[:], in_=XN.t[:])
                for half in range(2):
                    A = nextpa("c3")
                    for c in range(4):
                        cc = half * 4 + c
                        OP("pe", "transpose", [XN, B_ident], [A], out=A.t[:, c * 128:(c + 1) * 128], in_=XN.t[:, cc * 128:(cc + 1) * 128], identity=ident_f[:])
                    OP("act", "activation", [A], [xn2T], out=xn2T.t[:, half * 4:(half + 1) * 4, :], in_=A.t[:].rearrange("p (m t) -> p m t", m=4), func=AF.Copy)
                for c in range(KC):
                    OP("pe", "matmul", [xn2T, wrt], [psm], out=psm.t[:, 0:36], lhsT=xn2T.t[:, c, :], rhs=wrt.t[:, c, :], start=(c == 0), stop=False)
                OP("pe", "matmul", [ones_f, brt], [psm], out=psm.t[:, 0:36], lhsT=ones_f.t[0:1, :], rhs=brt.t[0:1, :], start=False, stop=True)
                L, RS, MG = lg[r2], rs[r2], mgt[r2]
                OP("dve", "tensor_copy", [psm], [L], out=L.t[:], in_=psm.t[:, 0:36])
                OP("dve", "tensor_reduce", [L], [RS], out=RS.t[:, 0:1], in_=L.t[:, 0:4], axis=AX.X, op=ALU.max)
                OP("dve", "tensor_scalar", [L, RS], [MG], out=MG.t[:], in0=L.t[:, 0:4], scalar1=RS.t[:, 0:1], scalar2=None, op0=ALU.is_equal)
                OP("dve", "tensor_scalar", [RS], [RS], out=RS.t[:, 1:2], in0=RS.t[:, 0:1], scalar1=-1.0, scalar2=None, op0=ALU.mult)
                JF = nextjf()
                OP("act", "activation", [L, RS], [RS, JF], out=JF.t[:, 0:4], in_=L.t[:, 0:4], func=AF.Exp, bias=RS.t[:, 1:2], accum_out=RS.t[:, 2:3])
                OP("dve", "reciprocal", [RS], [RS], out=RS.t[:, 3:4], in_=RS.t[:, 2:3])
                E_, E2, M1, M2 = els[r2], els2[r2], mk1[r2], mk2[r2]
                OP("dve", "tensor_scalar", [L, MG], [E_], out=E_.t[:], in0=L.t[:, 4:12], scalar1=MG.t[:, 0:1], scalar2=None, op0=ALU.mult)
                for g_ in range(1, 4):
                    OP("dve", "scalar_tensor_tensor", [L, MG, E_], [E_], out=E_.t[:], in0=L.t[:, 4 + 8 * g_:12 + 8 * g_], scalar=MG.t[:, g_:g_ + 1],
                       in1=E_.t[:], op0=ALU.mult, op1=ALU.add)
                OP("dve", "tensor_reduce", [E_], [RS], out=RS.t[:, 4:5], in_=E_.t[:], axis=AX.X, op=ALU.max)
                OP("dve", "tensor_scalar", [E_, RS], [M1], out=M1.t[:], in0=E_.t[:], scalar1=RS.t[:, 4:5], scalar2=None, op0=ALU.is_equal)
                OP("dve", "scalar_tensor_tensor", [M1, E_], [E2], out=E2.t[:], in0=M1.t[:], scalar=-1e30, in1=E_.t[:], op0=ALU.mult, op1=ALU.add)
                OP("dve", "tensor_reduce", [E2], [RS], out=RS.t[:, 5:6], in_=E2.t[:], axis=AX.X, op=ALU.max)
                OP("dve", "tensor_scalar", [E2, RS], [M2], out=M2.t[:], in0=E2.t[:], scalar1=RS.t[:, 5:6], scalar2=None, op0=ALU.is_equal)
                OP("dve", "tensor_scalar", [RS], [RS], out=RS.t[:, 6:7], in0=RS.t[:, 4:5], scalar1=-1.0, scalar2=None, op0=ALU.mult)
                OP("act", "activation", [RS], [RS], out=RS.t[:, 7:8], in_=RS.t[:, 5:6], func=AF.Exp, bias=RS.t[:, 6:7])
                OP("dve", "tensor_scalar", [RS], [RS], out=RS.t[:, 8:9], in0=RS.t[:, 7:8], scalar1=1.0, scalar2=None, op0=ALU.add)
                OP("dve", "reciprocal", [RS], [RS], out=RS.t[:, 9:10], in_=RS.t[:, 8:9])
                OP("dve", "tensor_tensor", [RS], [route_g], out=route_g.t[:, i, 0:1], in0=RS.t[:, 9:10], in1=RS.t[:, 3:4], op=ALU.mult)
                OP("dve", "tensor_tensor", [RS, route_g], [route_g], out=route_g.t[:, i, 1:2], in0=route_g.t[:, i, 0:1], in1=RS.t[:, 7:8], op=ALU.mult)
                for g_ in range(4):
                    es_ = slice(g_ * 8, (g_ + 1) * 8)
                    OP("dve", "tensor_scalar", [M1, MG], [A1[r2]], out=A1[r2].t[:, es_], in0=M1.t[:], scalar1=MG.t[:, g_:g_ + 1], scalar2=None, op0=ALU.mult)
                    OP("dve", "tensor_scalar", [M2, MG], [A2[r2]], out=A2[r2].t[:, es_], in0=M2.t[:], scalar1=MG.t[:, g_:g_ + 1], scalar2=None, op0=ALU.mult)
                OP("dve", "tensor_tensor", [A1[r2], A2[r2]], [AA[r2]], out=AA[r2].t[:], in0=A1[r2].t[:], in1=A2[r2].t[:], op=ALU.add)
                OP("pe", "matmul", [ctri, AA[r2]], [psm], out=psm.t[:, 64:96], lhsT=ctri.t[:, 384:512], rhs=AA[r2].t[:], start=True, stop=True)
                OP("pe", "matmul", [ones_f, AA[r2]], [psm], out=psm.t[:, 96:128], lhsT=ones_f.t[:], rhs=AA[r2].t[:], start=True, stop=True)
                PC = posc[r2]
                OP("dve", "tensor_tensor", [psm, Rr], [PC], out=PC.t[:], in0=psm.t[:, 64:96], in1=Rr.t[:], op=ALU.add)
                OP("dve", "tensor_tensor", [psm, Rr], [Rr], out=Rr.t[:], in0=psm.t[:, 96:128], in1=Rr.t[:], op=ALU.add)
                OP("dve", "tensor_tensor", [PC, ecap], [PC], out=PC.t[:], in0=PC.t[:], in1=ecap.t[:], op=ALU.add)
                JF = nextjf()
                OP("dve", "scalar_tensor_tensor", [A1[r2], PC], [RS, JF], out=JF.t[:], in0=A1[r2].t[:], scalar=1.0, in1=PC.t[:], op0=ALU.mult, op1=ALU.mult,
                   accum_out=RS.t[:, 10:11])
                JF = nextjf()
                OP("dve", "scalar_tensor_tensor", [A2[r2], PC], [RS, JF], out=JF.t[:], in0=A2[r2].t[:], scalar=1.0, in1=PC.t[:], op0=ALU.mult, op1=ALU.mult,
                   accum_out=RS.t[:, 11:12])
                OP("dve", "tensor_copy", [RS], [route_i], out=route_i.t[:, i, :], in_=RS.t[:, 10:12])
                if stage < 5:
                    return
                for k_ in range(2):
                    DMA("pool", xn2b[rb], [xn2b[rb], route_i], [], name="indirect_dma_start", out=xbuf_d[:, :],
                        out_offset=bass.IndirectOffsetOnAxis(ap=route_i.t[:, i, k_:k_ + 1], axis=0), in_=xn2b[rb].t[:, :], in_offset=None)

            for hh in range(4):
                OP("dve", "tensor_copy", [ctri], [gmask], out=gmask.t[:, hh * 128:(hh + 1) * 128], in_=ctri.t[:, 128:256])
            ORDER3 = debug[8] if (debug and len(debug) > 8) else 0
            if stage >= 1 and ORDER3 == 1:
                for j in range(min(3, NTILE)):
                    load_x(j)
                for i in range(NTILE):
                    if i + 3 < NTILE:
                        load_x(i + 3)
                    s1a(i)
                    s1b(i)
                    s2(i)
                    s3(i)
            elif stage >= 1:
                load_x(0)
                load_x(1)
                s1a(0)
                s1a(1)
                if stage >= 2:
                    s1b(0)
                for i in range(NTILE):
                    if i + 2 < NTILE:
                        load_x(i + 2)
                        s1a(i + 2)
                    if i + 1 < NTILE and stage >= 2:
                        s1b(i + 1)
                    if stage >= 3:
                        s2(i)
                        if i >= 1:
                            s3(i - 1)
                if stage >= 3:
                    s3(NTILE - 1)
            sc.run_block("p3")

        if stage >= 6:
            with ExitStack() as e4:
                NEX = NE if not (debug and len(debug) > 7) else debug[7]
                RW = 4
                wall = mk(e4, "wall", [128, 6144], BF16, RW)
                wgu = [TB(w_.t[:, 0:4096].rearrange("p (c n) -> p c n", n=512), "x") for w_ in wall]
                wd = [TB(w_.t[:, 4096:6144].rearrange("p (c n) -> p c n", n=1024), "x") for w_ in wall]
                for k_ in range(RW):
                    wgu[k_].b = wall[k_].b
                    wd[k_].b = wall[k_].b
                xe = mk(e4, "xe", [128, 3, D], BF16, 3)
                xeT = mk(e4, "xeT", [128, KC, CAP], BF16, 2)
                sgm = mk(e4, "sgm", [128, CAP], F32, 2)
                hT = mk(e4, "hT", [128, 2, CAP], BF16, 2)
                ye = mk(e4, "ye", [128, 3, D], BF16, 2)
                ptp4 = mk(e4, "ptp4", [128, KC, 128], BF16, psum=True)
                pg = mk(e4, "pg", [128, 512], F32, 4, psum=True)
                py = mk(e4, "py", [128, 512], F32, 3, psum=True)
                c4 = {"py": 0}

                def load_w(e):
                    s_ = e % RW
                    DMA("pool", wall[s_], [], [wall[s_]], out=wall[s_].t[:], in_=wbf_d[e])

                def load_xe(e):
                    s_ = e % 3
                    DMA("sp", xe[s_], [], [xe[s_]], out=xe[s_].t[:], in_=xbuf_d[e * CAP:(e + 1) * CAP, :].rearrange("(s p) d -> p s d", p=128))

                def stA(e):
                    s_ = e % 2
                    xev = xe[e % 3].t[:].rearrange("p s (k c) -> p s c k", c=KC)
                    for s in range(3):
                        for c in range(KC):
                            OP("pe", "transpose", [xe[e % 3], B_ident], [ptp4], out=ptp4.t[:, c, :], in_=xev[:, s, c, :], identity=ident_b[:])
                        OP("act", "activation", [ptp4], [xeT[s_]], out=xeT[s_].t[:, :, s * 128:(s + 1) * 128], in_=ptp4.t[:], func=AF.Copy)

                def stB(e):
                    s_ = e % 2
                    w_ = e % RW
                    wv = wgu[w_].t[:].rearrange("p c (w j m) -> p c w m j", w=2, m=2)
                    for m in range(2):
                        for which in range(2):
                            P_ = pg[which * 2 + m]
                            for c in range(KC):
                                OP("pe", "matmul", [wgu[w_], xeT[s_]], [P_], out=P_.t[:, 0:CAP], lhsT=wv[:, c, which, m, :],
                                   rhs=xeT[s_].t[:, c, :], start=(c == 0), stop=(c == KC - 1))
                        G, U, SG = pg[m], pg[2 + m], sgm[m]
                        OP("act", "activation", [G], [SG], out=SG.t[:], in_=G.t[:, 0:CAP], func=AF.Exp, scale=-1.0)
                        OP("act", "activation", [SG], [SG], out=SG.t[:], in_=SG.t[:], func=AF.Ln, bias=1.0)
                        OP("act", "activation", [SG], [SG], out=SG.t[:], in_=SG.t[:], func=AF.Exp, scale=-1.0)
                        OP("dve", "tensor_tensor", [G, SG], [SG], out=SG.t[:], in0=G.t[:, 0:CAP], in1=SG.t[:], op=ALU.mult)
                        OP("dve", "tensor_tensor", [U, SG], [hT[s_]], out=hT[s_].t[:, m, :], in0=U.t[:, 0:CAP], in1=SG.t[:], op=ALU.mult)

                def stC(e):
                    s_ = e % 2
                    for s in range(3):
                        for half in range(2):
                            Y = py[c4["py"] % 3]
                            c4["py"] += 1
                            for m in range(2):
                                OP("pe", "matmul", [hT[s_], wd[e % RW]], [Y], out=Y.t[:], lhsT=hT[s_].t[:, m, s * 128:(s + 1) * 128],
                                   rhs=wd[e % RW].t[:, m, half * 512:(half + 1) * 512], start=(m == 0), stop=(m == 1))
                            if half == 0:
                                OP("act", "activation", [Y], [ye[s_]], out=ye[s_].t[:, s, 0:512], in_=Y.t[:], func=AF.Copy)
                            else:
                                OP("dve", "tensor_copy", [Y], [ye[s_]], out=ye[s_].t[:, s, 512:1024], in_=Y.t[:])
                    DMA("sp", ye[s_], [ye[s_]], [], out=ybuf_d[e * CAP:(e + 1) * CAP, :].rearrange("(s p) d -> p s d", p=128), in_=ye[s_].t[:])

                for e in range(min(RW, NEX)):
                    load_w(e)
                for e in range(min(3, NEX)):
                    load_xe(e)
                stA(0)
                for e in range(NEX):
                    if e + 1 < NEX:
                        stA(e + 1)
                    stB(e)
                    if e >= 1:
                        stC(e - 1)
                        if e - 1 + RW < NEX:
                            load_w(e - 1 + RW)
                    if e + 3 < NEX:
                        load_xe(e + 3)
                stC(NEX - 1)
                sc.run_block("p4")

        if stage >= 7:
            with ExitStack() as e5:
                gf = mk(e5, "gf", [128, D], F32)
                R5 = 5
                hh = mk(e5, "hh", [128, D], F32, R5)
                yy = [mk(e5, "yy%d" % k_, [128, D], BF16, R5) for k_ in range(2)]
                hf = mk(e5, "hf", [128, D], F32, 2)
                ob = mk(e5, "ob", [128, D], F32, 3)
                st5 = mk(e5, "st5", [128, 4], F32, 2)
                B_out = Buf("out")
                DMA("sp", gf, [], [gf], out=gf.t[:], in_=g_fin.partition_broadcast(128))

                def load5(i):
                    s_ = i % R5
                    DMA("sp", hh[s_], [], [hh[s_]], out=hh[s_].t[:], in_=h1_d[i * 128:(i + 1) * 128, :])
                    for k_ in range(2):
                        DMA("pool", yy[k_][s_], [route_i], [yy[k_][s_]], name="indirect_dma_start", out=yy[k_][s_].t[:, :], out_offset=None,
                            in_=ybuf_d[:, :], in_offset=bass.IndirectOffsetOnAxis(ap=route_i.t[:, i, k_:k_ + 1], axis=0))

                NT5 = NTILE
                for i in range(min(R5 - 1, NT5)):
                    load5(i)
                for i in range(NT5):
                    if i + R5 - 1 < NT5:
                        load5(i + R5 - 1)
                    s_, r2 = i % R5, i % 2
                    OP("dve", "scalar_tensor_tensor", [yy[0][s_], route_g, hh[s_]], [hf[r2]], out=hf[r2].t[:], in0=yy[0][s_].t[:],
                       scalar=route_g.t[:, i, 0:1], in1=hh[s_].t[:], op0=ALU.mult, op1=ALU.add)
                    OP("dve", "scalar_tensor_tensor", [yy[1][s_], route_g, hf[r2]], [hf[r2]], out=hf[r2].t[:], in0=yy[1][s_].t[:],
                       scalar=route_g.t[:, i, 1:2], in1=hf[r2].t[:], op0=ALU.mult, op1=ALU.add)
                    ST = st5[r2]
                    OP("act", "activation", [hf[r2]], [ST, ob[i % 3]], out=ob[i % 3].t[:], in_=hf[r2].t[:], func=AF.Square, accum_out=ST.t[:, 0:1])
                    OP("act", "activation", [ST], [ST], out=ST.t[:, 1:2], in_=ST.t[:, 0:1], func=AF.Ln, scale=1.0 / D, bias=EPS)
                    OP("act", "activation", [ST], [ST], out=ST.t[:, 2:3], in_=ST.t[:, 1:2], func=AF.Exp, scale=-0.5)
                    OP("dve", "scalar_tensor_tensor", [hf[r2], ST, gf], [ob[i % 3]], out=ob[i % 3].t[:], in0=hf[r2].t[:], scalar=ST.t[:, 2:3], in1=gf.t[:],
                       op0=ALU.mult, op1=ALU.mult)
                    DMA("act", ob[i % 3], [ob[i % 3]], [], out=out[i * 128:(i + 1) * 128, :], in_=ob[i % 3].t[:])
                sc.run_block("p5")

        if dbg is not None and 3 <= stage <= 5:
            with ExitStack() as ed:
                dt_ = mk(ed, "dbg_t", [128, D], F32)
                for i in range(NTILE):
                    DMA("sp", dt_, [B_h1d], [dt_], out=dt_.t[:], in_=h1_d[i * 128:(i + 1) * 128, :])
                    DMA("sp", dt_, [dt_], [], out=dbg[i * 128:(i + 1) * 128, :], in_=dt_.t[:])
                if stage >= 4:
                    dr = mk(ed, "dbg_r", [128, NT * 2], F32)
                    nn = NTILE * 2
                    OP("dve", "tensor_copy", [route_i], [dr], out=dr.t[:, 0:nn], in_=route_i.t[:].rearrange("p t k -> p (t k)")[:, 0:nn])
                    DMA("sp", dr, [dr], [], out=dbg[S:S + 128, 0:nn], in_=dr.t[:, 0:nn])
                    DMA("sp", route_g, [route_g], [], out=dbg[S:S + 128, 64:64 + nn], in_=route_g.t[:].rearrange("p t k -> p (t k)")[:, 0:nn])
                sc.run_block("dbg")
    return nc


def _consts():
    ident = np.eye(128, dtype=np.float32)
    half = 16
    inv = (10000.0 ** (-np.arange(half, dtype=np.float32) / half)).astype(np.float32)
    rope = np.zeros((128, 2), np.float32)
    for g_ in range(4):
        rope[g_ * 32:g_ * 32 + 16, 0] = inv
        rope[g_ * 32 + 16:g_ * 32 + 32, 0] = inv
        rope[g_ * 32:g_ * 32 + 16, 1] = -1.0
        rope[g_ * 32 + 16:g_ * 32 + 32, 1] = 1.0
    tri = np.zeros((128, 512), np.float32)
    kk = np.arange(128)
    tri[:, 0:128] = (kk[:, None] <= kk[None, :]).astype(np.float32)
    tri[:, 128:256] = (((kk[:, None] // 64) == (kk[None, :] // 64)) & (kk[:, None] <= kk[None, :])).astype(np.float32)
    tri[:, 384:512] = (kk[:, None] < kk[None, :]).astype(np.float32)
    same = (kk[:, None] // 64) == (kk[None, :] // 64)
    gla = np.zeros((128, 256), np.float32)
    gla[:, 0:128] = np.where(same & (kk[:, None] <= kk[None, :]), -1.0 / 16.0, 0.0)
    gla[:, 128:256] = np.where(same & (kk[:, None] > kk[None, :]), -1.0 / 16.0, 0.0)
    ecap = np.tile((np.arange(32, dtype=np.float32) * CAP)[None, :], (128, 1))
    return ident, rope, tri, gla, ecap


def prepare_inputs(inp):
    f = lambda a: np.ascontiguousarray(np.asarray(a, dtype=np.float32))
    w_in = f(inp["w_in"][0])
    gq, gk, gv, glr, gog, cq, ckv, kr = np.split(w_in, np.cumsum([256, 256, 512, 16, 512, 384, 256])[:], axis=1)
    kr_sw = np.concatenate([kr[:, 16:], kr[:, :16]], axis=1)
    wuq = f(inp["mla_w_uq"][0]).reshape(384, 8, 96)
    wuq_r = wuq[:, :, 64:]
    wuq_sw = np.concatenate([wuq_r[:, :, 16:], wuq_r[:, :, :16]], axis=2)
    wuq_all = np.concatenate([wuq, wuq_sw], axis=2).reshape(384, 8 * 128)
    wukv = f(inp["mla_w_ukv"][0]).reshape(256, 8, 128)
    ident, rope, tri, gla, ecap = _consts()
    common = {
        "g_attn": f(inp["attn_norm_w"]).reshape(1, D),
        "g_ffn": f(inp["ffn_norm_w"]).reshape(1, D),
        "g_fin": f(inp["final_norm_w"]).reshape(1, D),
        "g_q": f(inp["mla_q_norm_w"]).reshape(1, 384),
        "g_kv": f(inp["mla_kv_norm_w"]).reshape(1, 256),
        "g_gla": f(inp["gla_norm_w"]).reshape(1, 128),
        "w_mla": f(np.concatenate([cq, ckv], axis=1)),
        "w_kr": f(np.concatenate([kr, kr_sw], axis=1)),
        "w_gf": f(np.concatenate([gq, gk, glr], axis=1)),
        "w_gt": f(np.concatenate([gk, gv, gog], axis=1)),
        "w_uq": f(wuq_all),
        "w_ukn": f(wukv[:, :, :64].reshape(256, 512)),
        "w_uv": f(wukv[:, :, 64:].reshape(256, 512)),
        "gate_up": f(np.concatenate([inp["gla_gate_up"][0], np.asarray(inp["gla_gate_bias"][0]).reshape(1, 256)], axis=0)),
        "w_out": f(inp["w_out"][0]),
        "w_rt": f(np.concatenate([inp["router_group_w"][0], inp["router_expert_w"][0]], axis=1)),
        "b_rt": f(np.concatenate([inp["router_group_b"][0], inp["router_expert_b"][0]]).reshape(1, 36)),
        "w_eg": f(inp["expert_w_gate"][0]),
        "w_eu": f(inp["expert_w_up"][0]),
        "w_ed": f(inp["expert_w_down"][0]),
        "c_ident": ident,
        "c_rope": rope,
        "c_tri": tri,
        "c_gla": gla,
        "c_ecap": ecap,
    }
    xs_ = np.asarray(inp["x"], dtype=np.float32)
    ps_ = np.asarray(inp["positions"], dtype=np.int32)
    maps = []
    for c in range(8):
        m = dict(common)
        m["x"] = np.ascontiguousarray(xs_[c])
        m["pos"] = np.ascontiguousarray(ps_[c].reshape(1, S))
        maps.append(m)
    return maps


def kernel(**inputs):
    nc = build_program()
    maps = prepare_inputs(inputs)
    res = run_bass_kernel_spmd(nc, maps, core_ids=list(range(8)))
    return np.stack([np.asarray(r["out"]).reshape(S, D) for r in res.results], axis=0).astype(np.float32)
```

```python
import numpy as np
import ml_dtypes
import concourse.bass as bass
import concourse.mybir as mybir
from concourse.bass_utils import run_bass_kernel_spmd

F32 = mybir.dt.float32
BF16 = mybir.dt.bfloat16
I32 = mybir.dt.int32
AF = mybir.ActivationFunctionType
ALU = mybir.AluOpType
AX = mybir.AxisListType

S = 4096
NT = 32
D = 1024
KC = 8
EPS = 1e-6
NE = 32
CAP = 384
NSLOT = NE * CAP
TWO_PI = 2.0 * np.pi


class Buf:
    def __init__(self, name):
        self.name = name
        self.lw = None
        self.rd = []


ENGS = ["pe", "act", "dve", "pool", "sp"]
INORDER = {"sp", "pool"}
CRIT = 0
CP_PRIO = True
PA_CFG = {'a1': [0], 'a2': [1], 'a3': [1], 'a4': [0], 'b': [2], 'c1': [3], 'c2': [3], 'c3': [2]}
PE_SLOW = 1.05
SLAT = 0.2
XLAT = 0.4


def _free(ap):
    n = 1
    for s_ in list(ap.shape)[1:]:
        n *= int(s_)
    return n


def _est(eng, name, kw):
    try:
        if name in ("dma_start", "indirect_dma_start"):
            o = kw["in_"] if kw.get("out_offset", None) is not None else kw["out"]
            nbytes = int(o.shape[0]) * _free(o) * (2 if o.dtype == BF16 else 4)
            occ = 0.15 if eng in ("sp", "act") else 1.2
            return occ, 2.5 + nbytes / 150e3
        if name == "matmul":
            n = _free(kw["rhs"])
            f = 4 if kw["rhs"].dtype == F32 else 1
            d = max(64, n) * f / 2400.0 * PE_SLOW
            return d, d + 0.1
        if name == "transpose":
            f = 2 if kw["in_"].dtype == F32 else 1
            d = 128 * f / 2400.0 * PE_SLOW
            return d, d + 0.1
        if name == "activation":
            d = (_free(kw["in_"]) + 230) / 1400.0 + (0.1 if "accum_out" in kw else 0.0)
            return d, d
        n = _free(kw.get("out", kw.get("ap")))
        if name == "reciprocal":
            n *= 7
        if eng == "pool":
            d = 0.9 + n * 1.2 / 960.0
        else:
            d = 0.14 + n / 960.0
        return d, d
    except Exception:
        return 0.3, 0.3


class Sched:
    def __init__(self, nc, eng_sems, dma_sems):
        self.nc = nc
        self.eng_sems = eng_sems
        self.dma_pool = [(h, 0) for h in dma_sems]
        self.dma_used = []
        self.dma_map = {}
        self.dma_q = {}
        self.cnt = {e: 0 for e in ENGS}
        self.dma_cnt = {}
        self.dma_last = {}
        self.waited = {e: {} for e in ENGS}
        self.nodes = []
        self.base = 0
        self.tok = {}
        self.reorder = True

    def sem(self, key):
        if key[0] == "E":
            return self.eng_sems[key[1]]
        return self._sem_of[key[1]]

    def _node(self, eng, name, kw, reads, writes, dma_key=None):
        nid = self.base + len(self.nodes)
        deps = set()

        def add(d):
            if d is None or d < self.base:
                return
            deps.add(d)
            dk = self.nodes[d - self.base]["dma_key"]
            if dk is not None:
                deps.add(self.dma_last[dk])

        for b_ in reads:
            add(b_.lw)
        for b_ in writes:
            add(b_.lw)
            for r_ in b_.rd:
                add(r_)
        deps = {d for d in deps if d >= self.base}
        occ, lat = _est(eng, name, kw)
        self.nodes.append({"id": nid, "eng": eng, "name": name, "kw": kw, "deps": deps, "occ": occ, "lat": lat,
                           "dma_key": dma_key})
        for b_ in writes:
            b_.lw = nid
            b_.rd = []
        for b_ in reads:
            if b_.lw != nid:
                b_.rd.append(nid)
        return nid

    def op(self, eng, name, reads=(), writes=(), **kw):
        self._node(eng, name, kw, reads, writes)

    def dma(self, eng, sb, reads=(), writes=(), name="dma_start", **kw):
        bn = sb.name
        if bn not in self.dma_map:
            if eng == "sp" and self.dma_used:
                h, c0 = self.dma_used.pop()
            else:
                h, c0 = self.dma_pool.pop()
            self.dma_map[bn] = h
            self.dma_cnt[("D", bn)] = c0
            self.dma_q[bn] = eng
        assert self.dma_q[bn] == eng, "all DMAs touching one SBUF buffer must use one queue: " + bn
        k = ("D", bn)
        nid = self._node(eng, name, kw, reads, writes, dma_key=k)
        self.dma_last[k] = nid

    def _schedule(self):
        import heapq
        nodes = self.nodes
        n = len(nodes)
        succ = [[] for _ in range(n)]
        indeg = [0] * n
        for nd in nodes:
            i = nd["id"] - self.base
            for d in nd["deps"]:
                succ[d - self.base].append(i)
                indeg[i] += 1
        prio = list(range(n))
        if CP_PRIO:
            cp = [0.0] * n
            for i in range(n - 1, -1, -1):
                m = 0.0
                for s_ in succ[i]:
                    if cp[s_] > m:
                        m = cp[s_]
                cp[i] = m + nodes[i]["lat"]
            order_idx = sorted(range(n), key=lambda j: (-cp[j], j))
            for r_, j in enumerate(order_idx):
                prio[j] = r_
        fin = [0.0] * n
        why = [None] * n
        rdep = [None] * n
        last_on = {e: None for e in ENGS}
        ready_at = [0.0] * n
        free_at = {e: 0.0 for e in ENGS}
        waiting = {e: [] for e in ENGS}
        avail = {e: [] for e in ENGS}
        inorder_q = {e: [i for i in range(n) if nodes[i]["eng"] == e] for e in INORDER}
        inorder_pos = {e: 0 for e in INORDER}
        order = {e: [] for e in ENGS}
        for i in range(n):
            if indeg[i] == 0 and nodes[i]["eng"] not in INORDER:
                heapq.heappush(waiting[nodes[i]["eng"]], (0.0, i))
        done = 0
        while done < n:
            best = None
            for e in ENGS:
                if e in INORDER:
                    p = inorder_pos[e]
                    if p >= len(inorder_q[e]):
                        continue
                    i = inorder_q[e][p]
                    if indeg[i] > 0:
                        continue
                    stt = max(free_at[e], ready_at[i])
                    cand = (stt, i, e)
                else:
                    w, av = waiting[e], avail[e]
                    while w and w[0][0] <= free_at[e]:
                        j_ = heapq.heappop(w)[1]
                        heapq.heappush(av, (prio[j_], j_))
                    if av:
                        cand = (free_at[e], av[0][1], e)
                    elif w:
                        cand = (w[0][0], w[0][1], e)
                    else:
                        continue
                if best is None or cand < best:
                    best = cand
            assert best is not None, "scheduler deadlock"
            stt, i, e = best
            if e in INORDER:
                inorder_pos[e] += 1
            else:
                if avail[e] and avail[e][0][1] == i:
                    heapq.heappop(avail[e])
                else:
                    heapq.heappop(waiting[e])
            nd = nodes[i]
            why[i] = ("dep", rdep[i]) if (rdep[i] is not None and ready_at[i] >= free_at[e] - 1e-9) else ("eng", last_on[e])
            last_on[e] = i
            free_at[e] = stt + nd["occ"]
            fin[i] = stt + nd["lat"]
            order[e].append(i)
            done += 1
            for s_ in succ[i]:
                indeg[s_] -= 1
                lat = XLAT if nodes[s_]["eng"] != e else (0.0 if e == "pe" else SLAT)
                if fin[i] + lat > ready_at[s_]:
                    ready_at[s_] = fin[i] + lat
                    rdep[s_] = i
                if indeg[s_] == 0 and nodes[s_]["eng"] not in INORDER:
                    heapq.heappush(waiting[nodes[s_]["eng"]], (ready_at[s_], s_))
        self.est_us = max(fin) if n else 0.0
        if CRIT and n:
            i = max(range(n), key=lambda j: fin[j])
            path = []
            while i is not None and len(path) < 100000:
                path.append(i)
                i = why[i][1] if why[i] else None
            agg = {}
            for i in path:
                nd = nodes[i]
                o = nd["kw"].get("out", nd["kw"].get("ap"))
                key = (nd["eng"], nd["name"], str(getattr(getattr(o, "tensor", None), "name", "?")), why[i][0] if why[i] else "-")
                a_ = agg.setdefault(key, [0, 0.0])
                a_[0] += 1
                a_[1] += nd["lat"]
            print("  critical path (%d nodes):" % len(path))
            for k_, v_ in sorted(agg.items(), key=lambda kv: -kv[1][1])[:CRIT]:
                print("    %-60s n=%5d  t=%7.1f" % (k_, v_[0], v_[1]))
        self.est_busy = {e: sum(nodes[i]["occ"] for i in order[e]) for e in ENGS}
        return order

    def run_block(self, name=None):
        nodes = self.nodes
        n = len(nodes)
        if self.reorder:
            order = self._schedule()
        else:
            order = {e: [i for i in range(n) if nodes[i]["eng"] == e] for e in ENGS}
        tok = {}
        for e in ENGS:
            for i in order[e]:
                nd = nodes[i]
                if nd["dma_key"] is not None:
                    k = nd["dma_key"]
                    self.dma_cnt[k] = self.dma_cnt.get(k, 0) + 16
                    tok[i] = (k, self.dma_cnt[k], 16)
                else:
                    self.cnt[e] += 1
                    tok[i] = (("E", e), self.cnt[e], 1)
        prog = {e: [] for e in ENGS}
        for e in ENGS:
            wd = self.waited[e]
            for i in order[e]:
                nd = nodes[i]
                need = {}
                for d in nd["deps"]:
                    k, v, _ = tok[d - self.base]
                    if k == ("E", "pe") and e == "pe":
                        continue
                    if need.get(k, 0) < v:
                        need[k] = v
                for k, v in need.items():
                    if wd.get(k, 0) < v:
                        wd[k] = v
                        prog[e].append(("wait", k, v))
                prog[e].append(("inst", (nd["name"], nd["kw"]), tok[i][0], tok[i][2]))
        wd = self.waited["sp"]
        for k, v in self.dma_cnt.items():
            if wd.get(k, 0) < v:
                wd[k] = v
                prog["sp"].append(("wait", k, v))
        for e in ENGS:
            k, v = ("E", e), self.cnt[e]
            if e != "sp" and v > 0 and wd.get(k, 0) < v:
                wd[k] = v
                prog["sp"].append(("wait", k, v))
        self.base += n
        self.nodes = []
        nc = self.nc
        self._sem_of = dict(self.dma_map)
        clear_list = []
        for bn, h in self.dma_map.items():
            c_ = self.dma_cnt.pop(("D", bn))
            if self.dma_q[bn] == "sp":
                self.dma_used.append((h, c_))
            for e in ENGS:
                self.waited[e].pop(("D", bn), None)
        self.dma_map = {}
        self.dma_q = {}
        self.dma_last = {}

        def replay(lst, clear=()):
            def body(engine):
                for o in lst:
                    if o[0] == "wait":
                        engine.wait_ge(self.sem(o[1]), o[2])
                    else:
                        getattr(engine, o[1][0])(**o[1][1]).then_inc(self.sem(o[2]), o[3])
                for h in clear:
                    engine.sem_clear(h)
            return body

        with nc.Block() as block:
            block.tensor(replay(prog["pe"]))
            block.scalar(replay(prog["act"]))
            block.vector(replay(prog["dve"]))
            block.gpsimd(replay(prog["pool"]))
            block.sync(replay(prog["sp"], clear_list))
        if name:
            print("[sched] block %s: %d ops, est %.0f us, busy %s" % (name, n, getattr(self, "est_us", 0.0),
                  {e: int(v) for e, v in getattr(self, "est_busy", {}).items()}))


def build_program(debug=None):
    nc = bass.Bass("TRN2", target_bir_lowering=False)

    def din(name, shape, dt=F32):
        return nc.dram_tensor(name, list(shape), dt, kind="ExternalInput").ap()

    x = din("x", [S, D])
    pos = din("pos", [1, S], I32)
    g_attn = din("g_attn", [1, D])
    g_ffn = din("g_ffn", [1, D])
    g_fin = din("g_fin", [1, D])
    g_q = din("g_q", [1, 384])
    g_kv = din("g_kv", [1, 256])
    g_gla = din("g_gla", [1, 128])
    w_mla = din("w_mla", [D, 640])
    w_kr = din("w_kr", [D, 64])
    w_gf = din("w_gf", [D, 528])
    w_gt = din("w_gt", [D, 1280])
    w_uq = din("w_uq", [384, 8 * 128])
    w_ukn = din("w_ukn", [256, 512])
    w_uv = din("w_uv", [256, 512])
    gate_up = din("gate_up", [17, 256])
    w_out = din("w_out", [D, D])
    w_rt = din("w_rt", [D, 36])
    b_rt = din("b_rt", [1, 36])
    w_eg = din("w_eg", [NE, D, 256])
    w_eu = din("w_eu", [NE, D, 256])
    w_ed = din("w_ed", [NE, 256, D])
    c_ident = din("c_ident", [128, 128])
    c_rope = din("c_rope", [128, 2])
    c_tri = din("c_tri", [128, 4 * 128])
    c_gla = din("c_gla", [128, 256])
    c_ecap = din("c_ecap", [128, 32])
    out = nc.dram_tensor("out", [S, D], F32, kind="ExternalOutput").ap()
    dbg = None
    if debug:
        dbg = nc.dram_tensor("dbg", list(debug[:2]), F32, kind="ExternalOutput").ap()

    h1_d = nc.dram_tensor("h1_d", [S, D], F32).ap()
    xbuf_d = nc.dram_tensor("xbuf_d", [NSLOT, D], BF16).ap()
    ybuf_d = nc.dram_tensor("ybuf_d", [NSLOT, D], BF16).ap()
    wbf_d = nc.dram_tensor("wbf_d", [NE, 128, 6144], BF16).ap()

    from contextlib import ExitStack
    es = ExitStack()
    with es:
        eng_sems = {e: es.enter_context(nc.semaphore("sem_" + e)) for e in ENGS}
        dma_sems = [es.enter_context(nc.semaphore("dsem%d" % i)) for i in range(56)]
        sc = Sched(nc, eng_sems, dma_sems)

        def sb(stack, name, shape, dt):
            return stack.enter_context(nc.sbuf_tensor(name, list(shape), dt))

        def ps(stack, name, shape, dt):
            return stack.enter_context(nc.psum_tensor(name, list(shape), dt))

        ident_f = sb(es, "ident_f", [128, 128], F32)
        ident_b = sb(es, "ident_b", [128, 128], BF16)
        ymT = sb(es, "ymT", [128, NT, 512], BF16)
        B_ident = Buf("ident")
        B_ymT = Buf("ymT")
        sc.dma("sp", B_ident, writes=[B_ident], out=ident_f[:], in_=c_ident)
        sc.op("dve", "tensor_copy", out=ident_b[:], in_=ident_f[:], reads=[B_ident], writes=[B_ident])

        skip_front = bool(debug and len(debug) > 5 and debug[5])
        if skip_front:
            sc.op("dve", "memset", ap=ymT[:], constant=0.0, writes=[B_ymT])
        class TB:
            def __init__(self, t, name):
                self.t = t
                self.b = Buf(name)

        def mk(stack, name, shape, dt, n=None, psum=False):
            f = ps if psum else sb
            name = "m_" + name
            if n is None:
                return TB(f(stack, name, shape, dt), name)
            return [TB(f(stack, "%s%d" % (name, i), shape, dt), "%s%d" % (name, i)) for i in range(n)]

        def bl(lst):
            return [getattr(o, "b", o) for o in lst]

        def OP(eng, name, reads, writes, **kw):
            sc.op(eng, name, reads=bl(reads), writes=bl(writes), **kw)

        def DMA(eng, sbuf, reads, writes, name="dma_start", **kw):
            sc.dma(eng, getattr(sbuf, "b", sbuf), reads=bl(reads), writes=bl(writes), name=name, **kw)

        zero_t = sb(es, "zero_t", [128, D], BF16)
        B_zero = Buf("zero_t")
        es12 = ExitStack()
        with es12:
          if not skip_front:
              ctab = sb(es12, "ctab", [128, S], BF16)
              stab = sb(es12, "stab", [128, S], BF16)
              cnT = sb(es12, "cnT", [128, 5, S], BF16)
              krT = sb(es12, "krT", [128, S], BF16)
              B_tab = Buf("tab")
              wuq_sb = sb(es12, "wuq_sb", [128, 3, 1024], BF16)
              wukn_sb = sb(es12, "wukn_sb", [128, 2, 512], BF16)
              wuv_sb = sb(es12, "wuv_sb", [128, 2, 512], BF16)
              B_w2 = Buf("w2")
              B_cnT = Buf("cnT")
              B_krT = Buf("krT")

              e0 = ExitStack()
              if True:
                  ropec = sb(e0, "ropec", [128, 2], F32)
                  posi = sb(e0, "posi", [128, 1024], I32)
                  ang = sb(e0, "ang", [128, 1024], F32)
                  kf = sb(e0, "kf", [128, 1024], F32)
                  ki = sb(e0, "ki", [128, 1024], I32)
                  r1 = sb(e0, "r1", [128, 1024], F32)
                  tabt = [sb(e0, "tabt%d" % i, [128, 1024], BF16) for i in range(2)]
                  B_ropec, B_posi, B_ang, B_kf, B_ki, B_r1 = (Buf(n) for n in ["ropec", "posi", "ang", "kf", "ki", "r1"])
                  B_tabt = [Buf("tabt0"), Buf("tabt1")]
                  sc.dma("sp", B_ropec, writes=[B_ropec], out=ropec[:], in_=c_rope)
                  for g_ in range(4):
                      sc.dma("sp", B_posi, writes=[B_posi], out=posi[g_ * 32:(g_ + 1) * 32, :],
                             in_=pos[:, g_ * 1024:(g_ + 1) * 1024].partition_broadcast(32))
                  sc.op("dve", "tensor_copy", out=ang[:], in_=posi[:], reads=[B_posi], writes=[B_ang])
                  sc.op("dve", "tensor_scalar", out=ang[:], in0=ang[:], scalar1=ropec[:, 0:1], scalar2=None,
                        op0=ALU.mult, reads=[B_ang, B_ropec], writes=[B_ang])
                  for which in range(2):
                      shift = 0.0 if which == 0 else np.pi / 2.0
                      sc.op("dve", "tensor_scalar", out=kf[:], in0=ang[:], scalar1=shift, scalar2=1.0 / TWO_PI, op0=ALU.add, op1=ALU.mult,
                            reads=[B_ang], writes=[B_kf])
                      sc.op("dve", "tensor_copy", out=ki[:], in_=kf[:], reads=[B_kf], writes=[B_ki])
                      sc.op("dve", "tensor_copy", out=kf[:], in_=ki[:], reads=[B_ki], writes=[B_kf])
                      sc.op("dve", "scalar_tensor_tensor", out=r1[:], in0=kf[:], scalar=-TWO_PI, in1=ang[:],
                            op0=ALU.mult, op1=ALU.add, reads=[B_kf, B_ang], writes=[B_r1])
                      if which == 1:
                          sc.op("dve", "tensor_scalar", out=r1[:], in0=r1[:], scalar1=np.pi / 2.0, scalar2=None,
                                op0=ALU.add, reads=[B_r1], writes=[B_r1])
                      sc.op("dve", "tensor_scalar", out=kf[:], in0=r1[:], scalar1=np.pi, scalar2=-TWO_PI,
                            op0=ALU.is_gt, op1=ALU.mult, reads=[B_r1], writes=[B_kf])
                      sc.op("dve", "tensor_tensor", out=r1[:], in0=r1[:], in1=kf[:], op=ALU.add, reads=[B_r1, B_kf], writes=[B_r1])
                      sc.op("dve", "tensor_scalar", out=kf[:], in0=r1[:], scalar1=-np.pi, scalar2=TWO_PI,
                            op0=ALU.is_lt, op1=ALU.mult, reads=[B_r1], writes=[B_kf])
                      sc.op("dve", "tensor_tensor", out=r1[:], in0=r1[:], in1=kf[:], op=ALU.add, reads=[B_r1, B_kf], writes=[B_r1])
                      sc.op("dve", "tensor_scalar", out=r1[:], in0=r1[:], scalar1=3.1415925, scalar2=-3.1415925,
                            op0=ALU.min, op1=ALU.max, reads=[B_r1], writes=[B_r1])
                      if which == 0:
                          sc.op("act", "activation", out=tabt[0][:], in_=r1[:], func=AF.Sin, scale=ropec[:, 1:2],
                                reads=[B_r1, B_ropec], writes=[B_tabt[0]])
                      else:
                          sc.op("act", "activation", out=tabt[1][:], in_=r1[:], func=AF.Sin, reads=[B_r1], writes=[B_tabt[1]])
                      dst = stab if which == 0 else ctab
                      for g_ in range(4):
                          sc.dma("act", B_tabt[which], reads=[B_tabt[which]], writes=[B_tab], out=dst[64:96, g_ * 1024:(g_ + 1) * 1024],
                                 in_=tabt[which][g_ * 32:(g_ + 1) * 32, :])

              with ExitStack() as e1:
                  NXT = 4
                  xt = [sb(e1, "xt%d" % i, [128, D], F32) for i in range(NXT)]
                  B_xt = [Buf("xt%d" % i) for i in range(NXT)]
                  g1 = sb(e1, "g1", [128, D], F32)
                  gqk = sb(e1, "gqk", [128, 640], F32)
                  B_g = Buf("g1")
                  wm_sb = sb(e1, "wm_sb", [128, KC, 640], BF16)
                  wk_sb = sb(e1, "wk_sb", [128, KC, 64], BF16)
                  B_wm = Buf("wm_sb")
                  B_wk = Buf("wk_sb")
                  RS1 = 3
                  st = [sb(e1, "st%d" % i, [128, 8], F32) for i in range(RS1)]
                  B_st = [Buf("st%d" % i) for i in range(RS1)]
                  xs = [sb(e1, "xs%d" % i, [128, D], BF16) for i in range(RS1)]
                  B_xs = [Buf("xs%d" % i) for i in range(RS1)]
                  xnT = [sb(e1, "xnT%d" % i, [128, KC, 512], BF16) for i in range(2)]
                  B_xnT = [Buf("xnT%d" % i) for i in range(2)]
                  cn = [sb(e1, "cn%d" % i, [128, 640], BF16) for i in range(RS1)]
                  B_cn = [Buf("cn%d" % i) for i in range(RS1)]
                  kt1 = sb(e1, "kt1", [128, 512], F32)
                  kt2 = sb(e1, "kt2", [128, 512], F32)
                  B_kt = Buf("kt")
                  p_tp = ps(e1, "p_tp", [128, KC, 128], BF16)
                  p_mm = [ps(e1, "p_mm%d" % i, [128, 1024], F32) for i in range(2)]
                  p_tp2 = ps(e1, "p_tp2", [128, 5, 128], BF16)
                  p_kr = ps(e1, "p_kr", [128, 2, 512], F32)
                  B_ptp, B_ptp2, B_pkr = Buf("p_tp"), Buf("p_tp2"), Buf("p_kr")
                  B_pmm = [Buf("p_mm0"), Buf("p_mm1")]

                  sc.dma("sp", B_g, writes=[B_g], out=g1[:], in_=g_attn.partition_broadcast(128))
                  sc.dma("sp", B_g, writes=[B_g], out=gqk[:, 0:384], in_=g_q.partition_broadcast(128))
                  sc.dma("sp", B_g, writes=[B_g], out=gqk[:, 384:640], in_=g_kv.partition_broadcast(128))
                  for c in range(KC):
                      sc.dma("pool", B_wm, writes=[B_wm], out=wm_sb[:, c, :], in_=w_mla[c * 128:(c + 1) * 128, :])
                      sc.dma("pool", B_wk, writes=[B_wk], out=wk_sb[:, c, :], in_=w_kr[c * 128:(c + 1) * 128, :])

                  def load_w2():
                      for c in range(3):
                          sc.dma("pool", B_w2, reads=[B_cnT], writes=[B_w2], out=wuq_sb[:, c, :], in_=w_uq[c * 128:(c + 1) * 128, :])
                      for c in range(2):
                          sc.dma("pool", B_w2, reads=[B_cnT], writes=[B_w2], out=wukn_sb[:, c, :], in_=w_ukn[c * 128:(c + 1) * 128, :])
                          sc.dma("pool", B_w2, reads=[B_cnT], writes=[B_w2], out=wuv_sb[:, c, :], in_=w_uv[c * 128:(c + 1) * 128, :])

                  def load_x(i):
                      s_ = i % NXT
                      sc.dma("sp", B_xt[s_], writes=[B_xt[s_]], out=xt[s_][:], in_=x[i * 128:(i + 1) * 128, :])

                  load_x(0)
                  load_x(1)
                  load_x(2)
                  for i in range(NT):
                      if i + 3 < NT:
                          load_x(i + 3)
                      if i == 8:
                          load_w2()
                      s3, s2, sp_ = i % NXT, i % RS1, i % 2
                      blk, tb = i // 4, i % 4
                      xb = blk % 2
                      tcols = slice(tb * 128, (tb + 1) * 128)
                      gcols = slice(i * 128, (i + 1) * 128)
                      sc.op("act", "activation", out=xs[s2][:], in_=xt[s3][:], func=AF.Square, accum_out=st[s2][:, 0:1],
                            reads=[B_xt[s3]], writes=[B_st[s2], B_xs[s2]])
                      sc.op("act", "activation", out=st[s2][:, 1:2], in_=st[s2][:, 0:1], func=AF.Ln, scale=1.0 / D, bias=EPS,
                            reads=[B_st[s2]], writes=[B_st[s2]])
                      sc.op("act", "activation", out=st[s2][:, 2:3], in_=st[s2][:, 1:2], func=AF.Exp, scale=-0.5,
                            reads=[B_st[s2]], writes=[B_st[s2]])
                      sc.op("dve", "scalar_tensor_tensor", out=xs[s2][:], in0=xt[s3][:], scalar=st[s2][:, 2:3], in1=g1[:],
                                                                    op0=ALU.mult, op1=ALU.mult,
                            reads=[B_xt[s3], B_st[s2], B_g], writes=[B_xs[s2]])
                      for c in range(KC):
                          sc.op("pe", "transpose", out=p_tp[:, c, :], in_=xs[s2][:, c * 128:(c + 1) * 128], identity=ident_b[:],
                                reads=[B_xs[s2], B_ident], writes=[B_ptp])
                      sc.op("act", "activation", out=xnT[xb][:, :, tcols], in_=p_tp[:], func=AF.Copy,
                            reads=[B_ptp], writes=[B_xnT[xb]])
                      for c in range(KC):
                          sc.op("pe", "matmul", out=p_mm[sp_][:, 0:384], lhsT=xnT[xb][:, c, tcols], rhs=wm_sb[:, c, 0:384],
                                                              start=(c == 0), stop=(c == KC - 1),
                                reads=[B_xnT[xb], B_wm], writes=[B_pmm[sp_]])
                      for c in range(KC):
                          sc.op("pe", "matmul", out=p_mm[sp_][:, 512:768], lhsT=xnT[xb][:, c, tcols], rhs=wm_sb[:, c, 384:640],
                                                              start=(c == 0), stop=(c == KC - 1),
                                reads=[B_xnT[xb], B_wm], writes=[B_pmm[sp_]])
                      sc.op("act", "activation", out=cn[s2][:, 0:384], in_=p_mm[sp_][:, 0:384], func=AF.Square, accum_out=st[s2][:, 3:4],
                            reads=[B_pmm[sp_]], writes=[B_st[s2], B_cn[s2]])
                      sc.op("act", "activation", out=cn[s2][:, 384:640], in_=p_mm[sp_][:, 512:768], func=AF.Square, accum_out=st[s2][:, 4:5],
                            reads=[B_pmm[sp_]], writes=[B_st[s2], B_cn[s2]])
                      sc.op("act", "activation", out=st[s2][:, 5:6], in_=st[s2][:, 3:4], func=AF.Ln, scale=1.0 / 384, bias=EPS,
                            reads=[B_st[s2]], writes=[B_st[s2]])
                      sc.op("act", "activation", out=st[s2][:, 6:7], in_=st[s2][:, 4:5], func=AF.Ln, scale=1.0 / 256, bias=EPS,
                            reads=[B_st[s2]], writes=[B_st[s2]])
                      sc.op("act", "activation", out=st[s2][:, 5:7], in_=st[s2][:, 5:7], func=AF.Exp, scale=-0.5,
                            reads=[B_st[s2]], writes=[B_st[s2]])
                      sc.op("dve", "scalar_tensor_tensor", out=cn[s2][:, 0:384], in0=p_mm[sp_][:, 0:384], scalar=st[s2][:, 5:6],
                                                                    in1=gqk[:, 0:384], op0=ALU.mult, op1=ALU.mult,
                            reads=[B_pmm[sp_], B_st[s2], B_g], writes=[B_cn[s2]])
                      sc.op("dve", "scalar_tensor_tensor", out=cn[s2][:, 384:640], in0=p_mm[sp_][:, 512:768], scalar=st[s2][:, 6:7],
                                                                    in1=gqk[:, 384:640], op0=ALU.mult, op1=ALU.mult,
                            reads=[B_pmm[sp_], B_st[s2], B_g], writes=[B_cn[s2]])
                      for c in range(5):
                          sc.op("pe", "transpose", out=p_tp2[:, c, :], in_=cn[s2][:, c * 128:(c + 1) * 128], identity=ident_b[:],
                                reads=[B_cn[s2], B_ident], writes=[B_ptp2])
                      sc.op("act", "activation", out=cnT[:, :, gcols], in_=p_tp2[:], func=AF.Copy,
                            reads=[B_ptp2], writes=[B_cnT])
                      if tb == 3:
                          bcols = slice(blk * 512, (blk + 1) * 512)
                          for j in range(2):
                              for c in range(KC):
                                  sc.op("pe", "matmul", out=p_kr[64:96, j, :], lhsT=wk_sb[:, c, j * 32:(j + 1) * 32],
                                                                           rhs=xnT[xb][:, c, :], start=(c == 0), stop=(c == KC - 1),
                                        reads=[B_xnT[xb], B_wk], writes=[B_pkr])
                          P = slice(64, 96)
                          sc.op("dve", "tensor_tensor", out=kt1[P, :], in0=p_kr[P, 0, :], in1=ctab[P, bcols], op=ALU.mult,
                                reads=[B_pkr, B_tab], writes=[B_kt])
                          sc.op("dve", "tensor_tensor", out=kt2[P, :], in0=p_kr[P, 1, :], in1=stab[P, bcols], op=ALU.mult,
                                reads=[B_pkr, B_tab, B_kt], writes=[B_kt])
                          sc.op("dve", "tensor_tensor", out=krT[P, bcols], in0=kt1[P, :], in1=kt2[P, :], op=ALU.add,
                                reads=[B_kt], writes=[B_krT])
                  sc.run_block("p1")
              e0.close()

              with ExitStack() as e2:
                  SCALE = float(96 ** -0.5)
                  trif = sb(e2, "trif", [128, 128], F32)
                  trib = sb(e2, "trib", [128, 128], BF16)
                  ones_b = sb(e2, "ones_b", [128, 64], BF16)
                  B_c2 = Buf("c2")
                  QT = [sb(e2, "QT%d" % i, [128, S], BF16) for i in range(2)]
                  KT = [sb(e2, "KT%d" % i, [128, S], BF16) for i in range(2)]
                  VV = [sb(e2, "VV%d" % i, [128, NT, 65], BF16) for i in range(2)]
                  B_QT = [Buf("QT0"), Buf("QT1")]
                  B_KT = [Buf("KT0"), Buf("KT1")]
                  B_VV = [Buf("VV0"), Buf("VV1")]
                  NPT = 24
                  PT = [sb(e2, "PT%d" % i, [128, 512], BF16) for i in range(NPT)]
                  B_PT = [Buf("PT%d" % i) for i in range(NPT)]
                  qt1 = sb(e2, "qt1", [128, 512], F32)
                  qt2 = sb(e2, "qt2", [128, 512], F32)
                  B_qt = Buf("qt")
                  rr = [sb(e2, "rr%d" % i, [128, 8], F32) for i in range(2)]
                  B_rr = [Buf("rr0"), Buf("rr1")]
                  NST = 4
                  psT = [ps(e2, "psT%d" % i, [128, 512], F32) for i in range(NST)]
                  B_psT = [Buf("psT%d" % i) for i in range(NST)]
                  poT = [ps(e2, "poT%d" % i, [128, 512], F32) for i in range(2)]
                  B_poT = [Buf("poT0"), Buf("poT1")]
                  NBB = 2
                  pbb = [ps(e2, "pbb%d" % i, [128, 512], F32) for i in range(NBB)]
                  B_pbb = [Buf("pbb%d" % i) for i in range(NBB)]

                  sc.dma("sp", B_c2, writes=[B_c2], out=trif[:], in_=c_tri[:, 0:128])
                  sc.op("dve", "tensor_copy", out=trib[:], in_=trif[:], reads=[B_c2], writes=[B_c2])
                  sc.op("dve", "memset", ap=ones_b[:], constant=1.0, writes=[B_c2])
                  for i in range(2):
                      sc.op("dve", "memset", ap=VV[i][:, :, 64:65], constant=1.0, writes=[B_VV[i]])

                  sc.op("dve", "memset", ap=zero_t[:], constant=0.0, writes=[B_zero])
                  for r_ in range(NSLOT // 128):
                      sc.dma("sp", B_zero, reads=[B_zero], out=xbuf_d[r_ * 128:(r_ + 1) * 128, :], in_=zero_t[:])
                  st2 = {"bb": 0, "ps": 0, "pt": 0, "nq": 0}
                  P = slice(64, 96)

                  def build_jobs(h):
                      hb = h % 2
                      jobs = []
                      for b in range(8):
                          bc = slice(b * 512, (b + 1) * 512)

                          def jq(b=b, bc=bc):
                              k_ = st2["bb"] % NBB; st2["bb"] += 1
                              for c in range(3):
                                  sc.op("pe", "matmul", out=pbb[k_][0:96, :], lhsT=wuq_sb[:, c, h * 128:h * 128 + 96], rhs=cnT[:, c, bc],
                                        start=(c == 0), stop=(c == 2), reads=[B_w2, B_cnT], writes=[B_pbb[k_]])
                              sc.op("dve", "tensor_copy", out=QT[hb][0:64, bc], in_=pbb[k_][0:64, :],
                                    reads=[B_pbb[k_]], writes=[B_QT[hb]])
                              sc.op("dve", "tensor_tensor", out=qt1[P, :], in0=pbb[k_][P, :], in1=ctab[P, bc], op=ALU.mult,
                                    reads=[B_pbb[k_], B_tab], writes=[B_qt])
                              k2 = st2["bb"] % NBB; st2["bb"] += 1
                              for c in range(3):
                                  sc.op("pe", "matmul", out=pbb[k2][P, :], lhsT=wuq_sb[:, c, h * 128 + 96:h * 128 + 128], rhs=cnT[:, c, bc],
                                        start=(c == 0), stop=(c == 2), reads=[B_w2, B_cnT], writes=[B_pbb[k2]])
                              sc.op("dve", "tensor_tensor", out=qt2[P, :], in0=pbb[k2][P, :], in1=stab[P, bc], op=ALU.mult,
                                    reads=[B_pbb[k2], B_tab, B_qt], writes=[B_qt])
                              sc.op("dve", "tensor_tensor", out=QT[hb][P, bc], in0=qt1[P, :], in1=qt2[P, :], op=ALU.add,
                                    reads=[B_qt], writes=[B_QT[hb]])

                          def jk(b=b, bc=bc):
                              k_ = st2["bb"] % NBB; st2["bb"] += 1
                              for c in range(2):
                                  sc.op("pe", "matmul", out=pbb[k_][0:64, :], lhsT=wukn_sb[:, c, h * 64:(h + 1) * 64], rhs=cnT[:, 3 + c, bc],
                                        start=(c == 0), stop=(c == 1), reads=[B_w2, B_cnT], writes=[B_pbb[k_]])
                              sc.op("dve", "tensor_copy", out=KT[hb][0:64, bc], in_=pbb[k_][0:64, :],
                                    reads=[B_pbb[k_]], writes=[B_KT[hb]])
                              sc.op("pool", "tensor_copy", out=KT[hb][P, bc], in_=krT[P, bc], reads=[B_krT], writes=[B_KT[hb]])

                          def jv(b=b, bc=bc):
                              k_ = st2["bb"] % NBB; st2["bb"] += 1
                              for t in range(4):
                                  tcs = slice(b * 512 + t * 128, b * 512 + (t + 1) * 128)
                                  for c in range(2):
                                      sc.op("pe", "matmul", out=pbb[k_][:, t * 64:(t + 1) * 64], lhsT=cnT[:, 3 + c, tcs],
                                            rhs=wuv_sb[:, c, h * 64:(h + 1) * 64], start=(c == 0), stop=(c == 1),
                                            reads=[B_w2, B_cnT], writes=[B_pbb[k_]])
                              sc.op("dve", "tensor_copy", out=VV[hb][:, b * 4:(b + 1) * 4, 0:64],
                                    in_=pbb[k_][:, 0:256].rearrange("p (t d) -> p t d", t=4),
                                    reads=[B_pbb[k_]], writes=[B_VV[hb]])

                          jobs += [jq, jk, jv]
                      return jobs

                  def attention(h, side_jobs):
                      hb = h % 2
                      iters = []
                      for qb in range(8):
                          for kt in range(4 * qb + 4):
                              iters.append((qb, kt))
                      nside = len(side_jobs)
                      every = max(1, len(iters) // (nside + 1)) if nside else 0
                      for n, (qb, kt) in enumerate(iters):
                          j = kt - 4 * qb
                          c0 = max(j, 0) * 128
                          k_ = st2["ps"] % NST; st2["ps"] += 1
                          sc.op("pe", "matmul", out=psT[k_][:, c0:512], lhsT=KT[hb][0:96, kt * 128:(kt + 1) * 128],
                                rhs=QT[hb][0:96, qb * 512 + c0:(qb + 1) * 512], start=True, stop=True,
                                reads=[B_KT[hb], B_QT[hb]], writes=[B_psT[k_]])
                          r = st2["pt"] % NPT; st2["pt"] += 1
                          sc.op("act", "activation", out=PT[r][:, c0:512], in_=psT[k_][:, c0:512], func=AF.Exp, scale=SCALE,
                                reads=[B_psT[k_]], writes=[B_PT[r]])
                          if kt >= 4 * qb:
                              sc.op("dve", "tensor_tensor", out=PT[r][:, c0:c0 + 128], in0=PT[r][:, c0:c0 + 128], in1=trib[:], op=ALU.mult,
                                    reads=[B_PT[r], B_c2], writes=[B_PT[r]])
                          pb = qb % 2
                          for t in range(c0 // 128, 4):
                              sc.op("pe", "matmul", out=poT[pb][:, t * 128:t * 128 + 65], lhsT=PT[r][:, t * 128:(t + 1) * 128], rhs=VV[hb][:, kt, 0:65],
                                    start=(kt == 0 and t == 0), stop=(kt == 4 * qb + 3 and t == 3), skip_group_check=True,
                                    reads=[B_VV[hb], B_PT[r]], writes=[B_poT[pb]])
                          if kt == 4 * qb + 3:
                              q2 = st2["nq"] % 2; st2["nq"] += 1
                              pv = poT[pb][:].rearrange("p (t c) -> p t c", t=4)
                              sc.op("dve", "reciprocal", out=rr[q2][:, 0:4], in_=pv[:, :, 64], reads=[B_poT[pb]], writes=[B_rr[q2]])
                              for t in range(4):
                                  sc.op("dve", "tensor_scalar", out=ymT[:, qb * 4 + t, h * 64:(h + 1) * 64], in0=poT[pb][:, t * 128:t * 128 + 64],
                                        scalar1=rr[q2][:, t:t + 1], scalar2=None, op0=ALU.mult, reads=[B_poT[pb], B_rr[q2]], writes=[B_ymT])
                          if nside and n % every == every - 1 and side_jobs:
                              side_jobs.pop(0)()
                      while side_jobs:
                          side_jobs.pop(0)()

                  B_wconv = Buf("wconv")

                  def conv_job(e):
                      def job():
                          gu = wbf_d[e][:, 0:4096].rearrange("p (c n) -> p c n", n=512)
                          DMA("pool", B_wconv, [], [], out=gu[:, :, 0:256], in_=w_eg[e].rearrange("(p c) n -> p c n", p=128))
                          DMA("pool", B_wconv, [], [], out=gu[:, :, 256:512], in_=w_eu[e].rearrange("(p c) n -> p c n", p=128))
                          DMA("pool", B_wconv, [], [], out=wbf_d[e][:, 4096:6144].rearrange("p (c n) -> p c n", n=1024),
                              in_=w_ed[e].rearrange("(p c) n -> p c n", p=128))
                      return job

                  NH = 8 if not (debug and len(debug) > 2) else debug[2]
                  for j in build_jobs(0):
                      j()
                  for h in range(NH):
                      side = build_jobs(h + 1) if h + 1 < NH else []
                      convs = [conv_job(e) for e in range(4 * h, 4 * h + 4)] if NH == 8 else []
                      merged = []
                      while side or convs:
                          for _ in range(6):
                              if side:
                                  merged.append(side.pop(0))
                          if convs:
                              merged.append(convs.pop(0))
                      attention(h, merged)
                  sc.run_block("p2")

        stage = debug[3] if (debug and len(debug) > 3) else 7
        route_i = mk(es, "route_i", [128, NT, 2], I32)
        route_g = mk(es, "route_g", [128, NT, 2], F32)
        B_xbuf, B_ybuf, B_h1d = Buf("xbuf"), Buf("ybuf"), Buf("h1d")

        with ExitStack() as e3:
            NTILE = NT if not (debug and len(debug) > 4) else debug[4]
            wgf = mk(e3, "wgf", [128, KC, 528], BF16)
            wgt = mk(e3, "wgt", [128, KC, 1280], BF16)
            wo = mk(e3, "wo", [128, KC, D], BF16)
            wrt = mk(e3, "wrt", [128, KC, 36], F32)
            brt = mk(e3, "brt", [1, 36], F32)
            gup = mk(e3, "gup", [32, 256], F32)
            g1 = mk(e3, "g1", [128, D], F32)
            g2 = mk(e3, "g2", [128, D], F32)
            ggl = mk(e3, "ggl", [128, 512], F32)
            cgla = mk(e3, "cgla", [128, 256], F32)
            ctri = mk(e3, "ctri", [128, 512], F32)
            gmask = mk(e3, "gmask", [128, 512], BF16)
            ecap = mk(e3, "ecap", [128, 32], F32)
            ones_f = mk(e3, "ones_f", [128, 128], F32)
            jfr = mk(e3, "jfr", [128, 32], F32, 4)
            jfc = {"i": 0}

            def nextjf():
                jfc["i"] += 1
                return jfr[jfc["i"] % 4]
            NX = debug[10] if (debug and len(debug) > 10) else 5
            R2N = debug[9] if (debug and len(debug) > 9) else 2
            RBIG = 2
            xt = mk(e3, "xt", [128, D], F32, NX)
            st = mk(e3, "st", [128, 8], F32, R2N)
            xs = mk(e3, "xs", [128, D], BF16, R2N)
            xnT = mk(e3, "xnT", [128, KC, 128], BF16, R2N)
            R3 = 3
            v_sb = mk(e3, "v_sb", [128, 512], BF16, R3)
            gk_sb = mk(e3, "gk_sb", [128, 256], F32, R3)
            qk_sb = mk(e3, "qk_sb", [128, 4, 128], F32, R3)
            gmul = mk(e3, "gmul", [128, 512], F32, R3)
            glrT = mk(e3, "glrT", [32, 128], F32, R3)
            sg = mk(e3, "sg", [128, 512], F32)
            ez = mk(e3, "ez", [128, 256], F32)
            sp_sb = mk(e3, "sp_sb", [128, 256], F32, R2N)
            Eq = mk(e3, "Eq", [128, 2, 128], F32, R2N)
            Ek = mk(e3, "Ek", [128, 2, 128], F32, R2N)
            Er = mk(e3, "Er", [128, 256], F32, R2N)
            dec = mk(e3, "dec", [128, 2, 2], F32, R2N)
            qeT = mk(e3, "qeT", [128, 2, 128], BF16, R2N)
            qbd = mk(e3, "qbd", [128, 2, 256], BF16, R2N)
            keT = mk(e3, "keT", [128, 2, 128], BF16, R2N)
            kdz = mk(e3, "kdz", [128, 2, 256], BF16, R2N)
            attm = mk(e3, "attm", [128, 512], BF16, R2N)
            S32 = mk(e3, "S32", [128, 2, 128], F32)
            Sb = mk(e3, "Sb", [128, 2, 256], BF16)
            so = mk(e3, "so", [128, 8], F32, R2N)
            yg = mk(e3, "yg", [128, 512], BF16, R2N)
            ygT = mk(e3, "ygT", [128, 4, 128], BF16, R2N)
            ymTt = mk(e3, "ymTt", [128, 4, 128], BF16, R2N)
            h1s = mk(e3, "h1s", [128, D], F32, RBIG)
            xn2 = mk(e3, "xn2", [128, D], F32, RBIG)
            xn2b = mk(e3, "xn2b", [128, D], BF16, RBIG)
            xn2T = mk(e3, "xn2T", [128, KC, 128], F32)
            lg = mk(e3, "lg", [128, 36], F32, R2N)
            rs = mk(e3, "rs", [128, 16], F32, 2)
            mgt = mk(e3, "mgt", [128, 4], F32, R2N)
            els = mk(e3, "els", [128, 8], F32, R2N)
            els2 = mk(e3, "els2", [128, 8], F32, R2N)
            mk1 = mk(e3, "mk1", [128, 8], F32, R2N)
            mk2 = mk(e3, "mk2", [128, 8], F32, R2N)
            A1 = mk(e3, "A1", [128, 32], F32, R2N)
            A2 = mk(e3, "A2", [128, 32], F32, R2N)
            AA = mk(e3, "AA", [128, 32], F32, R2N)
            posc = mk(e3, "posc", [128, 32], F32, R2N)
            Rr = mk(e3, "Rr", [128, 32], F32)
            p_tp = mk(e3, "p_tp", [128, KC, 128], BF16, psum=True)
            NPA = 4
            pa = mk(e3, "pa", [128, 512], F32, NPA, psum=True)
            po = mk(e3, "po", [128, 512], F32, psum=True)
            pu = mk(e3, "pu", [128, 2, 2, 128], F32, psum=True)
            pmix = mk(e3, "pmix", [128, 512], F32, psum=True)
            pyT = TB(pmix.t[:, 0:256].bitcast(BF16).rearrange("p (m t) -> p m t", m=4), "pyT")
            psm = TB(pmix.t[:, 256:384], "psm")
            pyT.b = pmix.b
            psm.b = pmix.b

            for c in range(KC):
                r = slice(c * 128, (c + 1) * 128)
                DMA("pool", wgt, [], [wgt], out=wgt.t[:, c, :], in_=w_gt[r, :])
                DMA("pool", wgf, [], [wgf], out=wgf.t[:, c, :], in_=w_gf[r, :])
            for c in range(KC):
                r = slice(c * 128, (c + 1) * 128)
                DMA("pool", wo, [], [wo], out=wo.t[:, c, :], in_=w_out[r, :])
            for c in range(KC):
                r = slice(c * 128, (c + 1) * 128)
                DMA("sp", wrt, [], [wrt], out=wrt.t[:, c, :], in_=w_rt[r, :])
            DMA("sp", brt, [], [brt], out=brt.t[:], in_=b_rt)
            OP("dve", "memset", [], [gup], ap=gup.t[:], constant=0.0)
            DMA("sp", gup, [], [gup], out=gup.t[0:17, :], in_=gate_up)
            DMA("sp", g1, [], [g1], out=g1.t[:], in_=g_attn.partition_broadcast(128))
            DMA("sp", g2, [], [g2], out=g2.t[:], in_=g_ffn.partition_broadcast(128))
            for hh in range(4):
                DMA("sp", ggl, [], [ggl], out=ggl.t[:, hh * 128:(hh + 1) * 128], in_=g_gla.partition_broadcast(128))
            DMA("sp", cgla, [], [cgla], out=cgla.t[:], in_=c_gla)
            DMA("sp", ctri, [], [ctri], out=ctri.t[:], in_=c_tri)
            DMA("sp", ecap, [], [ecap], out=ecap.t[:], in_=c_ecap)
            OP("dve", "memset", [], [ones_f], ap=ones_f.t[:], constant=1.0)
            OP("dve", "memset", [], [S32], ap=S32.t[:], constant=0.0)
            OP("dve", "memset", [], [Sb], ap=Sb.t[:], constant=0.0)
            OP("dve", "memset", [], [Rr], ap=Rr.t[:], constant=0.0)
            for k_ in range(R3):
                OP("dve", "memset", [], [glrT[k_]], ap=glrT[k_].t[:], constant=1.0)
            for k_ in range(R2N):
                OP("dve", "memset", [], [qbd[k_]], ap=qbd[k_].t[:], constant=0.0)
                OP("dve", "memset", [], [kdz[k_]], ap=kdz[k_].t[:], constant=0.0)
            cnt = {"pa": 0}

            pac = {}

            def nextpa(tag="a"):
                g_ = PA_CFG.get(tag, PA_CFG.get(tag[0]))
                key = tuple(g_)
                k_ = pac.get(key, 0)
                pac[key] = k_ + 1
                return pa[g_[k_ % len(g_)]]

            def load_x(i):
                X = xt[i % NX]
                DMA("sp", X, [], [X], out=X.t[:], in_=x[i * 128:(i + 1) * 128, :])

            LN8 = float(np.log(0.125))

            def s1a(i):
                X, ST, XS, XT = xt[i % NX], st[i % R2N], xs[i % R2N], xnT[i % R2N]
                r3 = i % R3
                OP("act", "activation", [X], [ST, XS], out=XS.t[:], in_=X.t[:], func=AF.Square, accum_out=ST.t[:, 0:1])
                OP("act", "activation", [ST], [ST], out=ST.t[:, 1:2], in_=ST.t[:, 0:1], func=AF.Ln, scale=1.0 / D, bias=EPS)
                OP("act", "activation", [ST], [ST], out=ST.t[:, 2:3], in_=ST.t[:, 1:2], func=AF.Exp, scale=-0.5)
                OP("dve", "scalar_tensor_tensor", [X, ST, g1], [XS], out=XS.t[:], in0=X.t[:], scalar=ST.t[:, 2:3], in1=g1.t[:],
                   op0=ALU.mult, op1=ALU.mult)
                for c in range(KC):
                    OP("pe", "transpose", [XS, B_ident], [p_tp], out=p_tp.t[:, c, :], in_=XS.t[:, c * 128:(c + 1) * 128], identity=ident_b[:])
                OP("act", "activation", [p_tp], [XT], out=XT.t[:], in_=p_tp.t[:], func=AF.Copy)
                A = nextpa("a1")
                for c in range(KC):
                    OP("pe", "matmul", [XT, wgt], [A], out=A.t[:, 0:256], lhsT=XT.t[:, c, :], rhs=wgt.t[:, c, 0:256],
                       start=(c == 0), stop=(c == KC - 1))
                for c in range(KC):
                    OP("pe", "matmul", [XT, wgf], [A], out=A.t[0:16, 256:384], lhsT=wgf.t[:, c, 512:528], rhs=XT.t[:, c, :],
                       start=(c == 0), stop=(c == KC - 1))
                OP("act", "activation", [A], [gk_sb[r3]], out=gk_sb[r3].t[:], in_=A.t[:, 0:256], func=AF.Copy)
                OP("act", "activation", [A], [glrT[r3]], out=glrT[r3].t[0:16, :], in_=A.t[0:16, 256:384], func=AF.Copy)
                A = nextpa("a2")
                for c in range(KC):
                    OP("pe", "matmul", [XT, wgt], [A], out=A.t[:], lhsT=XT.t[:, c, :], rhs=wgt.t[:, c, 256:768],
                       start=(c == 0), stop=(c == KC - 1))
                OP("act", "activation", [A], [v_sb[r3]], out=v_sb[r3].t[:], in_=A.t[:], func=AF.Copy)
                A = nextpa("a3")
                for c in range(KC):
                    OP("pe", "matmul", [XT, wgt], [A], out=A.t[:], lhsT=XT.t[:, c, :], rhs=wgt.t[:, c, 768:1280],
                       start=(c == 0), stop=(c == KC - 1))
                OP("act", "activation", [A], [sg], out=sg.t[:], in_=A.t[:], func=AF.Exp, scale=-1.0)
                OP("act", "activation", [sg], [sg], out=sg.t[:], in_=sg.t[:], func=AF.Ln, bias=1.0)
                OP("act", "activation", [sg], [sg], out=sg.t[:], in_=sg.t[:], func=AF.Exp, scale=-1.0)
                OP("dve", "tensor_tensor", [A, sg], [sg], out=sg.t[:], in0=A.t[:], in1=sg.t[:], op=ALU.mult)
                OP("dve", "tensor_tensor", [sg, ggl], [gmul[r3]], out=gmul[r3].t[:], in0=sg.t[:], in1=ggl.t[:], op=ALU.mult)
                A = nextpa("a4")
                for m in range(4):
                    for c in range(KC):
                        OP("pe", "matmul", [XT, wgf], [A], out=A.t[:, m * 128:(m + 1) * 128], lhsT=wgf.t[:, c, m * 128:(m + 1) * 128],
                           rhs=XT.t[:, c, :], start=(c == 0), stop=(c == KC - 1))
                OP("act", "activation", [A], [qk_sb[r3]], out=qk_sb[r3].t[:], in_=A.t[:].rearrange("p (m t) -> p m t", m=4), func=AF.Copy)

            sub = debug[6] if (debug and len(debug) > 6) else 9

            def s1b(i):
                r3, r2 = i % R3, i % R2N
                A = nextpa("b1")
                OP("pe", "matmul", [glrT[r3], gup], [A], out=A.t[:, 0:256], lhsT=glrT[r3].t[0:17, :], rhs=gup.t[0:17, :], start=True, stop=True)
                OP("act", "activation", [A], [ez], out=ez.t[:], in_=A.t[:, 0:256], func=AF.Exp, scale=-1.0)
                OP("act", "activation", [ez], [sp_sb[r2]], out=sp_sb[r2].t[:], in_=ez.t[:], func=AF.Ln, bias=1.0)
                if sub < 2:
                    return
                OP("pe", "matmul", [sp_sb[r2], cgla], [A], out=A.t[:, 256:512], lhsT=cgla.t[:, 128:256], rhs=sp_sb[r2].t[:], start=True, stop=True)
                Bk = nextpa("b2")
                for m in range(2):
                    OP("pe", "matmul", [sp_sb[r2], cgla], [Bk], out=Bk.t[:, m * 128:(m + 1) * 128], lhsT=sp_sb[r2].t[:, m * 128:(m + 1) * 128],
                       rhs=cgla.t[:, 0:128], start=True, stop=True)
                bT = Bk.t[:, 0:256].rearrange("p (m t) -> p m t", m=2)
                if sub < 3:
                    return
                OP("act", "activation", [A], [Er[r2]], out=Er[r2].t[:], in_=A.t[:, 256:512], func=AF.Exp)
                OP("act", "activation", [Bk], [Eq[r2]], out=Eq[r2].t[:], in_=bT, func=AF.Exp, bias=LN8)
                OP("act", "activation", [Bk], [Ek[r2]], out=Ek[r2].t[:], in_=bT, func=AF.Exp, scale=-1.0)
                if sub < 4:
                    return
                bT4 = Bk.t[:, 0:256].rearrange("p (m c j) -> p m c j", m=2, c=2)
                OP("act", "activation", [Bk], [dec[r2]], out=dec[r2].t[:], in_=bT4[:, :, :, 63], func=AF.Exp)
                if sub < 5:
                    return
                OP("dve", "tensor_tensor", [qk_sb[r3], Eq[r2]], [qeT[r2]], out=qeT[r2].t[:], in0=qk_sb[r3].t[:, 0:2, :], in1=Eq[r2].t[:], op=ALU.mult)
                OP("dve", "tensor_tensor", [qk_sb[r3], Ek[r2]], [keT[r2]], out=keT[r2].t[:], in0=qk_sb[r3].t[:, 2:4, :], in1=Ek[r2].t[:], op=ALU.mult)
                OP("dve", "tensor_tensor", [qk_sb[r3], Eq[r2]], [qbd[r2]], out=qbd[r2].t[0:64, :, 0:128], in0=qk_sb[r3].t[0:64, 0:2, :], in1=Eq[r2].t[0:64, :, :], op=ALU.mult)
                OP("dve", "tensor_tensor", [qk_sb[r3], Eq[r2]], [qbd[r2]], out=qbd[r2].t[64:128, :, 128:256], in0=qk_sb[r3].t[64:128, 0:2, :], in1=Eq[r2].t[64:128, :, :], op=ALU.mult)
                OP("dve", "tensor_tensor", [gk_sb[r3], Er[r2]], [kdz[r2]], out=kdz[r2].t[0:64, 0, :], in0=gk_sb[r3].t[0:64, :], in1=Er[r2].t[0:64, :], op=ALU.mult)
                OP("dve", "tensor_tensor", [gk_sb[r3], Er[r2]], [kdz[r2]], out=kdz[r2].t[64:128, 1, :], in0=gk_sb[r3].t[64:128, :], in1=Er[r2].t[64:128, :], op=ALU.mult)
                if sub < 6:
                    return
                Ck = nextpa("b3")
                for hp in range(2):
                    OP("pe", "matmul", [keT[r2], qbd[r2]], [Ck], out=Ck.t[:, hp * 256:(hp + 1) * 256], lhsT=keT[r2].t[:, hp, :], rhs=qbd[r2].t[:, hp, :],
                       start=True, stop=True)
                OP("dve", "tensor_tensor", [Ck, gmask], [attm[r2]], out=attm[r2].t[:], in0=Ck.t[:], in1=gmask.t[:], op=ALU.mult)

            def s2(i):
                r3, r2 = i % R3, i % R2N
                for cc in range(2):
                    for hh in range(4):
                        pb_ = slice((hh % 2) * 64, (hh % 2) * 64 + 64)
                        OP("pe", "matmul", [kdz[r2], v_sb[r3]], [pu], out=pu.t[pb_, cc, hh // 2, :], lhsT=kdz[r2].t[:, cc, hh * 64:(hh + 1) * 64],
                           rhs=v_sb[r3].t[:, hh * 128:(hh + 1) * 128], start=True, stop=True)
                for hh in range(4):
                    OP("pe", "matmul", [attm[r2], v_sb[r3]], [po], out=po.t[:, hh * 128:(hh + 1) * 128], lhsT=attm[r2].t[:, hh * 128:(hh + 1) * 128],
                       rhs=v_sb[r3].t[:, hh * 128:(hh + 1) * 128], start=(hh == 0), stop=False, skip_group_check=True)
                for cc in range(2):
                    cs_ = slice(cc * 64, cc * 64 + 64)
                    for hp in range(2):
                        OP("pe", "matmul", [qeT[r2], Sb], [po], out=po.t[cs_, hp * 256:(hp + 1) * 256], lhsT=qeT[r2].t[:, hp, cs_],
                           rhs=Sb.t[:, hp, :], start=False, stop=(cc == 1 and hp == 1), skip_group_check=True)
                    for hp in range(2):
                        OP("dve", "scalar_tensor_tensor", [S32, dec[r2], pu], [S32], out=S32.t[:, hp, :], in0=S32.t[:, hp, :],
                           scalar=dec[r2].t[:, hp, cc:cc + 1], in1=pu.t[:, cc, hp, :], op0=ALU.mult, op1=ALU.add)
                    OP("act", "activation", [S32], [Sb], out=Sb.t[0:64, :, 0:128], in_=S32.t[0:64, :, :], func=AF.Copy)
                    OP("dve", "tensor_copy", [S32], [Sb], out=Sb.t[64:128, :, 128:256], in_=S32.t[64:128, :, :])
                SO = so[r2]
                for hh in range(4):
                    OP("act", "activation", [po], [SO, yg[r2]], out=yg[r2].t[:, hh * 128:(hh + 1) * 128], in_=po.t[:, hh * 128:(hh + 1) * 128], func=AF.Square,
                       accum_out=SO.t[:, hh:hh + 1])
                OP("act", "activation", [SO], [SO], out=SO.t[:, 4:8], in_=SO.t[:, 0:4], func=AF.Ln, scale=1.0 / 128, bias=EPS)
                OP("act", "activation", [SO], [SO], out=SO.t[:, 4:8], in_=SO.t[:, 4:8], func=AF.Exp, scale=-0.5)
                for hh in range(4):
                    hs_ = slice(hh * 128, (hh + 1) * 128)
                    OP("dve", "scalar_tensor_tensor", [po, SO, gmul[r3]], [yg[r2]], out=yg[r2].t[:, hs_], in0=po.t[:, hs_],
                       scalar=SO.t[:, 4 + hh:5 + hh], in1=gmul[r3].t[:, hs_], op0=ALU.mult, op1=ALU.mult)
                for hh in range(4):
                    OP("pe", "transpose", [yg[r2], B_ident], [pyT], out=pyT.t[:, hh, :], in_=yg[r2].t[:, hh * 128:(hh + 1) * 128], identity=ident_b[:])
                OP("act", "activation", [pyT], [ygT[r2]], out=ygT[r2].t[:], in_=pyT.t[:], func=AF.Copy)

            def s3(i):
                r2 = i % R2N
                rb = i % RBIG
                X = xt[i % NX]
                gc = slice(i * 128, (i + 1) * 128)
                H = h1s[rb]
                A = nextpa("c1")
                ymv = A.t[:, 0:256].bitcast(BF16).rearrange("p (m t) -> p m t", m=4)
                for c in range(4):
                    OP("pe", "transpose", [B_ymT, B_ident], [A], out=ymv[:, c, :], in_=ymT[:, i, c * 128:(c + 1) * 128], identity=ident_b[:])
                OP("act", "activation", [A], [ymTt[r2]], out=ymTt[r2].t[:], in_=ymv, func=AF.Copy)
                for half in range(2):
                    hc = slice(half * 512, (half + 1) * 512)
                    A = nextpa("c2")
                    for c in range(4):
                        OP("pe", "matmul", [ygT[r2], wo], [A], out=A.t[:], lhsT=ygT[r2].t[:, c, :], rhs=wo.t[:, c, hc], start=(c == 0), stop=False)
                    for c in range(4):
                        OP("pe", "matmul", [ymTt[r2], wo], [A], out=A.t[:], lhsT=ymTt[r2].t[:, c, :], rhs=wo.t[:, 4 + c, hc], start=False, stop=(c == 3))
                    OP("dve", "tensor_tensor", [A, X], [H], out=H.t[:, hc], in0=A.t[:], in1=X.t[:, hc], op=ALU.add)
                DMA("sp", H, [H], [], out=h1_d[gc, :], in_=H.t[:])
                if stage < 4:
                    return
                ST = st[r2]
                OP("act", "activation", [H], [ST, xn2b[rb]], out=xn2b[rb].t[:], in_=H.t[:], func=AF.Square, accum_out=ST.t[:, 4:5])
                OP("act", "activation", [ST], [ST], out=ST.t[:, 5:6], in_=ST.t[:, 4:5], func=AF.Ln, scale=1.0 / D, bias=EPS)
                OP("act", "activation", [ST], [ST], out=ST.t[:, 6:7], in_=ST.t[:, 5:6], func=AF.Exp, scale=-0.5)
                XN = xn2[rb]
                OP("dve", "scalar_tensor_tensor", [H, ST, g2], [XN], out=XN.t[:], in0=H.t[:], scalar=ST.t[:, 6:7], in1=g2.t[:],
                   op0=ALU.mult, op1=ALU.mult)
                OP("pool", "tensor_copy", [XN], [xn2b[rb]], out=xn2b[rb].t[:], in_=XN.t[:])
                for half in range(2):
                    A = nextpa("c3")
                    for c in range(4):
                        cc = half * 4 + c
                        OP("pe", "transpose", [XN, B_ident], [A], out=A.t[:, c * 128:(c + 1) * 128], in_=XN.t[:, cc * 128:(cc + 1) * 128], identity=ident_f[:])
                    OP("act", "activation", [A], [xn2T], out=xn2T.t[:, half * 4:(half + 1) * 4, :], in_=A.t[:].rearrange("p (m t) -> p m t", m=4), func=AF.Copy)
                for c in range(KC):
                    OP("pe", "matmul", [xn2T, wrt], [psm], out=psm.t[:, 0:36], lhsT=xn2T.t[:, c, :], rhs=wrt.t[:, c, :], start=(c == 0), stop=False)
                OP("pe", "matmul", [ones_f, brt], [psm], out=psm.t[:, 0:36], lhsT=ones_f.t[0:1, :], rhs=brt.t[0:1, :], start=False, stop=True)
                L, RS, MG = lg[r2], rs[r2], mgt[r2]
                OP("dve", "tensor_copy", [psm], [L], out=L.t[:], in_=psm.t[:, 0:36])
                OP("dve", "tensor_reduce", [L], [RS], out=RS.t[:, 0:1], in_=L.t[:, 0:4], axis=AX.X, op=ALU.max)
                OP("dve", "tensor_scalar", [L, RS], [MG], out=MG.t[:], in0=L.t[:, 0:4], scalar1=RS.t[:, 0:1], scalar2=None, op0=ALU.is_equal)
                OP("dve", "tensor_scalar", [RS], [RS], out=RS.t[:, 1:2], in0=RS.t[:, 0:1], scalar1=-1.0, scalar2=None, op0=ALU.mult)
                JF = nextjf()
                OP("act", "activation", [L, RS], [RS, JF], out=JF.t[:, 0:4], in_=L.t[:, 0:4], func=AF.Exp, bias=RS.t[:, 1:2], accum_out=RS.t[:, 2:3])
                OP("dve", "reciprocal", [RS], [RS], out=RS.t[:, 3:4], in_=RS.t[:, 2:3])
                E_, E2, M1, M2 = els[r2], els2[r2], mk1[r2], mk2[r2]
                OP("dve", "tensor_scalar", [L, MG], [E_], out=E_.t[:], in0=L.t[:, 4:12], scalar1=MG.t[:, 0:1], scalar2=None, op0=ALU.mult)
                for g_ in range(1, 4):
                    OP("dve", "scalar_tensor_tensor", [L, MG, E_], [E_], out=E_.t[:], in0=L.t[:, 4 + 8 * g_:12 + 8 * g_], scalar=MG.t[:, g_:g_ + 1],
                       in1=E_.t[:], op0=ALU.mult, op1=ALU.add)
                OP("dve", "tensor_reduce", [E_], [RS], out=RS.t[:, 4:5], in_=E_.t[:], axis=AX.X, op=ALU.max)
                OP("dve", "tensor_scalar", [E_, RS], [M1], out=M1.t[:], in0=E_.t[:], scalar1=RS.t[:, 4:5], scalar2=None, op0=ALU.is_equal)
                OP("dve", "scalar_tensor_tensor", [M1, E_], [E2], out=E2.t[:], in0=M1.t[:], scalar=-1e30, in1=E_.t[:], op0=ALU.mult, op1=ALU.add)
                OP("dve", "tensor_reduce", [E2], [RS], out=RS.t[:, 5:6], in_=E2.t[:], axis=AX.X, op=ALU.max)
                OP("dve", "tensor_scalar", [E2, RS], [M2], out=M2.t[:], in0=E2.t[:], scalar1=RS.t[:, 5:6], scalar2=None, op0=ALU.is_equal)
                OP("dve", "tensor_scalar", [RS], [RS], out=RS.t[:, 6:7], in0=RS.t[:, 4:5], scalar1=-1.0, scalar2=None, op0=ALU.mult)
                OP("act", "activation", [RS], [RS], out=RS.t[:, 7:8], in_=RS.t[:, 5:6], func=AF.Exp, bias=RS.t[:, 6:7])
                OP("dve", "tensor_scalar", [RS], [RS], out=RS.t[:, 8:9], in0=RS.t[:, 7:8], scalar1=1.0, scalar2=None, op0=ALU.add)
                OP("dve", "reciprocal", [RS], [RS], out=RS.t[:, 9:10], in_=RS.t[:, 8:9])
                OP("dve", "tensor_tensor", [RS], [route_g], out=route_g.t[:, i, 0:1], in0=RS.t[:, 9:10], in1=RS.t[:, 3:4], op=ALU.mult)
                OP("dve", "tensor_tensor", [RS, route_g], [route_g], out=route_g.t[:, i, 1:2], in0=route_g.t[:, i, 0:1], in1=RS.t[:, 7:8], op=ALU.mult)
                for g_ in range(4):
                    es_ = slice(g_ * 8, (g_ + 1) * 8)
                    OP("dve", "tensor_scalar", [M1, MG], [A1[r2]], out=A1[r2].t[:, es_], in0=M1.t[:], scalar1=MG.t[:, g_:g_ + 1], scalar2=None, op0=ALU.mult)
                    OP("dve", "tensor_scalar", [M2, MG], [A2[r2]], out=A2[r2].t[:, es_], in0=M2.t[:], scalar1=MG.t[:, g_:g_ + 1], scalar2=None, op0=ALU.mult)
                OP("dve", "tensor_tensor", [A1[r2], A2[r2]], [AA[r2]], out=AA[r2].t[:], in0=A1[r2].t[:], in1=A2[r2].t[:], op=ALU.add)
                OP("pe", "matmul", [ctri, AA[r2]], [psm], out=psm.t[:, 64:96], lhsT=ctri.t[:, 384:512], rhs=AA[r2].t[:], start=True, stop=True)
                OP("pe", "matmul", [ones_f, AA[r2]], [psm], out=psm.t[:, 96:128], lhsT=ones_f.t[:], rhs=AA[r2].t[:], start=True, stop=True)
                PC = posc[r2]
                OP("dve", "tensor_tensor", [psm, Rr], [PC], out=PC.t[:], in0=psm.t[:, 64:96], in1=Rr.t[:], op=ALU.add)
                OP("dve", "tensor_tensor", [psm, Rr], [Rr], out=Rr.t[:], in0=psm.t[:, 96:128], in1=Rr.t[:], op=ALU.add)
                OP("dve", "tensor_tensor", [PC, ecap], [PC], out=PC.t[:], in0=PC.t[:], in1=ecap.t[:], op=ALU.add)
                JF = nextjf()
                OP("dve", "scalar_tensor_tensor", [A1[r2], PC], [RS, JF], out=JF.t[:], in0=A1[r2].t[:], scalar=1.0, in1=PC.t[:], op0=ALU.mult, op1=ALU.mult,
                   accum_out=RS.t[:, 10:11])
                JF = nextjf()
                OP("dve", "scalar_tensor_tensor", [A2[r2], PC], [RS, JF], out=JF.t[:], in0=A2[r2].t[:], scalar=1.0, in1=PC.t[:], op0=ALU.mult, op1=ALU.mult,
                   accum_out=RS.t[:, 11:12])
                OP("dve", "tensor_copy", [RS], [route_i], out=route_i.t[:, i, :], in_=RS.t[:, 10:12])
                if stage < 5:
                    return
                for k_ in range(2):
                    DMA("pool", xn2b[rb], [xn2b[rb], route_i], [], name="indirect_dma_start", out=xbuf_d[:, :],
                        out_offset=bass.IndirectOffsetOnAxis(ap=route_i.t[:, i, k_:k_ + 1], axis=0), in_=xn2b[rb].t[:, :], in_offset=None)

            for hh in range(4):
                OP("dve", "tensor_copy", [ctri], [gmask], out=gmask.t[:, hh * 128:(hh + 1) * 128], in_=ctri.t[:, 128:256])
            ORDER3 = debug[8] if (debug and len(debug) > 8) else 0
            if stage >= 1 and ORDER3 == 1:
                for j in range(min(3, NTILE)):
                    load_x(j)
                for i in range(NTILE):
                    if i + 3 < NTILE:
                        load_x(i + 3)
                    s1a(i)
                    s1b(i)
                    s2(i)
                    s3(i)
            elif stage >= 1:
                load_x(0)
                load_x(1)
                s1a(0)
                s1a(1)
                if stage >= 2:
                    s1b(0)
                for i in range(NTILE):
                    if i + 2 < NTILE:
                        load_x(i + 2)
                        s1a(i + 2)
                    if i + 1 < NTILE and stage >= 2:
                        s1b(i + 1)
                    if stage >= 3:
                        s2(i)
                        if i >= 1:
                            s3(i - 1)
                if stage >= 3:
                    s3(NTILE - 1)
            sc.run_block("p3")

        if stage >= 6:
            with ExitStack() as e4:
                NEX = NE if not (debug and len(debug) > 7) else debug[7]
                RW = 4
                wall = mk(e4, "wall", [128, 6144], BF16, RW)
                wgu = [TB(w_.t[:, 0:4096].rearrange("p (c n) -> p c n", n=512), "x") for w_ in wall]
                wd = [TB(w_.t[:, 4096:6144].rearrange("p (c n) -> p c n", n=1024), "x") for w_ in wall]
                for k_ in range(RW):
                    wgu[k_].b = wall[k_].b
                    wd[k_].b = wall[k_].b
                xe = mk(e4, "xe", [128, 3, D], BF16, 3)
                xeT = mk(e4, "xeT", [128, KC, CAP], BF16, 2)
                sgm = mk(e4, "sgm", [128, CAP], F32, 2)
                hT = mk(e4, "hT", [128, 2, CAP], BF16, 2)
                ye = mk(e4, "ye", [128, 3, D], BF16, 2)
                ptp4 = mk(e4, "ptp4", [128, KC, 128], BF16, psum=True)
                pg = mk(e4, "pg", [128, 512], F32, 4, psum=True)
                py = mk(e4, "py", [128, 512], F32, 3, psum=True)
                c4 = {"py": 0}

                def load_w(e):
                    s_ = e % RW
                    DMA("pool", wall[s_], [], [wall[s_]], out=wall[s_].t[:], in_=wbf_d[e])

                def load_xe(e):
                    s_ = e % 3
                    DMA("sp", xe[s_], [], [xe[s_]], out=xe[s_].t[:], in_=xbuf_d[e * CAP:(e + 1) * CAP, :].rearrange("(s p) d -> p s d", p=128))

                def stA(e):
                    s_ = e % 2
                    xev = xe[e % 3].t[:].rearrange("p s (k c) -> p s c k", c=KC)
                    for s in range(3):
                        for c in range(KC):
                            OP("pe", "transpose", [xe[e % 3], B_ident], [ptp4], out=ptp4.t[:, c, :], in_=xev[:, s, c, :], identity=ident_b[:])
                        OP("act", "activation", [ptp4], [xeT[s_]], out=xeT[s_].t[:, :, s * 128:(s + 1) * 128], in_=ptp4.t[:], func=AF.Copy)

                def stB(e):
                    s_ = e % 2
                    w_ = e % RW
                    wv = wgu[w_].t[:].rearrange("p c (w j m) -> p c w m j", w=2, m=2)
                    for m in range(2):
                        for which in range(2):
                            P_ = pg[which * 2 + m]
                            for c in range(KC):
                                OP("pe", "matmul", [wgu[w_], xeT[s_]], [P_], out=P_.t[:, 0:CAP], lhsT=wv[:, c, which, m, :],
                                   rhs=xeT[s_].t[:, c, :], start=(c == 0), stop=(c == KC - 1))
                        G, U, SG = pg[m], pg[2 + m], sgm[m]
                        OP("act", "activation", [G], [SG], out=SG.t[:], in_=G.t[:, 0:CAP], func=AF.Exp, scale=-1.0)
                        OP("act", "activation", [SG], [SG], out=SG.t[:], in_=SG.t[:], func=AF.Ln, bias=1.0)
                        OP("act", "activation", [SG], [SG], out=SG.t[:], in_=SG.t[:], func=AF.Exp, scale=-1.0)
                        OP("dve", "tensor_tensor", [G, SG], [SG], out=SG.t[:], in0=G.t[:, 0:CAP], in1=SG.t[:], op=ALU.mult)
                        OP("dve", "tensor_tensor", [U, SG], [hT[s_]], out=hT[s_].t[:, m, :], in0=U.t[:, 0:CAP], in1=SG.t[:], op=ALU.mult)

                def stC(e):
                    s_ = e % 2
                    for s in range(3):
                        for half in range(2):
                            Y = py[c4["py"] % 3]
                            c4["py"] += 1
                            for m in range(2):
                                OP("pe", "matmul", [hT[s_], wd[e % RW]], [Y], out=Y.t[:], lhsT=hT[s_].t[:, m, s * 128:(s + 1) * 128],
                                   rhs=wd[e % RW].t[:, m, half * 512:(half + 1) * 512], start=(m == 0), stop=(m == 1))
                            if half == 0:
                                OP("act", "activation", [Y], [ye[s_]], out=ye[s_].t[:, s, 0:512], in_=Y.t[:], func=AF.Copy)
                            else:
                                OP("dve", "tensor_copy", [Y], [ye[s_]], out=ye[s_].t[:, s, 512:1024], in_=Y.t[:])
                    DMA("sp", ye[s_], [ye[s_]], [], out=ybuf_d[e * CAP:(e + 1) * CAP, :].rearrange("(s p) d -> p s d", p=128), in_=ye[s_].t[:])

                for e in range(min(RW, NEX)):
                    load_w(e)
                for e in range(min(3, NEX)):
                    load_xe(e)
                stA(0)
                for e in range(NEX):
                    if e + 1 < NEX:
                        stA(e + 1)
                    stB(e)
                    if e >= 1:
                        stC(e - 1)
                        if e - 1 + RW < NEX:
                            load_w(e - 1 + RW)
                    if e + 3 < NEX:
                        load_xe(e + 3)
                stC(NEX - 1)
                sc.run_block("p4")

        if stage >= 7:
            with ExitStack() as e5:
                gf = mk(e5, "gf", [128, D], F32)
                R5 = 7
                hh = mk(e5, "hh", [128, D], F32, R5)
                yy = [mk(e5, "yy%d" % k_, [128, D], BF16, R5) for k_ in range(2)]
                hf = mk(e5, "hf", [128, D], F32, 3)
                ob = mk(e5, "ob", [128, D], F32, 3)
                st5 = mk(e5, "st5", [128, 4], F32, 2)
                B_out = Buf("out")
                DMA("sp", gf, [], [gf], out=gf.t[:], in_=g_fin.partition_broadcast(128))

                def load5(i):
                    s_ = i % R5
                    DMA("sp", hh[s_], [], [hh[s_]], out=hh[s_].t[:], in_=h1_d[i * 128:(i + 1) * 128, :])
                    for k_ in range(2):
                        DMA("pool", yy[k_][s_], [route_i], [yy[k_][s_]], name="indirect_dma_start", out=yy[k_][s_].t[:, :], out_offset=None,
                            in_=ybuf_d[:, :], in_offset=bass.IndirectOffsetOnAxis(ap=route_i.t[:, i, k_:k_ + 1], axis=0))

                NT5 = NTILE
                for i in range(min(R5 - 1, NT5)):
                    load5(i)
                for i in range(NT5):
                    if i + R5 - 1 < NT5:
                        load5(i + R5 - 1)
                    s_, r2 = i % R5, i % 2
                    OP("dve", "scalar_tensor_tensor", [yy[0][s_], route_g, hh[s_]], [hf[i % 3]], out=hf[i % 3].t[:], in0=yy[0][s_].t[:],
                       scalar=route_g.t[:, i, 0:1], in1=hh[s_].t[:], op0=ALU.mult, op1=ALU.add)
                    OP("dve", "scalar_tensor_tensor", [yy[1][s_], route_g, hf[i % 3]], [hf[i % 3]], out=hf[i % 3].t[:], in0=yy[1][s_].t[:],
                       scalar=route_g.t[:, i, 1:2], in1=hf[i % 3].t[:], op0=ALU.mult, op1=ALU.add)
                    ST = st5[r2]
                    OP("act", "activation", [hf[i % 3]], [ST, ob[i % 3]], out=ob[i % 3].t[:], in_=hf[i % 3].t[:], func=AF.Square, accum_out=ST.t[:, 0:1])
                    OP("act", "activation", [ST], [ST], out=ST.t[:, 1:2], in_=ST.t[:, 0:1], func=AF.Ln, scale=1.0 / D, bias=EPS)
                    OP("act", "activation", [ST], [ST], out=ST.t[:, 2:3], in_=ST.t[:, 1:2], func=AF.Exp, scale=-0.5)
                    OP("dve", "scalar_tensor_tensor", [hf[i % 3], ST, gf], [ob[i % 3]], out=ob[i % 3].t[:], in0=hf[i % 3].t[:], scalar=ST.t[:, 2:3], in1=gf.t[:],
                       op0=ALU.mult, op1=ALU.mult)
                    DMA("sp", ob[i % 3], [ob[i % 3]], [], out=out[i * 128:(i + 1) * 128, :], in_=ob[i % 3].t[:])
                sc.run_block("p5")

        if dbg is not None and 3 <= stage <= 5:
            with ExitStack() as ed:
                dt_ = mk(ed, "dbg_t", [128, D], F32)
                for i in range(NTILE):
                    DMA("sp", dt_, [B_h1d], [dt_], out=dt_.t[:], in_=h1_d[i * 128:(i + 1) * 128, :])
                    DMA("sp", dt_, [dt_], [], out=dbg[i * 128:(i + 1) * 128, :], in_=dt_.t[:])
                if stage >= 4:
                    dr = mk(ed, "dbg_r", [128, NT * 2], F32)
                    nn = NTILE * 2
                    OP("dve", "tensor_copy", [route_i], [dr], out=dr.t[:, 0:nn], in_=route_i.t[:].rearrange("p t k -> p (t k)")[:, 0:nn])
                    DMA("sp", dr, [dr], [], out=dbg[S:S + 128, 0:nn], in_=dr.t[:, 0:nn])
                    DMA("sp", route_g, [route_g], [], out=dbg[S:S + 128, 64:64 + nn], in_=route_g.t[:].rearrange("p t k -> p (t k)")[:, 0:nn])
                sc.run_block("dbg")
    return nc


def _consts():
    ident = np.eye(128, dtype=np.float32)
    half = 16
    inv = (10000.0 ** (-np.arange(half, dtype=np.float32) / half)).astype(np.float32)
    rope = np.zeros((128, 2), np.float32)
    for g_ in range(4):
        rope[g_ * 32:g_ * 32 + 16, 0] = inv
        rope[g_ * 32 + 16:g_ * 32 + 32, 0] = inv
        rope[g_ * 32:g_ * 32 + 16, 1] = -1.0
        rope[g_ * 32 + 16:g_ * 32 + 32, 1] = 1.0
    tri = np.zeros((128, 512), np.float32)
    kk = np.arange(128)
    tri[:, 0:128] = (kk[:, None] <= kk[None, :]).astype(np.float32)
    tri[:, 128:256] = (((kk[:, None] // 64) == (kk[None, :] // 64)) & (kk[:, None] <= kk[None, :])).astype(np.float32)
    tri[:, 384:512] = (kk[:, None] < kk[None, :]).astype(np.float32)
    same = (kk[:, None] // 64) == (kk[None, :] // 64)
    gla = np.zeros((128, 256), np.float32)
    gla[:, 0:128] = np.where(same & (kk[:, None] <= kk[None, :]), -1.0 / 16.0, 0.0)
    gla[:, 128:256] = np.where(same & (kk[:, None] > kk[None, :]), -1.0 / 16.0, 0.0)
    ecap = np.tile((np.arange(32, dtype=np.float32) * CAP)[None, :], (128, 1))
    return ident, rope, tri, gla, ecap


def prepare_inputs(inp):
    f = lambda a: np.ascontiguousarray(np.asarray(a, dtype=np.float32))
    w_in = f(inp["w_in"][0])
    gq, gk, gv, glr, gog, cq, ckv, kr = np.split(w_in, np.cumsum([256, 256, 512, 16, 512, 384, 256])[:], axis=1)
    kr_sw = np.concatenate([kr[:, 16:], kr[:, :16]], axis=1)
    wuq = f(inp["mla_w_uq"][0]).reshape(384, 8, 96)
    wuq_r = wuq[:, :, 64:]
    wuq_sw = np.concatenate([wuq_r[:, :, 16:], wuq_r[:, :, :16]], axis=2)
    wuq_all = np.concatenate([wuq, wuq_sw], axis=2).reshape(384, 8 * 128)
    wukv = f(inp["mla_w_ukv"][0]).reshape(256, 8, 128)
    ident, rope, tri, gla, ecap = _consts()
    common = {
        "g_attn": f(inp["attn_norm_w"]).reshape(1, D),
        "g_ffn": f(inp["ffn_norm_w"]).reshape(1, D),
        "g_fin": f(inp["final_norm_w"]).reshape(1, D),
        "g_q": f(inp["mla_q_norm_w"]).reshape(1, 384),
        "g_kv": f(inp["mla_kv_norm_w"]).reshape(1, 256),
        "g_gla": f(inp["gla_norm_w"]).reshape(1, 128),
        "w_mla": f(np.concatenate([cq, ckv], axis=1)),
        "w_kr": f(np.concatenate([kr, kr_sw], axis=1)),
        "w_gf": f(np.concatenate([gq, gk, glr], axis=1)),
        "w_gt": f(np.concatenate([gk, gv, gog], axis=1)),
        "w_uq": f(wuq_all),
        "w_ukn": f(wukv[:, :, :64].reshape(256, 512)),
        "w_uv": f(wukv[:, :, 64:].reshape(256, 512)),
        "gate_up": f(np.concatenate([inp["gla_gate_up"][0], np.asarray(inp["gla_gate_bias"][0]).reshape(1, 256)], axis=0)),
        "w_out": f(inp["w_out"][0]),
        "w_rt": f(np.concatenate([inp["router_group_w"][0], inp["router_expert_w"][0]], axis=1)),
        "b_rt": f(np.concatenate([inp["router_group_b"][0], inp["router_expert_b"][0]]).reshape(1, 36)),
        "w_eg": f(inp["expert_w_gate"][0]),
        "w_eu": f(inp["expert_w_up"][0]),
        "w_ed": f(inp["expert_w_down"][0]),
        "c_ident": ident,
        "c_rope": rope,
        "c_tri": tri,
        "c_gla": gla,
        "c_ecap": ecap,
    }
    xs_ = np.asarray(inp["x"], dtype=np.float32)
    ps_ = np.asarray(inp["positions"], dtype=np.int32)
    maps = []
    for c in range(8):
        m = dict(common)
        m["x"] = np.ascontiguousarray(xs_[c])
        m["pos"] = np.ascontiguousarray(ps_[c].reshape(1, S))
        maps.append(m)
    return maps


def kernel(**inputs):
    nc = build_program()
    maps = prepare_inputs(inputs)
    res = run_bass_kernel_spmd(nc, maps, core_ids=list(range(8)))
    return np.stack([np.asarray(r["out"]).reshape(S, D) for r in res.results], axis=0).astype(np.float32)
```

```python
import numpy as np
import ml_dtypes
import concourse.bass as bass
import concourse.mybir as mybir
from concourse.bass_utils import run_bass_kernel_spmd

F32 = mybir.dt.float32
BF16 = mybir.dt.bfloat16
I32 = mybir.dt.int32
AF = mybir.ActivationFunctionType
ALU = mybir.AluOpType
AX = mybir.AxisListType

S = 4096
NT = 32
D = 1024
KC = 8
EPS = 1e-6
NE = 32
CAP = 384
NSLOT = NE * CAP
TWO_PI = 2.0 * np.pi


class Buf:
    def __init__(self, name):
        self.name = name
        self.lw = None
        self.rd = []


ENGS = ["pe", "act", "dve", "pool", "sp"]
INORDER = {"sp", "pool"}
CRIT = 0
CP_PRIO = True
PA_CFG = {'a1': [0], 'a2': [1], 'a3': [1], 'a4': [0], 'b': [2], 'c1': [3], 'c2': [3], 'c3': [2]}
PE_SLOW = 1.05
SLAT = 0.2
XLAT = 0.4


def _free(ap):
    n = 1
    for s_ in list(ap.shape)[1:]:
        n *= int(s_)
    return n


def _est(eng, name, kw):
    try:
        if name in ("dma_start", "indirect_dma_start"):
            o = kw["in_"] if kw.get("out_offset", None) is not None else kw["out"]
            nbytes = int(o.shape[0]) * _free(o) * (2 if o.dtype == BF16 else 4)
            occ = 0.15 if eng in ("sp", "act") else 1.2
            return occ, 2.5 + nbytes / 150e3
        if name == "matmul":
            n = _free(kw["rhs"])
            f = 4 if kw["rhs"].dtype == F32 else 1
            d = max(64, n) * f / 2400.0 * PE_SLOW
            return d, d + 0.1
        if name == "transpose":
            f = 2 if kw["in_"].dtype == F32 else 1
            d = 128 * f / 2400.0 * PE_SLOW
            return d, d + 0.1
        if name == "activation":
            d = (_free(kw["in_"]) + 230) / 1400.0 + (0.1 if "accum_out" in kw else 0.0)
            return d, d
        n = _free(kw.get("out", kw.get("ap")))
        if name == "reciprocal":
            n *= 7
        if eng == "pool":
            d = 0.9 + n * 1.2 / 960.0
        else:
            d = 0.14 + n / 960.0
        return d, d
    except Exception:
        return 0.3, 0.3


class Sched:
    def __init__(self, nc, eng_sems, dma_sems):
        self.nc = nc
        self.eng_sems = eng_sems
        self.dma_pool = [(h, 0) for h in dma_sems]
        self.dma_used = []
        self.dma_map = {}
        self.dma_q = {}
        self.cnt = {e: 0 for e in ENGS}
        self.dma_cnt = {}
        self.dma_last = {}
        self.waited = {e: {} for e in ENGS}
        self.nodes = []
        self.base = 0
        self.tok = {}
        self.reorder = True

    def sem(self, key):
        if key[0] == "E":
            return self.eng_sems[key[1]]
        return self._sem_of[key[1]]

    def _node(self, eng, name, kw, reads, writes, dma_key=None):
        nid = self.base + len(self.nodes)
        deps = set()

        def add(d):
            if d is None or d < self.base:
                return
            deps.add(d)
            dk = self.nodes[d - self.base]["dma_key"]
            if dk is not None:
                deps.add(self.dma_last[dk])

        for b_ in reads:
            add(b_.lw)
        for b_ in writes:
            add(b_.lw)
            for r_ in b_.rd:
                add(r_)
        deps = {d for d in deps if d >= self.base}
        occ, lat = _est(eng, name, kw)
        self.nodes.append({"id": nid, "eng": eng, "name": name, "kw": kw, "deps": deps, "occ": occ, "lat": lat,
                           "dma_key": dma_key})
        for b_ in writes:
            b_.lw = nid
            b_.rd = []
        for b_ in reads:
            if b_.lw != nid:
                b_.rd.append(nid)
        return nid

    def op(self, eng, name, reads=(), writes=(), **kw):
        self._node(eng, name, kw, reads, writes)

    def dma(self, eng, sb, reads=(), writes=(), name="dma_start", **kw):
        bn = sb.name
        if bn not in self.dma_map:
            if eng == "sp" and self.dma_used:
                h, c0 = self.dma_used.pop()
            else:
                h, c0 = self.dma_pool.pop()
            self.dma_map[bn] = h
            self.dma_cnt[("D", bn)] = c0
            self.dma_q[bn] = eng
        assert self.dma_q[bn] == eng, "all DMAs touching one SBUF buffer must use one queue: " + bn
        k = ("D", bn)
        nid = self._node(eng, name, kw, reads, writes, dma_key=k)
        self.dma_last[k] = nid

    def _schedule(self):
        import heapq
        nodes = self.nodes
        n = len(nodes)
        succ = [[] for _ in range(n)]
        indeg = [0] * n
        for nd in nodes:
            i = nd["id"] - self.base
            for d in nd["deps"]:
                succ[d - self.base].append(i)
                indeg[i] += 1
        prio = list(range(n))
        if CP_PRIO:
            cp = [0.0] * n
            for i in range(n - 1, -1, -1):
                m = 0.0
                for s_ in succ[i]:
                    if cp[s_] > m:
                        m = cp[s_]
                cp[i] = m + nodes[i]["lat"]
            order_idx = sorted(range(n), key=lambda j: (-cp[j], j))
            for r_, j in enumerate(order_idx):
                prio[j] = r_
        fin = [0.0] * n
        why = [None] * n
        rdep = [None] * n
        last_on = {e: None for e in ENGS}
        ready_at = [0.0] * n
        free_at = {e: 0.0 for e in ENGS}
        waiting = {e: [] for e in ENGS}
        avail = {e: [] for e in ENGS}
        inorder_q = {e: [i for i in range(n) if nodes[i]["eng"] == e] for e in INORDER}
        inorder_pos = {e: 0 for e in INORDER}
        order = {e: [] for e in ENGS}
        for i in range(n):
            if indeg[i] == 0 and nodes[i]["eng"] not in INORDER:
                heapq.heappush(waiting[nodes[i]["eng"]], (0.0, i))
        done = 0
        while done < n:
            best = None
            for e in ENGS:
                if e in INORDER:
                    p = inorder_pos[e]
                    if p >= len(inorder_q[e]):
                        continue
                    i = inorder_q[e][p]
                    if indeg[i] > 0:
                        continue
                    stt = max(free_at[e], ready_at[i])
                    cand = (stt, i, e)
                else:
                    w, av = waiting[e], avail[e]
                    while w and w[0][0] <= free_at[e]:
                        j_ = heapq.heappop(w)[1]
                        heapq.heappush(av, (prio[j_], j_))
                    if av:
                        cand = (free_at[e], av[0][1], e)
                    elif w:
                        cand = (w[0][0], w[0][1], e)
                    else:
                        continue
                if best is None or cand < best:
                    best = cand
            assert best is not None, "scheduler deadlock"
            stt, i, e = best
            if e in INORDER:
                inorder_pos[e] += 1
            else:
                if avail[e] and avail[e][0][1] == i:
                    heapq.heappop(avail[e])
                else:
                    heapq.heappop(waiting[e])
            nd = nodes[i]
            why[i] = ("dep", rdep[i]) if (rdep[i] is not None and ready_at[i] >= free_at[e] - 1e-9) else ("eng", last_on[e])
            last_on[e] = i
            free_at[e] = stt + nd["occ"]
            fin[i] = stt + nd["lat"]
            order[e].append(i)
            done += 1
            for s_ in succ[i]:
                indeg[s_] -= 1
                lat = XLAT if nodes[s_]["eng"] != e else (0.0 if e == "pe" else SLAT)
                if fin[i] + lat > ready_at[s_]:
                    ready_at[s_] = fin[i] + lat
                    rdep[s_] = i
                if indeg[s_] == 0 and nodes[s_]["eng"] not in INORDER:
                    heapq.heappush(waiting[nodes[s_]["eng"]], (ready_at[s_], s_))
        self.est_us = max(fin) if n else 0.0
        if CRIT and n:
            i = max(range(n), key=lambda j: fin[j])
            path = []
            while i is not None and len(path) < 100000:
                path.append(i)
                i = why[i][1] if why[i] else None
            agg = {}
            for i in path:
                nd = nodes[i]
                o = nd["kw"].get("out", nd["kw"].get("ap"))
                key = (nd["eng"], nd["name"], str(getattr(getattr(o, "tensor", None), "name", "?")), why[i][0] if why[i] else "-")
                a_ = agg.setdefault(key, [0, 0.0])
                a_[0] += 1
                a_[1] += nd["lat"]
            print("  critical path (%d nodes):" % len(path))
            for k_, v_ in sorted(agg.items(), key=lambda kv: -kv[1][1])[:CRIT]:
                print("    %-60s n=%5d  t=%7.1f" % (k_, v_[0], v_[1]))
        self.est_busy = {e: sum(nodes[i]["occ"] for i in order[e]) for e in ENGS}
        return order

    def run_block(self, name=None):
        nodes = self.nodes
        n = len(nodes)
        if self.reorder:
            order = self._schedule()
        else:
            order = {e: [i for i in range(n) if nodes[i]["eng"] == e] for e in ENGS}
        tok = {}
        for e in ENGS:
            for i in order[e]:
                nd = nodes[i]
                if nd["dma_key"] is not None:
                    k = nd["dma_key"]
                    self.dma_cnt[k] = self.dma_cnt.get(k, 0) + 16
                    tok[i] = (k, self.dma_cnt[k], 16)
                else:
                    self.cnt[e] += 1
                    tok[i] = (("E", e), self.cnt[e], 1)
        prog = {e: [] for e in ENGS}
        for e in ENGS:
            wd = self.waited[e]
            for i in order[e]:
                nd = nodes[i]
                need = {}
                for d in nd["deps"]:
                    k, v, _ = tok[d - self.base]
                    if k == ("E", "pe") and e == "pe":
                        continue
                    if need.get(k, 0) < v:
                        need[k] = v
                for k, v in need.items():
                    if wd.get(k, 0) < v:
                        wd[k] = v
                        prog[e].append(("wait", k, v))
                prog[e].append(("inst", (nd["name"], nd["kw"]), tok[i][0], tok[i][2]))
        wd = self.waited["sp"]
        for k, v in self.dma_cnt.items():
            if wd.get(k, 0) < v:
                wd[k] = v
                prog["sp"].append(("wait", k, v))
        for e in ENGS:
            k, v = ("E", e), self.cnt[e]
            if e != "sp" and v > 0 and wd.get(k, 0) < v:
                wd[k] = v
                prog["sp"].append(("wait", k, v))
        self.base += n
        self.nodes = []
        nc = self.nc
        self._sem_of = dict(self.dma_map)
        clear_list = []
        for bn, h in self.dma_map.items():
            c_ = self.dma_cnt.pop(("D", bn))
            if self.dma_q[bn] == "sp":
                self.dma_used.append((h, c_))
            for e in ENGS:
                self.waited[e].pop(("D", bn), None)
        self.dma_map = {}
        self.dma_q = {}
        self.dma_last = {}

        def replay(lst, clear=()):
            def body(engine):
                for o in lst:
                    if o[0] == "wait":
                        engine.wait_ge(self.sem(o[1]), o[2])
                    else:
                        getattr(engine, o[1][0])(**o[1][1]).then_inc(self.sem(o[2]), o[3])
                for h in clear:
                    engine.sem_clear(h)
            return body

        with nc.Block() as block:
            block.tensor(replay(prog["pe"]))
            block.scalar(replay(prog["act"]))
            block.vector(replay(prog["dve"]))
            block.gpsimd(replay(prog["pool"]))
            block.sync(replay(prog["sp"], clear_list))
        if name:
            print("[sched] block %s: %d ops, est %.0f us, busy %s" % (name, n, getattr(self, "est_us", 0.0),
                  {e: int(v) for e, v in getattr(self, "est_busy", {}).items()}))


def build_program(debug=None):
    nc = bass.Bass("TRN2", target_bir_lowering=False)

    def din(name, shape, dt=F32):
        return nc.dram_tensor(name, list(shape), dt, kind="ExternalInput").ap()

    x = din("x", [S, D])
    pos = din("pos", [1, S], I32)
    g_attn = din("g_attn", [1, D])
    g_ffn = din("g_ffn", [1, D])
    g_fin = din("g_fin", [1, D])
    g_q = din("g_q", [1, 384])
    g_kv = din("g_kv", [1, 256])
    g_gla = din("g_gla", [1, 128])
    w_mla = din("w_mla", [D, 640])
    w_kr = din("w_kr", [D, 64])
    w_gf = din("w_gf", [D, 528])
    w_gt = din("w_gt", [D, 1280])
    w_uq = din("w_uq", [384, 8 * 128])
    w_ukn = din("w_ukn", [256, 512])
    w_uv = din("w_uv", [256, 512])
    gate_up = din("gate_up", [17, 256])
    w_out = din("w_out", [D, D])
    w_rt = din("w_rt", [D, 36])
    b_rt = din("b_rt", [1, 36])
    w_eg = din("w_eg", [NE, D, 256])
    w_eu = din("w_eu", [NE, D, 256])
    w_ed = din("w_ed", [NE, 256, D])
    c_ident = din("c_ident", [128, 128])
    c_rope = din("c_rope", [128, 2])
    c_tri = din("c_tri", [128, 4 * 128])
    c_gla = din("c_gla", [128, 256])
    c_ecap = din("c_ecap", [128, 32])
    out = nc.dram_tensor("out", [S, D], F32, kind="ExternalOutput").ap()
    dbg = None
    if debug:
        dbg = nc.dram_tensor("dbg", list(debug[:2]), F32, kind="ExternalOutput").ap()

    h1_d = nc.dram_tensor("h1_d", [S, D], F32).ap()
    xbuf_d = nc.dram_tensor("xbuf_d", [NSLOT, D], BF16).ap()
    ybuf_d = nc.dram_tensor("ybuf_d", [NSLOT, D], BF16).ap()
    wbf_d = nc.dram_tensor("wbf_d", [NE, 128, 6144], BF16).ap()

    from contextlib import ExitStack
    es = ExitStack()
    with es:
        eng_sems = {e: es.enter_context(nc.semaphore("sem_" + e)) for e in ENGS}
        dma_sems = [es.enter_context(nc.semaphore("dsem%d" % i)) for i in range(56)]
        sc = Sched(nc, eng_sems, dma_sems)

        def sb(stack, name, shape, dt):
            return stack.enter_context(nc.sbuf_tensor(name, list(shape), dt))

        def ps(stack, name, shape, dt):
            return stack.enter_context(nc.psum_tensor(name, list(shape), dt))

        ident_f = sb(es, "ident_f", [128, 128], F32)
        ident_b = sb(es, "ident_b", [128, 128], BF16)
        ymT = sb(es, "ymT", [128, NT, 512], BF16)
        B_ident = Buf("ident")
        B_ymT = Buf("ymT")
        sc.dma("sp", B_ident, writes=[B_ident], out=ident_f[:], in_=c_ident)
        sc.op("dve", "tensor_copy", out=ident_b[:], in_=ident_f[:], reads=[B_ident], writes=[B_ident])

        skip_front = bool(debug and len(debug) > 5 and debug[5])
        if skip_front:
            sc.op("dve", "memset", ap=ymT[:], constant=0.0, writes=[B_ymT])
        class TB:
            def __init__(self, t, name):
                self.t = t
                self.b = Buf(name)

        def mk(stack, name, shape, dt, n=None, psum=False):
            f = ps if psum else sb
            name = "m_" + name
            if n is None:
                return TB(f(stack, name, shape, dt), name)
            return [TB(f(stack, "%s%d" % (name, i), shape, dt), "%s%d" % (name, i)) for i in range(n)]

        def bl(lst):
            return [getattr(o, "b", o) for o in lst]

        def OP(eng, name, reads, writes, **kw):
            sc.op(eng, name, reads=bl(reads), writes=bl(writes), **kw)

        def DMA(eng, sbuf, reads, writes, name="dma_start", **kw):
            sc.dma(eng, getattr(sbuf, "b", sbuf), reads=bl(reads), writes=bl(writes), name=name, **kw)

        zero_t = sb(es, "zero_t", [128, D], BF16)
        B_zero = Buf("zero_t")
        es12 = ExitStack()
        with es12:
          if not skip_front:
              ctab = sb(es12, "ctab", [128, S], BF16)
              stab = sb(es12, "stab", [128, S], BF16)
              cnT = sb(es12, "cnT", [128, 5, S], BF16)
              krT = sb(es12, "krT", [128, S], BF16)
              B_tab = Buf("tab")
              wuq_sb = sb(es12, "wuq_sb", [128, 3, 1024], BF16)
              wukn_sb = sb(es12, "wukn_sb", [128, 2, 512], BF16)
              wuv_sb = sb(es12, "wuv_sb", [128, 2, 512], BF16)
              B_w2 = Buf("w2")
              B_cnT = Buf("cnT")
              B_krT = Buf("krT")

              e0 = ExitStack()
              if True:
                  ropec = sb(e0, "ropec", [128, 2], F32)
                  posi = sb(e0, "posi", [128, 1024], I32)
                  ang = sb(e0, "ang", [128, 1024], F32)
                  kf = sb(e0, "kf", [128, 1024], F32)
                  ki = sb(e0, "ki", [128, 1024], I32)
                  r1 = sb(e0, "r1", [128, 1024], F32)
                  tabt = [sb(e0, "tabt%d" % i, [128, 1024], BF16) for i in range(2)]
                  B_ropec, B_posi, B_ang, B_kf, B_ki, B_r1 = (Buf(n) for n in ["ropec", "posi", "ang", "kf", "ki", "r1"])
                  B_tabt = [Buf("tabt0"), Buf("tabt1")]
                  sc.dma("sp", B_ropec, writes=[B_ropec], out=ropec[:], in_=c_rope)
                  for g_ in range(4):
                      sc.dma("sp", B_posi, writes=[B_posi], out=posi[g_ * 32:(g_ + 1) * 32, :],
                             in_=pos[:, g_ * 1024:(g_ + 1) * 1024].partition_broadcast(32))
                  sc.op("dve", "tensor_copy", out=ang[:], in_=posi[:], reads=[B_posi], writes=[B_ang])
                  sc.op("dve", "tensor_scalar", out=ang[:], in0=ang[:], scalar1=ropec[:, 0:1], scalar2=None,
                        op0=ALU.mult, reads=[B_ang, B_ropec], writes=[B_ang])
                  for which in range(2):
                      shift = 0.0 if which == 0 else np.pi / 2.0
                      sc.op("dve", "tensor_scalar", out=kf[:], in0=ang[:], scalar1=shift, scalar2=1.0 / TWO_PI, op0=ALU.add, op1=ALU.mult,
                            reads=[B_ang], writes=[B_kf])
                      sc.op("dve", "tensor_copy", out=ki[:], in_=kf[:], reads=[B_kf], writes=[B_ki])
                      sc.op("dve", "tensor_copy", out=kf[:], in_=ki[:], reads=[B_ki], writes=[B_kf])
                      sc.op("dve", "scalar_tensor_tensor", out=r1[:], in0=kf[:], scalar=-TWO_PI, in1=ang[:],
                            op0=ALU.mult, op1=ALU.add, reads=[B_kf, B_ang], writes=[B_r1])
                      if which == 1:
                          sc.op("dve", "tensor_scalar", out=r1[:], in0=r1[:], scalar1=np.pi / 2.0, scalar2=None,
                                op0=ALU.add, reads=[B_r1], writes=[B_r1])
                      sc.op("dve", "tensor_scalar", out=kf[:], in0=r1[:], scalar1=np.pi, scalar2=-TWO_PI,
                            op0=ALU.is_gt, op1=ALU.mult, reads=[B_r1], writes=[B_kf])
                      sc.op("dve", "tensor_tensor", out=r1[:], in0=r1[:], in1=kf[:], op=ALU.add, reads=[B_r1, B_kf], writes=[B_r1])
                      sc.op("dve", "tensor_scalar", out=kf[:], in0=r1[:], scalar1=-np.pi, scalar2=TWO_PI,
                            op0=ALU.is_lt, op1=ALU.mult, reads=[B_r1], writes=[B_kf])
                      sc.op("dve", "tensor_tensor", out=r1[:], in0=r1[:], in1=kf[:], op=ALU.add, reads=[B_r1, B_kf], writes=[B_r1])
                      sc.op("dve", "tensor_scalar", out=r1[:], in0=r1[:], scalar1=3.1415925, scalar2=-3.1415925,
                            op0=ALU.min, op1=ALU.max, reads=[B_r1], writes=[B_r1])
                      if which == 0:
                          sc.op("act", "activation", out=tabt[0][:], in_=r1[:], func=AF.Sin, scale=ropec[:, 1:2],
                                reads=[B_r1, B_ropec], writes=[B_tabt[0]])
                      else:
                          sc.op("act", "activation", out=tabt[1][:], in_=r1[:], func=AF.Sin, reads=[B_r1], writes=[B_tabt[1]])
                      dst = stab if which == 0 else ctab
                      for g_ in range(4):
                          sc.dma("act", B_tabt[which], reads=[B_tabt[which]], writes=[B_tab], out=dst[64:96, g_ * 1024:(g_ + 1) * 1024],
                                 in_=tabt[which][g_ * 32:(g_ + 1) * 32, :])

              with ExitStack() as e1:
                  NXT = 4
                  xt = [sb(e1, "xt%d" % i, [128, D], F32) for i in range(NXT)]
                  B_xt = [Buf("xt%d" % i) for i in range(NXT)]
                  g1 = sb(e1, "g1", [128, D], F32)
                  gqk = sb(e1, "gqk", [128, 640], F32)
                  B_g = Buf("g1")
                  wm_sb = sb(e1, "wm_sb", [128, KC, 640], BF16)
                  wk_sb = sb(e1, "wk_sb", [128, KC, 64], BF16)
                  B_wm = Buf("wm_sb")
                  B_wk = Buf("wk_sb")
                  RS1 = 3
                  st = [sb(e1, "st%d" % i, [128, 8], F32) for i in range(RS1)]
                  B_st = [Buf("st%d" % i) for i in range(RS1)]
                  xs = [sb(e1, "xs%d" % i, [128, D], BF16) for i in range(RS1)]
                  B_xs = [Buf("xs%d" % i) for i in range(RS1)]
                  xnT = [sb(e1, "xnT%d" % i, [128, KC, 512], BF16) for i in range(2)]
                  B_xnT = [Buf("xnT%d" % i) for i in range(2)]
                  cn = [sb(e1, "cn%d" % i, [128, 640], BF16) for i in range(RS1)]
                  B_cn = [Buf("cn%d" % i) for i in range(RS1)]
                  kt1 = sb(e1, "kt1", [128, 512], F32)
                  kt2 = sb(e1, "kt2", [128, 512], F32)
                  B_kt = Buf("kt")
                  p_tp = ps(e1, "p_tp", [128, KC, 128], BF16)
                  p_mm = [ps(e1, "p_mm%d" % i, [128, 1024], F32) for i in range(2)]
                  p_tp2 = ps(e1, "p_tp2", [128, 5, 128], BF16)
                  p_kr = ps(e1, "p_kr", [128, 2, 512], F32)
                  B_ptp, B_ptp2, B_pkr = Buf("p_tp"), Buf("p_tp2"), Buf("p_kr")
                  B_pmm = [Buf("p_mm0"), Buf("p_mm1")]

                  sc.dma("sp", B_g, writes=[B_g], out=g1[:], in_=g_attn.partition_broadcast(128))
                  sc.dma("sp", B_g, writes=[B_g], out=gqk[:, 0:384], in_=g_q.partition_broadcast(128))
                  sc.dma("sp", B_g, writes=[B_g], out=gqk[:, 384:640], in_=g_kv.partition_broadcast(128))
                  for c in range(KC):
                      sc.dma("pool", B_wm, writes=[B_wm], out=wm_sb[:, c, :], in_=w_mla[c * 128:(c + 1) * 128, :])
                      sc.dma("pool", B_wk, writes=[B_wk], out=wk_sb[:, c, :], in_=w_kr[c * 128:(c + 1) * 128, :])

                  def load_w2():
                      for c in range(3):
                          sc.dma("pool", B_w2, reads=[B_cnT], writes=[B_w2], out=wuq_sb[:, c, :], in_=w_uq[c * 128:(c + 1) * 128, :])
                      for c in range(2):
                          sc.dma("pool", B_w2, reads=[B_cnT], writes=[B_w2], out=wukn_sb[:, c, :], in_=w_ukn[c * 128:(c + 1) * 128, :])
                          sc.dma("pool", B_w2, reads=[B_cnT], writes=[B_w2], out=wuv_sb[:, c, :], in_=w_uv[c * 128:(c + 1) * 128, :])

                  def load_x(i):
                      s_ = i % NXT
                      sc.dma("sp", B_xt[s_], writes=[B_xt[s_]], out=xt[s_][:], in_=x[i * 128:(i + 1) * 128, :])

                  load_x(0)
                  load_x(1)
                  load_x(2)
                  for i in range(NT):
                      if i + 3 < NT:
                          load_x(i + 3)
                      if i == 8:
                          load_w2()
                      s3, s2, sp_ = i % NXT, i % RS1, i % 2
                      blk, tb = i // 4, i % 4
                      xb = blk % 2
                      tcols = slice(tb * 128, (tb + 1) * 128)
                      gcols = slice(i * 128, (i + 1) * 128)
                      sc.op("act", "activation", out=xs[s2][:], in_=xt[s3][:], func=AF.Square, accum_out=st[s2][:, 0:1],
                            reads=[B_xt[s3]], writes=[B_st[s2], B_xs[s2]])
                      sc.op("act", "activation", out=st[s2][:, 1:2], in_=st[s2][:, 0:1], func=AF.Ln, scale=1.0 / D, bias=EPS,
                            reads=[B_st[s2]], writes=[B_st[s2]])
                      sc.op("act", "activation", out=st[s2][:, 2:3], in_=st[s2][:, 1:2], func=AF.Exp, scale=-0.5,
                            reads=[B_st[s2]], writes=[B_st[s2]])
                      sc.op("dve", "scalar_tensor_tensor", out=xs[s2][:], in0=xt[s3][:], scalar=st[s2][:, 2:3], in1=g1[:],
                                                                    op0=ALU.mult, op1=ALU.mult,
                            reads=[B_xt[s3], B_st[s2], B_g], writes=[B_xs[s2]])
                      for c in range(KC):
                          sc.op("pe", "transpose", out=p_tp[:, c, :], in_=xs[s2][:, c * 128:(c + 1) * 128], identity=ident_b[:],
                                reads=[B_xs[s2], B_ident], writes=[B_ptp])
                      sc.op("act", "activation", out=xnT[xb][:, :, tcols], in_=p_tp[:], func=AF.Copy,
                            reads=[B_ptp], writes=[B_xnT[xb]])
                      for c in range(KC):
                          sc.op("pe", "matmul", out=p_mm[sp_][:, 0:384], lhsT=xnT[xb][:, c, tcols], rhs=wm_sb[:, c, 0:384],
                                                              start=(c == 0), stop=(c == KC - 1),
                                reads=[B_xnT[xb], B_wm], writes=[B_pmm[sp_]])
                      for c in range(KC):
                          sc.op("pe", "matmul", out=p_mm[sp_][:, 512:768], lhsT=xnT[xb][:, c, tcols], rhs=wm_sb[:, c, 384:640],
                                                              start=(c == 0), stop=(c == KC - 1),
                                reads=[B_xnT[xb], B_wm], writes=[B_pmm[sp_]])
                      sc.op("act", "activation", out=cn[s2][:, 0:384], in_=p_mm[sp_][:, 0:384], func=AF.Square, accum_out=st[s2][:, 3:4],
                            reads=[B_pmm[sp_]], writes=[B_st[s2], B_cn[s2]])
                      sc.op("act", "activation", out=cn[s2][:, 384:640], in_=p_mm[sp_][:, 512:768], func=AF.Square, accum_out=st[s2][:, 4:5],
                            reads=[B_pmm[sp_]], writes=[B_st[s2], B_cn[s2]])
                      sc.op("act", "activation", out=st[s2][:, 5:6], in_=st[s2][:, 3:4], func=AF.Ln, scale=1.0 / 384, bias=EPS,
                            reads=[B_st[s2]], writes=[B_st[s2]])
                      sc.op("act", "activation", out=st[s2][:, 6:7], in_=st[s2][:, 4:5], func=AF.Ln, scale=1.0 / 256, bias=EPS,
                            reads=[B_st[s2]], writes=[B_st[s2]])
                      sc.op("act", "activation", out=st[s2][:, 5:7], in_=st[s2][:, 5:7], func=AF.Exp, scale=-0.5,
                            reads=[B_st[s2]], writes=[B_st[s2]])
                      sc.op("dve", "scalar_tensor_tensor", out=cn[s2][:, 0:384], in0=p_mm[sp_][:, 0:384], scalar=st[s2][:, 5:6],
                                                                    in1=gqk[:, 0:384], op0=ALU.mult, op1=ALU.mult,
                            reads=[B_pmm[sp_], B_st[s2], B_g], writes=[B_cn[s2]])
                      sc.op("dve", "scalar_tensor_tensor", out=cn[s2][:, 384:640], in0=p_mm[sp_][:, 512:768], scalar=st[s2][:, 6:7],
                                                                    in1=gqk[:, 384:640], op0=ALU.mult, op1=ALU.mult,
                            reads=[B_pmm[sp_], B_st[s2], B_g], writes=[B_cn[s2]])
                      for c in range(5):
                          sc.op("pe", "transpose", out=p_tp2[:, c, :], in_=cn[s2][:, c * 128:(c + 1) * 128], identity=ident_b[:],
                                reads=[B_cn[s2], B_ident], writes=[B_ptp2])
                      sc.op("dve", "tensor_copy", out=cnT[:, :, gcols], in_=p_tp2[:],
                            reads=[B_ptp2], writes=[B_cnT])
                      if tb == 3:
                          bcols = slice(blk * 512, (blk + 1) * 512)
                          for j in range(2):
                              for c in range(KC):
                                  sc.op("pe", "matmul", out=p_kr[64:96, j, :], lhsT=wk_sb[:, c, j * 32:(j + 1) * 32],
                                                                           rhs=xnT[xb][:, c, :], start=(c == 0), stop=(c == KC - 1),
                                        reads=[B_xnT[xb], B_wk], writes=[B_pkr])
                          P = slice(64, 96)
                          sc.op("dve", "tensor_tensor", out=kt1[P, :], in0=p_kr[P, 0, :], in1=ctab[P, bcols], op=ALU.mult,
                                reads=[B_pkr, B_tab], writes=[B_kt])
                          sc.op("dve", "tensor_tensor", out=kt2[P, :], in0=p_kr[P, 1, :], in1=stab[P, bcols], op=ALU.mult,
                                reads=[B_pkr, B_tab, B_kt], writes=[B_kt])
                          sc.op("dve", "tensor_tensor", out=krT[P, bcols], in0=kt1[P, :], in1=kt2[P, :], op=ALU.add,
                                reads=[B_kt], writes=[B_krT])
                  sc.run_block("p1")
              e0.close()

              with ExitStack() as e2:
                  SCALE = float(96 ** -0.5)
                  trif = sb(e2, "trif", [128, 128], F32)
                  trib = sb(e2, "trib", [128, 128], BF16)
                  ones_b = sb(e2, "ones_b", [128, 64], BF16)
                  B_c2 = Buf("c2")
                  QT = [sb(e2, "QT%d" % i, [128, S], BF16) for i in range(2)]
                  KT = [sb(e2, "KT%d" % i, [128, S], BF16) for i in range(2)]
                  VV = [sb(e2, "VV%d" % i, [128, NT, 65], BF16) for i in range(2)]
                  B_QT = [Buf("QT0"), Buf("QT1")]
                  B_KT = [Buf("KT0"), Buf("KT1")]
                  B_VV = [Buf("VV0"), Buf("VV1")]
                  NPT = 24
                  PT = [sb(e2, "PT%d" % i, [128, 512], BF16) for i in range(NPT)]
                  B_PT = [Buf("PT%d" % i) for i in range(NPT)]
                  qt1 = sb(e2, "qt1", [128, 512], F32)
                  qt2 = sb(e2, "qt2", [128, 512], F32)
                  B_qt = Buf("qt")
                  rr = [sb(e2, "rr%d" % i, [128, 8], F32) for i in range(2)]
                  B_rr = [Buf("rr0"), Buf("rr1")]
                  NST = 4
                  psT = [ps(e2, "psT%d" % i, [128, 512], F32) for i in range(NST)]
                  B_psT = [Buf("psT%d" % i) for i in range(NST)]
                  poT = [ps(e2, "poT%d" % i, [128, 512], F32) for i in range(2)]
                  B_poT = [Buf("poT0"), Buf("poT1")]
                  NBB = 2
                  pbb = [ps(e2, "pbb%d" % i, [128, 512], F32) for i in range(NBB)]
                  B_pbb = [Buf("pbb%d" % i) for i in range(NBB)]

                  sc.dma("sp", B_c2, writes=[B_c2], out=trif[:], in_=c_tri[:, 0:128])
                  sc.op("dve", "tensor_copy", out=trib[:], in_=trif[:], reads=[B_c2], writes=[B_c2])
                  sc.op("dve", "memset", ap=ones_b[:], constant=1.0, writes=[B_c2])
                  for i in range(2):
                      sc.op("dve", "memset", ap=VV[i][:, :, 64:65], constant=1.0, writes=[B_VV[i]])

                  sc.op("dve", "memset", ap=zero_t[:], constant=0.0, writes=[B_zero])
                  for r_ in range(NSLOT // 128):
                      sc.dma("sp", B_zero, reads=[B_zero], out=xbuf_d[r_ * 128:(r_ + 1) * 128, :], in_=zero_t[:])
                  st2 = {"bb": 0, "ps": 0, "pt": 0, "nq": 0}
                  P = slice(64, 96)

                  def build_jobs(h):
                      hb = h % 2
                      jobs = []
                      for b in range(8):
                          bc = slice(b * 512, (b + 1) * 512)

                          def jq(b=b, bc=bc):
                              k_ = st2["bb"] % NBB; st2["bb"] += 1
                              for c in range(3):
                                  sc.op("pe", "matmul", out=pbb[k_][0:96, :], lhsT=wuq_sb[:, c, h * 128:h * 128 + 96], rhs=cnT[:, c, bc],
                                        start=(c == 0), stop=(c == 2), reads=[B_w2, B_cnT], writes=[B_pbb[k_]])
                              sc.op("dve", "tensor_copy", out=QT[hb][0:64, bc], in_=pbb[k_][0:64, :],
                                    reads=[B_pbb[k_]], writes=[B_QT[hb]])
                              sc.op("dve", "tensor_tensor", out=qt1[P, :], in0=pbb[k_][P, :], in1=ctab[P, bc], op=ALU.mult,
                                    reads=[B_pbb[k_], B_tab], writes=[B_qt])
                              k2 = st2["bb"] % NBB; st2["bb"] += 1
                              for c in range(3):
                                  sc.op("pe", "matmul", out=pbb[k2][P, :], lhsT=wuq_sb[:, c, h * 128 + 96:h * 128 + 128], rhs=cnT[:, c, bc],
                                        start=(c == 0), stop=(c == 2), reads=[B_w2, B_cnT], writes=[B_pbb[k2]])
                              sc.op("dve", "tensor_tensor", out=qt2[P, :], in0=pbb[k2][P, :], in1=stab[P, bc], op=ALU.mult,
                                    reads=[B_pbb[k2], B_tab, B_qt], writes=[B_qt])
                              sc.op("dve", "tensor_tensor", out=QT[hb][P, bc], in0=qt1[P, :], in1=qt2[P, :], op=ALU.add,
                                    reads=[B_qt], writes=[B_QT[hb]])

                          def jk(b=b, bc=bc):
                              k_ = st2["bb"] % NBB; st2["bb"] += 1
                              for c in range(2):
                                  sc.op("pe", "matmul", out=pbb[k_][0:64, :], lhsT=wukn_sb[:, c, h * 64:(h + 1) * 64], rhs=cnT[:, 3 + c, bc],
                                        start=(c == 0), stop=(c == 1), reads=[B_w2, B_cnT], writes=[B_pbb[k_]])
                              sc.op("dve", "tensor_copy", out=KT[hb][0:64, bc], in_=pbb[k_][0:64, :],
                                    reads=[B_pbb[k_]], writes=[B_KT[hb]])
                              sc.op("pool", "tensor_copy", out=KT[hb][P, bc], in_=krT[P, bc], reads=[B_krT], writes=[B_KT[hb]])

                          def jv(b=b, bc=bc):
                              k_ = st2["bb"] % NBB; st2["bb"] += 1
                              for t in range(4):
                                  tcs = slice(b * 512 + t * 128, b * 512 + (t + 1) * 128)
                                  for c in range(2):
                                      sc.op("pe", "matmul", out=pbb[k_][:, t * 64:(t + 1) * 64], lhsT=cnT[:, 3 + c, tcs],
                                            rhs=wuv_sb[:, c, h * 64:(h + 1) * 64], start=(c == 0), stop=(c == 1),
                                            reads=[B_w2, B_cnT], writes=[B_pbb[k_]])
                              sc.op("dve", "tensor_copy", out=VV[hb][:, b * 4:(b + 1) * 4, 0:64],
                                    in_=pbb[k_][:, 0:256].rearrange("p (t d) -> p t d", t=4),
                                    reads=[B_pbb[k_]], writes=[B_VV[hb]])

                          jobs += [jq, jk, jv]
                      return jobs

                  def attention(h, side_jobs):
                      hb = h % 2
                      iters = []
                      for qb in range(8):
                          for kt in range(4 * qb + 4):
                              iters.append((qb, kt))
                      nside = len(side_jobs)
                      every = max(1, len(iters) // (nside + 1)) if nside else 0
                      for n, (qb, kt) in enumerate(iters):
                          j = kt - 4 * qb
                          c0 = max(j, 0) * 128
                          k_ = st2["ps"] % NST; st2["ps"] += 1
                          sc.op("pe", "matmul", out=psT[k_][:, c0:512], lhsT=KT[hb][0:96, kt * 128:(kt + 1) * 128],
                                rhs=QT[hb][0:96, qb * 512 + c0:(qb + 1) * 512], start=True, stop=True,
                                reads=[B_KT[hb], B_QT[hb]], writes=[B_psT[k_]])
                          r = st2["pt"] % NPT; st2["pt"] += 1
                          sc.op("act", "activation", out=PT[r][:, c0:512], in_=psT[k_][:, c0:512], func=AF.Exp, scale=SCALE,
                                reads=[B_psT[k_]], writes=[B_PT[r]])
                          if kt >= 4 * qb:
                              sc.op("dve", "tensor_tensor", out=PT[r][:, c0:c0 + 128], in0=PT[r][:, c0:c0 + 128], in1=trib[:], op=ALU.mult,
                                    reads=[B_PT[r], B_c2], writes=[B_PT[r]])
                          pb = qb % 2
                          for t in range(c0 // 128, 4):
                              sc.op("pe", "matmul", out=poT[pb][:, t * 128:t * 128 + 65], lhsT=PT[r][:, t * 128:(t + 1) * 128], rhs=VV[hb][:, kt, 0:65],
                                    start=(kt == 0 and t == 0), stop=(kt == 4 * qb + 3 and t == 3), skip_group_check=True,
                                    reads=[B_VV[hb], B_PT[r]], writes=[B_poT[pb]])
                          if kt == 4 * qb + 3:
                              q2 = st2["nq"] % 2; st2["nq"] += 1
                              pv = poT[pb][:].rearrange("p (t c) -> p t c", t=4)
                              sc.op("dve", "reciprocal", out=rr[q2][:, 0:4], in_=pv[:, :, 64], reads=[B_poT[pb]], writes=[B_rr[q2]])
                              for t in range(4):
                                  sc.op("dve", "tensor_scalar", out=ymT[:, qb * 4 + t, h * 64:(h + 1) * 64], in0=poT[pb][:, t * 128:t * 128 + 64],
                                        scalar1=rr[q2][:, t:t + 1], scalar2=None, op0=ALU.mult, reads=[B_poT[pb], B_rr[q2]], writes=[B_ymT])
                          if nside and n % every == every - 1 and side_jobs:
                              side_jobs.pop(0)()
                      while side_jobs:
                          side_jobs.pop(0)()

                  B_wconv = Buf("wconv")

                  def conv_job(e):
                      def job():
                          gu = wbf_d[e][:, 0:4096].rearrange("p (c n) -> p c n", n=512)
                          DMA("pool", B_wconv, [], [], out=gu[:, :, 0:256], in_=w_eg[e].rearrange("(p c) n -> p c n", p=128))
                          DMA("pool", B_wconv, [], [], out=gu[:, :, 256:512], in_=w_eu[e].rearrange("(p c) n -> p c n", p=128))
                          DMA("pool", B_wconv, [], [], out=wbf_d[e][:, 4096:6144].rearrange("p (c n) -> p c n", n=1024),
                              in_=w_ed[e].rearrange("(p c) n -> p c n", p=128))
                      return job

                  NH = 8 if not (debug and len(debug) > 2) else debug[2]
                  for j in build_jobs(0):
                      j()
                  for h in range(NH):
                      side = build_jobs(h + 1) if h + 1 < NH else []
                      convs = [conv_job(e) for e in range(4 * h, 4 * h + 4)] if NH == 8 else []
                      merged = []
                      while side or convs:
                          for _ in range(6):
                              if side:
                                  merged.append(side.pop(0))
                          if convs:
                              merged.append(convs.pop(0))
                      attention(h, merged)
                  sc.run_block("p2")

        stage = debug[3] if (debug and len(debug) > 3) else 7
        route_i = mk(es, "route_i", [128, NT, 2], I32)
        route_g = mk(es, "route_g", [128, NT, 2], F32)
        B_xbuf, B_ybuf, B_h1d = Buf("xbuf"), Buf("ybuf"), Buf("h1d")

        with ExitStack() as e3:
            NTILE = NT if not (debug and len(debug) > 4) else debug[4]
            wgf = mk(e3, "wgf", [128, KC, 528], BF16)
            wgt = mk(e3, "wgt", [128, KC, 1280], BF16)
            wo = mk(e3, "wo", [128, KC, D], BF16)
            wrt = mk(e3, "wrt", [128, KC, 36], F32)
            brt = mk(e3, "brt", [1, 36], F32)
            gup = mk(e3, "gup", [32, 256], F32)
            g1 = mk(e3, "g1", [128, D], F32)
            g2 = mk(e3, "g2", [128, D], F32)
            ggl = mk(e3, "ggl", [128, 512], F32)
            cgla = mk(e3, "cgla", [128, 256], F32)
            ctri = mk(e3, "ctri", [128, 512], F32)
            gmask = mk(e3, "gmask", [128, 512], BF16)
            ecap = mk(e3, "ecap", [128, 32], F32)
            ones_f = mk(e3, "ones_f", [128, 128], F32)
            jfr = mk(e3, "jfr", [128, 32], F32, 4)
            jfc = {"i": 0}

            def nextjf():
                jfc["i"] += 1
                return jfr[jfc["i"] % 4]
            NX = debug[10] if (debug and len(debug) > 10) else 5
            R2N = debug[9] if (debug and len(debug) > 9) else 2
            RBIG = 2
            xt = mk(e3, "xt", [128, D], F32, NX)
            st = mk(e3, "st", [128, 8], F32, R2N)
            xs = mk(e3, "xs", [128, D], BF16, R2N)
            xnT = mk(e3, "xnT", [128, KC, 128], BF16, R2N)
            R3 = 3
            v_sb = mk(e3, "v_sb", [128, 512], BF16, R3)
            gk_sb = mk(e3, "gk_sb", [128, 256], F32, R3)
            qk_sb = mk(e3, "qk_sb", [128, 4, 128], F32, R3)
            gmul = mk(e3, "gmul", [128, 512], F32, R3)
            glrT = mk(e3, "glrT", [32, 128], F32, R3)
            sg = mk(e3, "sg", [128, 512], F32)
            ez = mk(e3, "ez", [128, 256], F32)
            sp_sb = mk(e3, "sp_sb", [128, 256], F32, R2N)
            Eq = mk(e3, "Eq", [128, 2, 128], F32, R2N)
            Ek = mk(e3, "Ek", [128, 2, 128], F32, R2N)
            Er = mk(e3, "Er", [128, 256], F32, R2N)
            dec = mk(e3, "dec", [128, 2, 2], F32, R2N)
            qeT = mk(e3, "qeT", [128, 2, 128], BF16, R2N)
            qbd = mk(e3, "qbd", [128, 2, 256], BF16, R2N)
            keT = mk(e3, "keT", [128, 2, 128], BF16, R2N)
            kdz = mk(e3, "kdz", [128, 2, 256], BF16, R2N)
            attm = mk(e3, "attm", [128, 512], BF16, R2N)
            S32 = mk(e3, "S32", [128, 2, 128], F32)
            Sb = mk(e3, "Sb", [128, 2, 256], BF16)
            so = mk(e3, "so", [128, 8], F32, R2N)
            yg = mk(e3, "yg", [128, 512], BF16, R2N)
            ygT = mk(e3, "ygT", [128, 4, 128], BF16, R2N)
            ymTt = mk(e3, "ymTt", [128, 4, 128], BF16, R2N)
            h1s = mk(e3, "h1s", [128, D], F32, RBIG)
            xn2 = mk(e3, "xn2", [128, D], F32, RBIG)
            xn2b = mk(e3, "xn2b", [128, D], BF16, RBIG)
            xn2T = mk(e3, "xn2T", [128, KC, 128], F32)
            lg = mk(e3, "lg", [128, 36], F32, R2N)
            rs = mk(e3, "rs", [128, 16], F32, 2)
            mgt = mk(e3, "mgt", [128, 4], F32, R2N)
            els = mk(e3, "els", [128, 8], F32, R2N)
            els2 = mk(e3, "els2", [128, 8], F32, R2N)
            mk1 = mk(e3, "mk1", [128, 8], F32, R2N)
            mk2 = mk(e3, "mk2", [128, 8], F32, R2N)
            A1 = mk(e3, "A1", [128, 32], F32, R2N)
            A2 = mk(e3, "A2", [128, 32], F32, R2N)
            AA = mk(e3, "AA", [128, 32], F32, R2N)
            posc = mk(e3, "posc", [128, 32], F32, R2N)
            Rr = mk(e3, "Rr", [128, 32], F32)
            p_tp = mk(e3, "p_tp", [128, KC, 128], BF16, psum=True)
            NPA = 4
            pa = mk(e3, "pa", [128, 512], F32, NPA, psum=True)
            po = mk(e3, "po", [128, 512], F32, psum=True)
            pu = mk(e3, "pu", [128, 2, 2, 128], F32, psum=True)
            pmix = mk(e3, "pmix", [128, 512], F32, psum=True)
            pyT = TB(pmix.t[:, 0:256].bitcast(BF16).rearrange("p (m t) -> p m t", m=4), "pyT")
            psm = TB(pmix.t[:, 256:384], "psm")
            pyT.b = pmix.b
            psm.b = pmix.b

            for c in range(KC):
                r = slice(c * 128, (c + 1) * 128)
                DMA("pool", wgt, [], [wgt], out=wgt.t[:, c, :], in_=w_gt[r, :])
                DMA("pool", wgf, [], [wgf], out=wgf.t[:, c, :], in_=w_gf[r, :])
            for c in range(KC):
                r = slice(c * 128, (c + 1) * 128)
                DMA("pool", wo, [], [wo], out=wo.t[:, c, :], in_=w_out[r, :])
            for c in range(KC):
                r = slice(c * 128, (c + 1) * 128)
                DMA("sp", wrt, [], [wrt], out=wrt.t[:, c, :], in_=w_rt[r, :])
            DMA("sp", brt, [], [brt], out=brt.t[:], in_=b_rt)
            OP("dve", "memset", [], [gup], ap=gup.t[:], constant=0.0)
            DMA("sp", gup, [], [gup], out=gup.t[0:17, :], in_=gate_up)
            DMA("sp", g1, [], [g1], out=g1.t[:], in_=g_attn.partition_broadcast(128))
            DMA("sp", g2, [], [g2], out=g2.t[:], in_=g_ffn.partition_broadcast(128))
            for hh in range(4):
                DMA("sp", ggl, [], [ggl], out=ggl.t[:, hh * 128:(hh + 1) * 128], in_=g_gla.partition_broadcast(128))
            DMA("sp", cgla, [], [cgla], out=cgla.t[:], in_=c_gla)
            DMA("sp", ctri, [], [ctri], out=ctri.t[:], in_=c_tri)
            DMA("sp", ecap, [], [ecap], out=ecap.t[:], in_=c_ecap)
            OP("dve", "memset", [], [ones_f], ap=ones_f.t[:], constant=1.0)
            OP("dve", "memset", [], [S32], ap=S32.t[:], constant=0.0)
            OP("dve", "memset", [], [Sb], ap=Sb.t[:], constant=0.0)
            OP("dve", "memset", [], [Rr], ap=Rr.t[:], constant=0.0)
            for k_ in range(R3):
                OP("dve", "memset", [], [glrT[k_]], ap=glrT[k_].t[:], constant=1.0)
            for k_ in range(R2N):
                OP("dve", "memset", [], [qbd[k_]], ap=qbd[k_].t[:], constant=0.0)
                OP("dve", "memset", [], [kdz[k_]], ap=kdz[k_].t[:], constant=0.0)
            cnt = {"pa": 0}

            pac = {}

            def nextpa(tag="a"):
                g_ = PA_CFG.get(tag, PA_CFG.get(tag[0]))
                key = tuple(g_)
                k_ = pac.get(key, 0)
                pac[key] = k_ + 1
                return pa[g_[k_ % len(g_)]]

            def load_x(i):
                X = xt[i % NX]
                DMA("sp", X, [], [X], out=X.t[:], in_=x[i * 128:(i + 1) * 128, :])

            LN8 = float(np.log(0.125))

            def s1a(i):
                X, ST, XS, XT = xt[i % NX], st[i % R2N], xs[i % R2N], xnT[i % R2N]
                r3 = i % R3
                OP("act", "activation", [X], [ST, XS], out=XS.t[:], in_=X.t[:], func=AF.Square, accum_out=ST.t[:, 0:1])
                OP("act", "activation", [ST], [ST], out=ST.t[:, 1:2], in_=ST.t[:, 0:1], func=AF.Ln, scale=1.0 / D, bias=EPS)
                OP("act", "activation", [ST], [ST], out=ST.t[:, 2:3], in_=ST.t[:, 1:2], func=AF.Exp, scale=-0.5)
                OP("dve", "scalar_tensor_tensor", [X, ST, g1], [XS], out=XS.t[:], in0=X.t[:], scalar=ST.t[:, 2:3], in1=g1.t[:],
                   op0=ALU.mult, op1=ALU.mult)
                for c in range(KC):
                    OP("pe", "transpose", [XS, B_ident], [p_tp], out=p_tp.t[:, c, :], in_=XS.t[:, c * 128:(c + 1) * 128], identity=ident_b[:])
                OP("act", "activation", [p_tp], [XT], out=XT.t[:], in_=p_tp.t[:], func=AF.Copy)
                A = nextpa("a1")
                for c in range(KC):
                    OP("pe", "matmul", [XT, wgt], [A], out=A.t[:, 0:256], lhsT=XT.t[:, c, :], rhs=wgt.t[:, c, 0:256],
                       start=(c == 0), stop=(c == KC - 1))
                for c in range(KC):
                    OP("pe", "matmul", [XT, wgf], [A], out=A.t[0:16, 256:384], lhsT=wgf.t[:, c, 512:528], rhs=XT.t[:, c, :],
                       start=(c == 0), stop=(c == KC - 1))
                OP("act", "activation", [A], [gk_sb[r3]], out=gk_sb[r3].t[:], in_=A.t[:, 0:256], func=AF.Copy)
                OP("act", "activation", [A], [glrT[r3]], out=glrT[r3].t[0:16, :], in_=A.t[0:16, 256:384], func=AF.Copy)
                A = nextpa("a2")
                for c in range(KC):
                    OP("pe", "matmul", [XT, wgt], [A], out=A.t[:], lhsT=XT.t[:, c, :], rhs=wgt.t[:, c, 256:768],
                       start=(c == 0), stop=(c == KC - 1))
                OP("act", "activation", [A], [v_sb[r3]], out=v_sb[r3].t[:], in_=A.t[:], func=AF.Copy)
                A = nextpa("a3")
                for c in range(KC):
                    OP("pe", "matmul", [XT, wgt], [A], out=A.t[:], lhsT=XT.t[:, c, :], rhs=wgt.t[:, c, 768:1280],
                       start=(c == 0), stop=(c == KC - 1))
                OP("act", "activation", [A], [sg], out=sg.t[:], in_=A.t[:], func=AF.Exp, scale=-1.0)
                OP("act", "activation", [sg], [sg], out=sg.t[:], in_=sg.t[:], func=AF.Ln, bias=1.0)
                OP("act", "activation", [sg], [sg], out=sg.t[:], in_=sg.t[:], func=AF.Exp, scale=-1.0)
                OP("dve", "tensor_tensor", [A, sg], [sg], out=sg.t[:], in0=A.t[:], in1=sg.t[:], op=ALU.mult)
                OP("dve", "tensor_tensor", [sg, ggl], [gmul[r3]], out=gmul[r3].t[:], in0=sg.t[:], in1=ggl.t[:], op=ALU.mult)
                A = nextpa("a4")
                for m in range(4):
                    for c in range(KC):
                        OP("pe", "matmul", [XT, wgf], [A], out=A.t[:, m * 128:(m + 1) * 128], lhsT=wgf.t[:, c, m * 128:(m + 1) * 128],
                           rhs=XT.t[:, c, :], start=(c == 0), stop=(c == KC - 1))
                OP("act", "activation", [A], [qk_sb[r3]], out=qk_sb[r3].t[:], in_=A.t[:].rearrange("p (m t) -> p m t", m=4), func=AF.Copy)

            sub = debug[6] if (debug and len(debug) > 6) else 9

            def s1b(i):
                r3, r2 = i % R3, i % R2N
                A = nextpa("b1")
                OP("pe", "matmul", [glrT[r3], gup], [A], out=A.t[:, 0:256], lhsT=glrT[r3].t[0:17, :], rhs=gup.t[0:17, :], start=True, stop=True)
                OP("act", "activation", [A], [ez], out=ez.t[:], in_=A.t[:, 0:256], func=AF.Exp, scale=-1.0)
                OP("act", "activation", [ez], [sp_sb[r2]], out=sp_sb[r2].t[:], in_=ez.t[:], func=AF.Ln, bias=1.0)
                if sub < 2:
                    return
                OP("pe", "matmul", [sp_sb[r2], cgla], [A], out=A.t[:, 256:512], lhsT=cgla.t[:, 128:256], rhs=sp_sb[r2].t[:], start=True, stop=True)
                Bk = nextpa("b2")
                for m in range(2):
                    OP("pe", "matmul", [sp_sb[r2], cgla], [Bk], out=Bk.t[:, m * 128:(m + 1) * 128], lhsT=sp_sb[r2].t[:, m * 128:(m + 1) * 128],
                       rhs=cgla.t[:, 0:128], start=True, stop=True)
                bT = Bk.t[:, 0:256].rearrange("p (m t) -> p m t", m=2)
                if sub < 3:
                    return
                OP("act", "activation", [A], [Er[r2]], out=Er[r2].t[:], in_=A.t[:, 256:512], func=AF.Exp)
                OP("act", "activation", [Bk], [Eq[r2]], out=Eq[r2].t[:], in_=bT, func=AF.Exp, bias=LN8)
                OP("act", "activation", [Bk], [Ek[r2]], out=Ek[r2].t[:], in_=bT, func=AF.Exp, scale=-1.0)
                if sub < 4:
                    return
                bT4 = Bk.t[:, 0:256].rearrange("p (m c j) -> p m c j", m=2, c=2)
                OP("act", "activation", [Bk], [dec[r2]], out=dec[r2].t[:], in_=bT4[:, :, :, 63], func=AF.Exp)
                if sub < 5:
                    return
                OP("dve", "tensor_tensor", [qk_sb[r3], Eq[r2]], [qeT[r2]], out=qeT[r2].t[:], in0=qk_sb[r3].t[:, 0:2, :], in1=Eq[r2].t[:], op=ALU.mult)
                OP("dve", "tensor_tensor", [qk_sb[r3], Ek[r2]], [keT[r2]], out=keT[r2].t[:], in0=qk_sb[r3].t[:, 2:4, :], in1=Ek[r2].t[:], op=ALU.mult)
                OP("dve", "tensor_tensor", [qk_sb[r3], Eq[r2]], [qbd[r2]], out=qbd[r2].t[0:64, :, 0:128], in0=qk_sb[r3].t[0:64, 0:2, :], in1=Eq[r2].t[0:64, :, :], op=ALU.mult)
                OP("dve", "tensor_tensor", [qk_sb[r3], Eq[r2]], [qbd[r2]], out=qbd[r2].t[64:128, :, 128:256], in0=qk_sb[r3].t[64:128, 0:2, :], in1=Eq[r2].t[64:128, :, :], op=ALU.mult)
                OP("dve", "tensor_tensor", [gk_sb[r3], Er[r2]], [kdz[r2]], out=kdz[r2].t[0:64, 0, :], in0=gk_sb[r3].t[0:64, :], in1=Er[r2].t[0:64, :], op=ALU.mult)
                OP("dve", "tensor_tensor", [gk_sb[r3], Er[r2]], [kdz[r2]], out=kdz[r2].t[64:128, 1, :], in0=gk_sb[r3].t[64:128, :], in1=Er[r2].t[64:128, :], op=ALU.mult)
                if sub < 6:
                    return
                Ck = nextpa("b3")
                for hp in range(2):
                    OP("pe", "matmul", [keT[r2], qbd[r2]], [Ck], out=Ck.t[:, hp * 256:(hp + 1) * 256], lhsT=keT[r2].t[:, hp, :], rhs=qbd[r2].t[:, hp, :],
                       start=True, stop=True)
                OP("dve", "tensor_tensor", [Ck, gmask], [attm[r2]], out=attm[r2].t[:], in0=Ck.t[:], in1=gmask.t[:], op=ALU.mult)

            def s2(i):
                r3, r2 = i % R3, i % R2N
                for cc in range(2):
                    for hh in range(4):
                        pb_ = slice((hh % 2) * 64, (hh % 2) * 64 + 64)
                        OP("pe", "matmul", [kdz[r2], v_sb[r3]], [pu], out=pu.t[pb_, cc, hh // 2, :], lhsT=kdz[r2].t[:, cc, hh * 64:(hh + 1) * 64],
                           rhs=v_sb[r3].t[:, hh * 128:(hh + 1) * 128], start=True, stop=True)
                for hh in range(4):
                    OP("pe", "matmul", [attm[r2], v_sb[r3]], [po], out=po.t[:, hh * 128:(hh + 1) * 128], lhsT=attm[r2].t[:, hh * 128:(hh + 1) * 128],
                       rhs=v_sb[r3].t[:, hh * 128:(hh + 1) * 128], start=(hh == 0), stop=False, skip_group_check=True)
                for cc in range(2):
                    cs_ = slice(cc * 64, cc * 64 + 64)
                    for hp in range(2):
                        OP("pe", "matmul", [qeT[r2], Sb], [po], out=po.t[cs_, hp * 256:(hp + 1) * 256], lhsT=qeT[r2].t[:, hp, cs_],
                           rhs=Sb.t[:, hp, :], start=False, stop=(cc == 1 and hp == 1), skip_group_check=True)
                    for hp in range(2):
                        OP("dve", "scalar_tensor_tensor", [S32, dec[r2], pu], [S32], out=S32.t[:, hp, :], in0=S32.t[:, hp, :],
                           scalar=dec[r2].t[:, hp, cc:cc + 1], in1=pu.t[:, cc, hp, :], op0=ALU.mult, op1=ALU.add)
                    OP("act", "activation", [S32], [Sb], out=Sb.t[0:64, :, 0:128], in_=S32.t[0:64, :, :], func=AF.Copy)
                    OP("dve", "tensor_copy", [S32], [Sb], out=Sb.t[64:128, :, 128:256], in_=S32.t[64:128, :, :])
                SO = so[r2]
                for hh in range(4):
                    OP("act", "activation", [po], [SO, yg[r2]], out=yg[r2].t[:, hh * 128:(hh + 1) * 128], in_=po.t[:, hh * 128:(hh + 1) * 128], func=AF.Square,
                       accum_out=SO.t[:, hh:hh + 1])
                OP("act", "activation", [SO], [SO], out=SO.t[:, 4:8], in_=SO.t[:, 0:4], func=AF.Ln, scale=1.0 / 128, bias=EPS)
                OP("act", "activation", [SO], [SO], out=SO.t[:, 4:8], in_=SO.t[:, 4:8], func=AF.Exp, scale=-0.5)
                for hh in range(4):
                    hs_ = slice(hh * 128, (hh + 1) * 128)
                    OP("dve", "scalar_tensor_tensor", [po, SO, gmul[r3]], [yg[r2]], out=yg[r2].t[:, hs_], in0=po.t[:, hs_],
                       scalar=SO.t[:, 4 + hh:5 + hh], in1=gmul[r3].t[:, hs_], op0=ALU.mult, op1=ALU.mult)
                for hh in range(4):
                    OP("pe", "transpose", [yg[r2], B_ident], [pyT], out=pyT.t[:, hh, :], in_=yg[r2].t[:, hh * 128:(hh + 1) * 128], identity=ident_b[:])
                OP("act", "activation", [pyT], [ygT[r2]], out=ygT[r2].t[:], in_=pyT.t[:], func=AF.Copy)

            def s3(i):
                r2 = i % R2N
                rb = i % RBIG
                X = xt[i % NX]
                gc = slice(i * 128, (i + 1) * 128)
                H = h1s[rb]
                A = nextpa("c1")
                ymv = A.t[:, 0:256].bitcast(BF16).rearrange("p (m t) -> p m t", m=4)
                for c in range(4):
                    OP("pe", "transpose", [B_ymT, B_ident], [A], out=ymv[:, c, :], in_=ymT[:, i, c * 128:(c + 1) * 128], identity=ident_b[:])
                OP("act", "activation", [A], [ymTt[r2]], out=ymTt[r2].t[:], in_=ymv, func=AF.Copy)
                for half in range(2):
                    hc = slice(half * 512, (half + 1) * 512)
                    A = nextpa("c2")
                    for c in range(4):
                        OP("pe", "matmul", [ygT[r2], wo], [A], out=A.t[:], lhsT=ygT[r2].t[:, c, :], rhs=wo.t[:, c, hc], start=(c == 0), stop=False)
                    for c in range(4):
                        OP("pe", "matmul", [ymTt[r2], wo], [A], out=A.t[:], lhsT=ymTt[r2].t[:, c, :], rhs=wo.t[:, 4 + c, hc], start=False, stop=(c == 3))
                    OP("dve", "tensor_tensor", [A, X], [H], out=H.t[:, hc], in0=A.t[:], in1=X.t[:, hc], op=ALU.add)
                DMA("sp", H, [H], [], out=h1_d[gc, :], in_=H.t[:])
                if stage < 4:
                    return
                ST = st[r2]
                OP("act", "activation", [H], [ST, xn2b[rb]], out=xn2b[rb].t[:], in_=H.t[:], func=AF.Square, accum_out=ST.t[:, 4:5])
                OP("act", "activation", [ST], [ST], out=ST.t[:, 5:6], in_=ST.t[:, 4:5], func=AF.Ln, scale=1.0 / D, bias=EPS)
                OP("act", "activation", [ST], [ST], out=ST.t[:, 6:7], in_=ST.t[:, 5:6], func=AF.Exp, scale=-0.5)
                XN = xn2[rb]
                OP("dve", "scalar_tensor_tensor", [H, ST, g2], [XN], out=XN.t[:], in0=H.t[:], scalar=ST.t[:, 6:7], in1=g2.t[:],
                   op0=ALU.mult, op1=ALU.mult)
                OP("pool", "tensor_copy", [XN], [xn2b[rb]], out=xn2b[rb].t[:], in_=XN.t[:])
                for half in range(2):
                    A = nextpa("c3")
                    for c in range(4):
                        cc = half * 4 + c
                        OP("pe", "transpose", [XN, B_ident], [A], out=A.t[:, c * 128:(c + 1) * 128], in_=XN.t[:, cc * 128:(cc + 1) * 128], identity=ident_f[:])
                    OP("act", "activation", [A], [xn2T], out=xn2T.t[:, half * 4:(half + 1) * 4, :], in_=A.t[:].rearrange("p (m t) -> p m t", m=4), func=AF.Copy)
                for c in range(KC):
                    OP("pe", "matmul", [xn2T, wrt], [psm], out=psm.t[:, 0:36], lhsT=xn2T.t[:, c, :], rhs=wrt.t[:, c, :], start=(c == 0), stop=False)
                OP("pe", "matmul", [ones_f, brt], [psm], out=psm.t[:, 0:36], lhsT=ones_f.t[0:1, :], rhs=brt.t[0:1, :], start=False, stop=True)
                L, RS, MG = lg[r2], rs[r2], mgt[r2]
                OP("dve", "tensor_copy", [psm], [L], out=L.t[:], in_=psm.t[:, 0:36])
                OP("dve", "tensor_reduce", [L], [RS], out=RS.t[:, 0:1], in_=L.t[:, 0:4], axis=AX.X, op=ALU.max)
                OP("dve", "tensor_scalar", [L, RS], [MG], out=MG.t[:], in0=L.t[:, 0:4], scalar1=RS.t[:, 0:1], scalar2=None, op0=ALU.is_equal)
                OP("dve", "tensor_scalar", [RS], [RS], out=RS.t[:, 1:2], in0=RS.t[:, 0:1], scalar1=-1.0, scalar2=None, op0=ALU.mult)
                JF = nextjf()
                OP("act", "activation", [L, RS], [RS, JF], out=JF.t[:, 0:4], in_=L.t[:, 0:4], func=AF.Exp, bias=RS.t[:, 1:2], accum_out=RS.t[:, 2:3])
                OP("dve", "reciprocal", [RS], [RS], out=RS.t[:, 3:4], in_=RS.t[:, 2:3])
                E_, E2, M1, M2 = els[r2], els2[r2], mk1[r2], mk2[r2]
                OP("dve", "tensor_scalar", [L, MG], [E_], out=E_.t[:], in0=L.t[:, 4:12], scalar1=MG.t[:, 0:1], scalar2=None, op0=ALU.mult)
                for g_ in range(1, 4):
                    OP("dve", "scalar_tensor_tensor", [L, MG, E_], [E_], out=E_.t[:], in0=L.t[:, 4 + 8 * g_:12 + 8 * g_], scalar=MG.t[:, g_:g_ + 1],
                       in1=E_.t[:], op0=ALU.mult, op1=ALU.add)
                OP("dve", "tensor_reduce", [E_], [RS], out=RS.t[:, 4:5], in_=E_.t[:], axis=AX.X, op=ALU.max)
                OP("dve", "tensor_scalar", [E_, RS], [M1], out=M1.t[:], in0=E_.t[:], scalar1=RS.t[:, 4:5], scalar2=None, op0=ALU.is_equal)
                OP("dve", "scalar_tensor_tensor", [M1, E_], [E2], out=E2.t[:], in0=M1.t[:], scalar=-1e30, in1=E_.t[:], op0=ALU.mult, op1=ALU.add)
                OP("dve", "tensor_reduce", [E2], [RS], out=RS.t[:, 5:6], in_=E2.t[:], axis=AX.X, op=ALU.max)
                OP("dve", "tensor_scalar", [E2, RS], [M2], out=M2.t[:], in0=E2.t[:], scalar1=RS.t[:, 5:6], scalar2=None, op0=ALU.is_equal)
                OP("dve", "tensor_scalar", [RS], [RS], out=RS.t[:, 6:7], in0=RS.t[:, 4:5], scalar1=-1.0, scalar2=None, op0=ALU.mult)
                OP("act", "activation", [RS], [RS], out=RS.t[:, 7:8], in_=RS.t[:, 5:6], func=AF.Exp, bias=RS.t[:, 6:7])
                OP("dve", "tensor_scalar", [RS], [RS], out=RS.t[:, 8:9], in0=RS.t[:, 7:8], scalar1=1.0, scalar2=None, op0=ALU.add)
                OP("dve", "reciprocal", [RS], [RS], out=RS.t[:, 9:10], in_=RS.t[:, 8:9])
                OP("dve", "tensor_tensor", [RS], [route_g], out=route_g.t[:, i, 0:1], in0=RS.t[:, 9:10], in1=RS.t[:, 3:4], op=ALU.mult)
                OP("dve", "tensor_tensor", [RS, route_g], [route_g], out=route_g.t[:, i, 1:2], in0=route_g.t[:, i, 0:1], in1=RS.t[:, 7:8], op=ALU.mult)
                for g_ in range(4):
                    es_ = slice(g_ * 8, (g_ + 1) * 8)
                    OP("dve", "tensor_scalar", [M1, MG], [A1[r2]], out=A1[r2].t[:, es_], in0=M1.t[:], scalar1=MG.t[:, g_:g_ + 1], scalar2=None, op0=ALU.mult)
                    OP("dve", "tensor_scalar", [M2, MG], [A2[r2]], out=A2[r2].t[:, es_], in0=M2.t[:], scalar1=MG.t[:, g_:g_ + 1], scalar2=None, op0=ALU.mult)
                OP("dve", "tensor_tensor", [A1[r2], A2[r2]], [AA[r2]], out=AA[r2].t[:], in0=A1[r2].t[:], in1=A2[r2].t[:], op=ALU.add)
                OP("pe", "matmul", [ctri, AA[r2]], [psm], out=psm.t[:, 64:96], lhsT=ctri.t[:, 384:512], rhs=AA[r2].t[:], start=True, stop=True)
                OP("pe", "matmul", [ones_f, AA[r2]], [psm], out=psm.t[:, 96:128], lhsT=ones_f.t[:], rhs=AA[r2].t[:], start=True, stop=True)
                PC = posc[r2]
                OP("dve", "tensor_tensor", [psm, Rr], [PC], out=PC.t[:], in0=psm.t[:, 64:96], in1=Rr.t[:], op=ALU.add)
                OP("dve", "tensor_tensor", [psm, Rr], [Rr], out=Rr.t[:], in0=psm.t[:, 96:128], in1=Rr.t[:], op=ALU.add)
                OP("dve", "tensor_tensor", [PC, ecap], [PC], out=PC.t[:], in0=PC.t[:], in1=ecap.t[:], op=ALU.add)
                JF = nextjf()
                OP("dve", "scalar_tensor_tensor", [A1[r2], PC], [RS, JF], out=JF.t[:], in0=A1[r2].t[:], scalar=1.0, in1=PC.t[:], op0=ALU.mult, op1=ALU.mult,
                   accum_out=RS.t[:, 10:11])
                JF = nextjf()
                OP("dve", "scalar_tensor_tensor", [A2[r2], PC], [RS, JF], out=JF.t[:], in0=A2[r2].t[:], scalar=1.0, in1=PC.t[:], op0=ALU.mult, op1=ALU.mult,
                   accum_out=RS.t[:, 11:12])
                OP("dve", "tensor_copy", [RS], [route_i], out=route_i.t[:, i, :], in_=RS.t[:, 10:12])
                if stage < 5:
                    return
                for k_ in range(2):
                    DMA("pool", xn2b[rb], [xn2b[rb], route_i], [], name="indirect_dma_start", out=xbuf_d[:, :],
                        out_offset=bass.IndirectOffsetOnAxis(ap=route_i.t[:, i, k_:k_ + 1], axis=0), in_=xn2b[rb].t[:, :], in_offset=None)

            for hh in range(4):
                OP("dve", "tensor_copy", [ctri], [gmask], out=gmask.t[:, hh * 128:(hh + 1) * 128], in_=ctri.t[:, 128:256])
            ORDER3 = debug[8] if (debug and len(debug) > 8) else 0
            if stage >= 1 and ORDER3 == 1:
                for j in range(min(3, NTILE)):
                    load_x(j)
                for i in range(NTILE):
                    if i + 3 < NTILE:
                        load_x(i + 3)
                    s1a(i)
                    s1b(i)
                    s2(i)
                    s3(i)
            elif stage >= 1:
                load_x(0)
                load_x(1)
                s1a(0)
                s1a(1)
                if stage >= 2:
                    s1b(0)
                for i in range(NTILE):
                    if i + 2 < NTILE:
                        load_x(i + 2)
                        s1a(i + 2)
                    if i + 1 < NTILE and stage >= 2:
                        s1b(i + 1)
                    if stage >= 3:
                        s2(i)
                        if i >= 1:
                            s3(i - 1)
                if stage >= 3:
                    s3(NTILE - 1)
            sc.run_block("p3")

        if stage >= 6:
            with ExitStack() as e4:
                NEX = NE if not (debug and len(debug) > 7) else debug[7]
                RW = 4
                wall = mk(e4, "wall", [128, 6144], BF16, RW)
                wgu = [TB(w_.t[:, 0:4096].rearrange("p (c n) -> p c n", n=512), "x") for w_ in wall]
                wd = [TB(w_.t[:, 4096:6144].rearrange("p (c n) -> p c n", n=1024), "x") for w_ in wall]
                for k_ in range(RW):
                    wgu[k_].b = wall[k_].b
                    wd[k_].b = wall[k_].b
                xe = mk(e4, "xe", [128, 3, D], BF16, 3)
                xeT = mk(e4, "xeT", [128, KC, CAP], BF16, 2)
                sgm = mk(e4, "sgm", [128, CAP], F32, 2)
                hT = mk(e4, "hT", [128, 2, CAP], BF16, 2)
                ye = mk(e4, "ye", [128, 3, D], BF16, 2)
                ptp4 = mk(e4, "ptp4", [128, KC, 128], BF16, psum=True)
                pg = mk(e4, "pg", [128, 512], F32, 4, psum=True)
                py = mk(e4, "py", [128, 512], F32, 3, psum=True)
                c4 = {"py": 0}

                def load_w(e):
                    s_ = e % RW
                    DMA("pool", wall[s_], [], [wall[s_]], out=wall[s_].t[:], in_=wbf_d[e])

                def load_xe(e):
                    s_ = e % 3
                    DMA("sp", xe[s_], [], [xe[s_]], out=xe[s_].t[:], in_=xbuf_d[e * CAP:(e + 1) * CAP, :].rearrange("(s p) d -> p s d", p=128))

                def stA(e):
                    s_ = e % 2
                    xev = xe[e % 3].t[:].rearrange("p s (k c) -> p s c k", c=KC)
                    for s in range(3):
                        for c in range(KC):
                            OP("pe", "transpose", [xe[e % 3], B_ident], [ptp4], out=ptp4.t[:, c, :], in_=xev[:, s, c, :], identity=ident_b[:])
                        OP("act", "activation", [ptp4], [xeT[s_]], out=xeT[s_].t[:, :, s * 128:(s + 1) * 128], in_=ptp4.t[:], func=AF.Copy)

                def stB(e):
                    s_ = e % 2
                    w_ = e % RW
                    wv = wgu[w_].t[:].rearrange("p c (w j m) -> p c w m j", w=2, m=2)
                    for m in range(2):
                        for which in range(2):
                            P_ = pg[which * 2 + m]
                            for c in range(KC):
                                OP("pe", "matmul", [wgu[w_], xeT[s_]], [P_], out=P_.t[:, 0:CAP], lhsT=wv[:, c, which, m, :],
                                   rhs=xeT[s_].t[:, c, :], start=(c == 0), stop=(c == KC - 1))
                        G, U, SG = pg[m], pg[2 + m], sgm[m]
                        OP("act", "activation", [G], [SG], out=SG.t[:], in_=G.t[:, 0:CAP], func=AF.Exp, scale=-1.0)
                        OP("act", "activation", [SG], [SG], out=SG.t[:], in_=SG.t[:], func=AF.Ln, bias=1.0)
                        OP("act", "activation", [SG], [SG], out=SG.t[:], in_=SG.t[:], func=AF.Exp, scale=-1.0)
                        OP("dve", "tensor_tensor", [G, SG], [SG], out=SG.t[:], in0=G.t[:, 0:CAP], in1=SG.t[:], op=ALU.mult)
                        OP("dve", "tensor_tensor", [U, SG], [hT[s_]], out=hT[s_].t[:, m, :], in0=U.t[:, 0:CAP], in1=SG.t[:], op=ALU.mult)

                def stC(e):
                    s_ = e % 2
                    for s in range(3):
                        for half in range(2):
                            Y = py[c4["py"] % 3]
                            c4["py"] += 1
                            for m in range(2):
                                OP("pe", "matmul", [hT[s_], wd[e % RW]], [Y], out=Y.t[:], lhsT=hT[s_].t[:, m, s * 128:(s + 1) * 128],
                                   rhs=wd[e % RW].t[:, m, half * 512:(half + 1) * 512], start=(m == 0), stop=(m == 1))
                            if half == 0:
                                OP("act", "activation", [Y], [ye[s_]], out=ye[s_].t[:, s, 0:512], in_=Y.t[:], func=AF.Copy)
                            else:
                                OP("dve", "tensor_copy", [Y], [ye[s_]], out=ye[s_].t[:, s, 512:1024], in_=Y.t[:])
                    DMA("sp", ye[s_], [ye[s_]], [], out=ybuf_d[e * CAP:(e + 1) * CAP, :].rearrange("(s p) d -> p s d", p=128), in_=ye[s_].t[:])

                for e in range(min(RW, NEX)):
                    load_w(e)
                for e in range(min(3, NEX)):
                    load_xe(e)
                stA(0)
                for e in range(NEX):
                    if e + 1 < NEX:
                        stA(e + 1)
                    stB(e)
                    if e >= 1:
                        stC(e - 1)
                        if e - 1 + RW < NEX:
                            load_w(e - 1 + RW)
                    if e + 3 < NEX:
                        load_xe(e + 3)
                stC(NEX - 1)
                sc.run_block("p4")

        if stage >= 7:
            with ExitStack() as e5:
                gf = mk(e5, "gf", [128, D], F32)
                R5 = 5
                hh = mk(e5, "hh", [128, D], F32, R5)
                yy = [mk(e5, "yy%d" % k_, [128, D], BF16, R5) for k_ in range(2)]
                hf = mk(e5, "hf", [128, D], F32, 2)
                ob = mk(e5, "ob", [128, D], F32, 3)
                st5 = mk(e5, "st5", [128, 4], F32, 2)
                B_out = Buf("out")
                DMA("sp", gf, [], [gf], out=gf.t[:], in_=g_fin.partition_broadcast(128))

                def load5(i):
                    s_ = i % R5
                    DMA("sp", hh[s_], [], [hh[s_]], out=hh[s_].t[:], in_=h1_d[i * 128:(i + 1) * 128, :])
                    for k_ in range(2):
                        DMA("pool", yy[k_][s_], [route_i], [yy[k_][s_]], name="indirect_dma_start", out=yy[k_][s_].t[:, :], out_offset=None,
                            in_=ybuf_d[:, :], in_offset=bass.IndirectOffsetOnAxis(ap=route_i.t[:, i, k_:k_ + 1], axis=0))

                NT5 = NTILE
                for i in range(min(R5 - 1, NT5)):
                    load5(i)
                for i in range(NT5):
                    if i + R5 - 1 < NT5:
                        load5(i + R5 - 1)
                    s_, r2 = i % R5, i % 2
                    OP("dve", "scalar_tensor_tensor", [yy[0][s_], route_g, hh[s_]], [hf[r2]], out=hf[r2].t[:], in0=yy[0][s_].t[:],
                       scalar=route_g.t[:, i, 0:1], in1=hh[s_].t[:], op0=ALU.mult, op1=ALU.add)
                    OP("dve", "scalar_tensor_tensor", [yy[1][s_], route_g, hf[r2]], [hf[r2]], out=hf[r2].t[:], in0=yy[1][s_].t[:],
                       scalar=route_g.t[:, i, 1:2], in1=hf[r2].t[:], op0=ALU.mult, op1=ALU.add)
                    ST = st5[r2]
                    OP("act", "activation", [hf[r2]], [ST, ob[i % 3]], out=ob[i % 3].t[:], in_=hf[r2].t[:], func=AF.Square, accum_out=ST.t[:, 0:1])
                    OP("act", "activation", [ST], [ST], out=ST.t[:, 1:2], in_=ST.t[:, 0:1], func=AF.Ln, scale=1.0 / D, bias=EPS)
                    OP("act", "activation", [ST], [ST], out=ST.t[:, 2:3], in_=ST.t[:, 1:2], func=AF.Exp, scale=-0.5)
                    OP("dve", "scalar_tensor_tensor", [hf[r2], ST, gf], [ob[i % 3]], out=ob[i % 3].t[:], in0=hf[r2].t[:], scalar=ST.t[:, 2:3], in1=gf.t[:],
                       op0=ALU.mult, op1=ALU.mult)
                    DMA("sp", ob[i % 3], [ob[i % 3]], [], out=out[i * 128:(i + 1) * 128, :], in_=ob[i % 3].t[:])
                sc.run_block("p5")

        if dbg is not None and 3 <= stage <= 5:
            with ExitStack() as ed:
                dt_ = mk(ed, "dbg_t", [128, D], F32)
                for i in range(NTILE):
                    DMA("sp", dt_, [B_h1d], [dt_], out=dt_.t[:], in_=h1_d[i * 128:(i + 1) * 128, :])
                    DMA("sp", dt_, [dt_], [], out=dbg[i * 128:(i + 1) * 128, :], in_=dt_.t[:])
                if stage >= 4:
                    dr = mk(ed, "dbg_r", [128, NT * 2], F32)
                    nn = NTILE * 2
                    OP("dve", "tensor_copy", [route_i], [dr], out=dr.t[:, 0:nn], in_=route_i.t[:].rearrange("p t k -> p (t k)")[:, 0:nn])
                    DMA("sp", dr, [dr], [], out=dbg[S:S + 128, 0:nn], in_=dr.t[:, 0:nn])
                    DMA("sp", route_g, [route_g], [], out=dbg[S:S + 128, 64:64 + nn], in_=route_g.t[:].rearrange("p t k -> p (t k)")[:, 0:nn])
                sc.run_block("dbg")
    return nc


def _consts():
    ident = np.eye(128, dtype=np.float32)
    half = 16
    inv = (10000.0 ** (-np.arange(half, dtype=np.float32) / half)).astype(np.float32)
    rope = np.zeros((128, 2), np.float32)
    for g_ in range(4):
        rope[g_ * 32:g_ * 32 + 16, 0] = inv
        rope[g_ * 32 + 16:g_ * 32 + 32, 0] = inv
        rope[g_ * 32:g_ * 32 + 16, 1] = -1.0
        rope[g_ * 32 + 16:g_ * 32 + 32, 1] = 1.0
    tri = np.zeros((128, 512), np.float32)
    kk = np.arange(128)
    tri[:, 0:128] = (kk[:, None] <= kk[None, :]).astype(np.float32)
    tri[:, 128:256] = (((kk[:, None] // 64) == (kk[None, :] // 64)) & (kk[:, None] <= kk[None, :])).astype(np.float32)
    tri[:, 384:512] = (kk[:, None] < kk[None, :]).astype(np.float32)
    same = (kk[:, None] // 64) == (kk[None, :] // 64)
    gla = np.zeros((128, 256), np.float32)
    gla[:, 0:128] = np.where(same & (kk[:, None] <= kk[None, :]), -1.0 / 16.0, 0.0)
    gla[:, 128:256] = np.where(same & (kk[:, None] > kk[None, :]), -1.0 / 16.0, 0.0)
    ecap = np.tile((np.arange(32, dtype=np.float32) * CAP)[None, :], (128, 1))
    return ident, rope, tri, gla, ecap


def prepare_inputs(inp):
    f = lambda a: np.ascontiguousarray(np.asarray(a, dtype=np.float32))
    w_in = f(inp["w_in"][0])
    gq, gk, gv, glr, gog, cq, ckv, kr = np.split(w_in, np.cumsum([256, 256, 512, 16, 512, 384, 256])[:], axis=1)
    kr_sw = np.concatenate([kr[:, 16:], kr[:, :16]], axis=1)
    wuq = f(inp["mla_w_uq"][0]).reshape(384, 8, 96)
    wuq_r = wuq[:, :, 64:]
    wuq_sw = np.concatenate([wuq_r[:, :, 16:], wuq_r[:, :, :16]], axis=2)
    wuq_all = np.concatenate([wuq, wuq_sw], axis=2).reshape(384, 8 * 128)
    wukv = f(inp["mla_w_ukv"][0]).reshape(256, 8, 128)
    ident, rope, tri, gla, ecap = _consts()
    common = {
        "g_attn": f(inp["attn_norm_w"]).reshape(1, D),
        "g_ffn": f(inp["ffn_norm_w"]).reshape(1, D),
        "g_fin": f(inp["final_norm_w"]).reshape(1, D),
        "g_q": f(inp["mla_q_norm_w"]).reshape(1, 384),
        "g_kv": f(inp["mla_kv_norm_w"]).reshape(1, 256),
        "g_gla": f(inp["gla_norm_w"]).reshape(1, 128),
        "w_mla": f(np.concatenate([cq, ckv], axis=1)),
        "w_kr": f(np.concatenate([kr, kr_sw], axis=1)),
        "w_gf": f(np.concatenate([gq, gk, glr], axis=1)),
        "w_gt": f(np.concatenate([gk, gv, gog], axis=1)),
        "w_uq": f(wuq_all),
        "w_ukn": f(wukv[:, :, :64].reshape(256, 512)),
        "w_uv": f(wukv[:, :, 64:].reshape(256, 512)),
        "gate_up": f(np.concatenate([inp["gla_gate_up"][0], np.asarray(inp["gla_gate_bias"][0]).reshape(1, 256)], axis=0)),
        "w_out": f(inp["w_out"][0]),
        "w_rt": f(np.concatenate([inp["router_group_w"][0], inp["router_expert_w"][0]], axis=1)),
        "b_rt": f(np.concatenate([inp["router_group_b"][0], inp["router_expert_b"][0]]).reshape(1, 36)),
        "w_eg": f(inp["expert_w_gate"][0]),
        "w_eu": f(inp["expert_w_up"][0]),
        "w_ed": f(inp["expert_w_down"][0]),
        "c_ident": ident,
        "c_rope": rope,
        "c_tri": tri,
        "c_gla": gla,
        "c_ecap": ecap,
    }
    xs_ = np.asarray(inp["x"], dtype=np.float32)
    ps_ = np.asarray(inp["positions"], dtype=np.int32)
    maps = []
    for c in range(8):
        m = dict(common)
        m["x"] = np.ascontiguousarray(xs_[c])
        m["pos"] = np.ascontiguousarray(ps_[c].reshape(1, S))
        maps.append(m)
    return maps


def kernel(**inputs):
    nc = build_program()
    maps = prepare_inputs(inputs)
    res = run_bass_kernel_spmd(nc, maps, core_ids=list(range(8)))
    return np.stack([np.asarray(r["out"]).reshape(S, D) for r in res.results], axis=0).astype(np.float32)
```
